# Optimizing a Trainium2 kernel written in Bass

```python
import math
import jax, jax.numpy as jnp
from jax import lax
import numpy as np

D_MODEL = 2048
BATCH = 2
SEQ = 8192
DEPTH = 1

HEAD_DIM = 128
ROT_DIM = HEAD_DIM // 4
ROPE_THETA = 500000.0
NEG_INF = -1e30
BIG = 1e30
EPS = 1e-5

DIFF_HEADS = 4
DIFF_VDIM = 2 * HEAD_DIM
DIFF_Q_BLOCK = 128

NSA_HEADS = 8
NSA_KV_GROUPS = 2
NSA_HPG = NSA_HEADS // NSA_KV_GROUPS
NSA_Q_BLOCK = 64
CMP_BLOCK = 32
CMP_STRIDE = 16
CMP_HIDDEN = 256
SLC_BLOCK = 64
SLC_TOPK = 16
WINDOW = 512

D_MIX = DIFF_HEADS * DIFF_VDIM + NSA_HEADS * HEAD_DIM
DIFF_QK_COLS = DIFF_HEADS * 2 * HEAD_DIM
DIFF_V_COLS = DIFF_HEADS * DIFF_VDIM
NSA_Q_COLS = NSA_HEADS * HEAD_DIM
NSA_KV_COLS = NSA_KV_GROUPS * HEAD_DIM
NSA_GATE_COLS = NSA_HEADS * 3
IN_SPLITS = (DIFF_QK_COLS, DIFF_QK_COLS, DIFF_V_COLS, NSA_Q_COLS,
             NSA_KV_COLS, NSA_KV_COLS, NSA_KV_COLS, NSA_KV_COLS, NSA_KV_COLS, NSA_KV_COLS,
             NSA_GATE_COLS)
IN_COLS = sum(IN_SPLITS)

N_GROUPS = 4
EXPERTS_PER_GROUP = 8
N_EXPERTS = N_GROUPS * EXPERTS_PER_GROUP
EXPERT_HIDDEN = 512
TOPK_IN_GROUP = 2
MOE_TOKEN_CHUNK = 1024

DN_ALPHA = (2.0 * DEPTH) ** 0.25
DN_BETA = (8.0 * DEPTH) ** -0.25

kernel_name = "hybrid_diffattn_nsa_hiermoe_deepnorm"


def _layernorm(x, g, b):
    xf = x.astype(jnp.float32)
    mu = jnp.mean(xf, axis=-1, keepdims=True)
    var = jnp.mean(jnp.square(xf - mu), axis=-1, keepdims=True)
    return ((xf - mu) * lax.rsqrt(var + EPS) * g.astype(jnp.float32) + b.astype(jnp.float32)).astype(x.dtype)


def _rope_cos_sin(seq_len):
    inv_freq = ROPE_THETA ** (-jnp.arange(0, ROT_DIM, 2, dtype=jnp.float32) / ROT_DIM)
    ang = jnp.arange(seq_len, dtype=jnp.float32)[:, None] * inv_freq[None, :]
    return jnp.cos(ang), jnp.sin(ang)


def _partial_rope(t, cos, sin):
    half = ROT_DIM // 2
    c = cos[None, :, None, :]
    s = sin[None, :, None, :]
    t1 = t[..., :half].astype(jnp.float32)
    t2 = t[..., half:ROT_DIM].astype(jnp.float32)
    rot = jnp.concatenate([t1 * c - t2 * s, t2 * c + t1 * s], axis=-1).astype(t.dtype)
    return jnp.concatenate([rot, t[..., ROT_DIM:]], axis=-1)


def _diff_attention(q, k, v, lam, subln_g, lambda_init):
    B, T, H, _, Dh = q.shape
    Dv = v.shape[-1]
    nb = T // DIFF_Q_BLOCK
    scale = Dh ** -0.5
    q_blocks = jnp.moveaxis(q.reshape(B, nb, DIFF_Q_BLOCK, H, 2, Dh), 1, 0)
    kpos = jnp.arange(T)

    def block(args):
        i, qi = args
        qpos = i * DIFF_Q_BLOCK + jnp.arange(DIFF_Q_BLOCK)
        s = jnp.einsum('bqhcd,bkhcd->bhcqk', qi, k, preferred_element_type=jnp.float32) * scale
        mask = kpos[None, :] <= qpos[:, None]
        p = jax.nn.softmax(jnp.where(mask, s, NEG_INF), axis=-1)
        a = p[:, :, 0] - lam * p[:, :, 1]
        return jnp.einsum('bhqk,bkhe->bqhe', a.astype(v.dtype), v)

    o = lax.map(block, (jnp.arange(nb), q_blocks))
    o = jnp.moveaxis(o, 0, 1).reshape(B, T, H, Dv)
    of = o.astype(jnp.float32)
    of = of * lax.rsqrt(jnp.mean(jnp.square(of), axis=-1, keepdims=True) + EPS) * subln_g.astype(jnp.float32)
    of = of * (1.0 - lambda_init)
    return of.astype(v.dtype).reshape(B, T, H * Dv)


def _compress(t, pos, w1, b1, w2, b2):
    B, T, G, Dh = t.shape
    n_cmp = (T - CMP_BLOCK) // CMP_STRIDE + 1
    idx = jnp.arange(n_cmp)[:, None] * CMP_STRIDE + jnp.arange(CMP_BLOCK)[None, :]
    blk = t[:, idx] + pos[None, None, :, None, :]
    blk = jnp.transpose(blk, (0, 1, 3, 2, 4)).reshape(B, n_cmp, G, CMP_BLOCK * Dh)
    h = jax.nn.gelu(jnp.einsum('bcgi,ih->bcgh', blk, w1) + b1)
    return jnp.einsum('bcgh,hd->bcgd', h, w2) + b2


def _nsa_attention(q, q_rot, k_cmp, v_cmp, k_slc, v_slc, k_win, v_win, gates,
                   cmp_pos, cmp_w1, cmp_b1, cmp_w2, cmp_b2):
    B, T, H, Dh = q.shape
    G = NSA_KV_GROUPS
    QB = NSA_Q_BLOCK
    scale = Dh ** -0.5
    kc = _compress(k_cmp, cmp_pos[0], cmp_w1[0], cmp_b1[0], cmp_w2[0], cmp_b2[0])
    vc = _compress(v_cmp, cmp_pos[1], cmp_w1[1], cmp_b1[1], cmp_w2[1], cmp_b2[1])
    n_cmp = kc.shape[1]
    n_slc = T // SLC_BLOCK
    top_k = min(SLC_TOPK, n_slc)
    ci = np.arange(n_cmp)[:, None] * CMP_STRIDE
    sj = np.arange(n_slc)[None, :] * SLC_BLOCK
    overlap = jnp.asarray((ci < sj + SLC_BLOCK) & (ci + CMP_BLOCK > sj), dtype=jnp.float32)
    cmp_end = jnp.arange(n_cmp) * CMP_STRIDE + CMP_BLOCK - 1
    blk_start = jnp.arange(n_slc) * SLC_BLOCK
    blk_id = jnp.arange(n_slc)
    ks_blk = jnp.transpose(k_slc.reshape(B, n_slc, SLC_BLOCK, G, Dh), (0, 3, 1, 2, 4))
    vs_blk = jnp.transpose(v_slc.reshape(B, n_slc, SLC_BLOCK, G, Dh), (0, 3, 1, 2, 4))
    kw_pad = jnp.pad(k_win, ((0, 0), (WINDOW, 0), (0, 0), (0, 0)))
    vw_pad = jnp.pad(v_win, ((0, 0), (WINDOW, 0), (0, 0), (0, 0)))
    b_idx = jnp.arange(B)[:, None, None, None]
    g_idx = jnp.arange(G)[None, :, None, None]
    nb = T // QB

    def to_blocks(t):
        return jnp.moveaxis(t.reshape(B, nb, QB, *t.shape[2:]), 1, 0)

    def block(args):
        i, qb, qrb, gb = args
        qpos = i * QB + jnp.arange(QB)
        qg = qb.reshape(B, QB, G, NSA_HPG, Dh)
        qrg = qrb.reshape(B, QB, G, NSA_HPG, Dh)
        s_c = jnp.einsum('bqghd,bcgd->bghqc', qg, kc, preferred_element_type=jnp.float32) * scale
        valid_c = cmp_end[None, :] <= qpos[:, None]
        p_c = jax.nn.softmax(jnp.where(valid_c, s_c, NEG_INF), axis=-1) * valid_c
        o_c = jnp.einsum('bghqc,bcgd->bqghd', p_c.astype(vc.dtype), vc)
        imp = jnp.einsum('bghqc,cs->bgqs', p_c, overlap)
        cur = qpos // SLC_BLOCK
        valid_s = blk_start[None, :] <= qpos[:, None]
        forced = (blk_id[None, :] == 0) | (blk_id[None, :] == cur[:, None]) | (blk_id[None, :] == cur[:, None] - 1)
        score = jnp.where(valid_s & forced, BIG, jnp.where(valid_s, imp, NEG_INF))
        _, sel = lax.top_k(score, top_k)
        ks = ks_blk[b_idx, g_idx, sel].reshape(B, G, QB, top_k * SLC_BLOCK, Dh)
        vs = vs_blk[b_idx, g_idx, sel].reshape(B, G, QB, top_k * SLC_BLOCK, Dh)
        tok = (sel[..., None] * SLC_BLOCK + jnp.arange(SLC_BLOCK)).reshape(B, G, QB, top_k * SLC_BLOCK)
        s_s = jnp.einsum('bqghd,bgqnd->bghqn', qrg, ks, preferred_element_type=jnp.float32) * scale
        mask_s = (tok <= qpos[None, None, :, None])[:, :, None]
        p_s = jax.nn.softmax(jnp.where(mask_s, s_s, NEG_INF), axis=-1)
        o_s = jnp.einsum('bghqn,bgqnd->bqghd', p_s.astype(vs.dtype), vs)
        start = i * QB
        kw = lax.dynamic_slice_in_dim(kw_pad, start, WINDOW + QB, axis=1)
        vw = lax.dynamic_slice_in_dim(vw_pad, start, WINDOW + QB, axis=1)
        kpos = start - WINDOW + jnp.arange(WINDOW + QB)
        dpos = qpos[:, None] - kpos[None, :]
        mask_w = (dpos >= 0) & (dpos < WINDOW) & (kpos[None, :] >= 0)
        s_w = jnp.einsum('bqghd,bkgd->bghqk', qrg, kw, preferred_element_type=jnp.float32) * scale
        p_w = jax.nn.softmax(jnp.where(mask_w, s_w, NEG_INF), axis=-1)
        o_w = jnp.einsum('bghqk,bkgd->bqghd', p_w.astype(vw.dtype), vw)
        gg = gb.reshape(B, QB, G, NSA_HPG, 3)
        o = gg[..., 0:1] * o_c + gg[..., 1:2] * o_s + gg[..., 2:3] * o_w
        return o.reshape(B, QB, H * Dh)

    o = lax.map(block, (jnp.arange(nb), to_blocks(q), to_blocks(q_rot), to_blocks(gates)))
    return jnp.moveaxis(o, 0, 1).reshape(B, T, H * Dh)


def _mixer(x, w_in, w_out, diff_lambda, diff_subln_g, cmp_pos, cmp_w1, cmp_b1, cmp_w2, cmp_b2,
           cos, sin, lambda_init):
    B, T, _ = x.shape
    proj = jnp.einsum('btd,dc->btc', x, w_in)
    offsets = [int(o) for o in np.cumsum(IN_SPLITS)[:-1]]
    dq, dk, dv, nq, kc, vc, ks, vs, kw, vw, ng = jnp.split(proj, offsets, axis=-1)
    dq = _partial_rope(dq.reshape(B, T, DIFF_HEADS * 2, HEAD_DIM), cos, sin).reshape(B, T, DIFF_HEADS, 2, HEAD_DIM)
    dk = _partial_rope(dk.reshape(B, T, DIFF_HEADS * 2, HEAD_DIM), cos, sin).reshape(B, T, DIFF_HEADS, 2, HEAD_DIM)
    dv = dv.reshape(B, T, DIFF_HEADS, DIFF_VDIM)
    lf = diff_lambda.astype(jnp.float32)
    lam = jnp.exp(jnp.sum(lf[0] * lf[1])) - jnp.exp(jnp.sum(lf[2] * lf[3])) + lambda_init
    o_diff = _diff_attention(dq, dk, dv, lam, diff_subln_g, lambda_init)
    nq = nq.reshape(B, T, NSA_HEADS, HEAD_DIM)
    nq_rot = _partial_rope(nq, cos, sin)
    kvr = lambda t: t.reshape(B, T, NSA_KV_GROUPS, HEAD_DIM)
    ks = _partial_rope(kvr(ks), cos, sin)
    kw = _partial_rope(kvr(kw), cos, sin)
    gates = jax.nn.sigmoid(ng.astype(jnp.float32)).astype(x.dtype).reshape(B, T, NSA_HEADS, 3)
    o_nsa = _nsa_attention(nq, nq_rot, kvr(kc), kvr(vc), ks, kvr(vs), kw, kvr(vw), gates,
                           cmp_pos, cmp_w1, cmp_b1, cmp_w2, cmp_b2)
    o = jnp.concatenate([o_diff, o_nsa], axis=-1)
    return jnp.einsum('btc,cd->btd', o, w_out)


def _hier_moe(x, router_group, router_expert, w_gate, w_up, w_down):
    B, T, D = x.shape
    xt = x.reshape(B * T, D)
    N = xt.shape[0]
    g_logits = jnp.einsum('nd,dg->ng', xt, router_group).astype(jnp.float32)
    g_prob = jax.nn.softmax(g_logits, axis=-1)
    g_w, g_sel = lax.top_k(g_prob, 1)
    e_logits = jnp.einsum('nd,de->ne', xt, router_expert).astype(jnp.float32).reshape(N, N_GROUPS, EXPERTS_PER_GROUP)
    e_logits = jnp.take_along_axis(e_logits, g_sel[:, :, None], axis=1)[:, 0]
    e_prob = jax.nn.softmax(e_logits, axis=-1)
    e_w, e_sel = lax.top_k(e_prob, TOPK_IN_GROUP)
    e_w = e_w / jnp.sum(e_w, axis=-1, keepdims=True)
    weight = g_w * e_w
    global_idx = g_sel * EXPERTS_PER_GROUP + e_sel
    dense_w = jnp.sum(jax.nn.one_hot(global_idx, N_EXPERTS, dtype=jnp.float32) * weight[..., None], axis=1)
    chunk = math.gcd(N, MOE_TOKEN_CHUNK)
    xc = xt.reshape(N // chunk, chunk, D)
    wc = dense_w.reshape(N // chunk, chunk, N_EXPERTS)

    def run(args):
        xb, wb = args
        h = jax.nn.silu(jnp.einsum('nd,edf->nef', xb, w_gate)) * jnp.einsum('nd,edf->nef', xb, w_up)
        h = h * wb[..., None].astype(h.dtype)
        return jnp.einsum('nef,efd->nd', h, w_down)

    y = lax.map(run, (xc, wc))
    return y.reshape(B, T, D)


def setup_inputs(seed: int = 0) -> dict:
    key = jax.random.key(seed)
    ks = jax.random.split(key, 19)
    L = DEPTH

    def nrm(k, shape, scale):
        return jax.random.normal(k, shape, jnp.float32) * scale

    return {
        "x": nrm(ks[0], (BATCH, SEQ, D_MODEL), 1.0),
        "w_in": nrm(ks[1], (L, D_MODEL, IN_COLS), D_MODEL ** -0.5),
        "diff_lambda": nrm(ks[2], (L, 4, HEAD_DIM), 0.1),
        "diff_subln_g": 1.0 + nrm(ks[3], (L, DIFF_VDIM), 0.02),
        "cmp_pos": nrm(ks[4], (L, 2, CMP_BLOCK, HEAD_DIM), 0.1),
        "cmp_w1": nrm(ks[5], (L, 2, CMP_BLOCK * HEAD_DIM, CMP_HIDDEN), (CMP_BLOCK * HEAD_DIM) ** -0.5),
        "cmp_b1": nrm(ks[6], (L, 2, CMP_HIDDEN), 0.01),
        "cmp_w2": nrm(ks[7], (L, 2, CMP_HIDDEN, HEAD_DIM), CMP_HIDDEN ** -0.5),
        "cmp_b2": nrm(ks[8], (L, 2, HEAD_DIM), 0.01),
        "w_out": nrm(ks[9], (L, D_MIX, D_MODEL), DN_BETA * D_MIX ** -0.5),
        "ln1_g": 1.0 + nrm(ks[10], (L, D_MODEL), 0.02),
        "ln1_b": nrm(ks[11], (L, D_MODEL), 0.02),
        "router_group": nrm(ks[12], (L, D_MODEL, N_GROUPS), D_MODEL ** -0.5),
        "router_expert": nrm(ks[13], (L, D_MODEL, N_EXPERTS), D_MODEL ** -0.5),
        "expert_w_gate": nrm(ks[14], (L, N_EXPERTS, D_MODEL, EXPERT_HIDDEN), D_MODEL ** -0.5),
        "expert_w_up": nrm(ks[15], (L, N_EXPERTS, D_MODEL, EXPERT_HIDDEN), D_MODEL ** -0.5),
        "expert_w_down": nrm(ks[16], (L, N_EXPERTS, EXPERT_HIDDEN, D_MODEL), DN_BETA * EXPERT_HIDDEN ** -0.5),
        "ln2_g": 1.0 + nrm(ks[17], (L, D_MODEL), 0.02),
        "ln2_b": nrm(ks[18], (L, D_MODEL), 0.02),
    }


def reference(x, w_in, diff_lambda, diff_subln_g, cmp_pos, cmp_w1, cmp_b1, cmp_w2, cmp_b2, w_out,
              ln1_g, ln1_b, router_group, router_expert, expert_w_gate, expert_w_up, expert_w_down,
              ln2_g, ln2_b):
    cos, sin = _rope_cos_sin(x.shape[1])
    for l in range(DEPTH):
        lambda_init = 0.8 - 0.6 * math.exp(-0.3 * l)
        h = _mixer(x, w_in[l], w_out[l], diff_lambda[l], diff_subln_g[l], cmp_pos[l], cmp_w1[l],
                   cmp_b1[l], cmp_w2[l], cmp_b2[l], cos, sin, lambda_init)
        x = _layernorm(DN_ALPHA * x + h, ln1_g[l], ln1_b[l])
        h = _hier_moe(x, router_group[l], router_expert[l], expert_w_gate[l], expert_w_up[l], expert_w_down[l])
        x = _layernorm(DN_ALPHA * x + h, ln2_g[l], ln2_b[l])
    return x
```

```python
import math
import numpy as np
import ml_dtypes
import concourse.bass as bass
import concourse.mybir as mybir
from concourse.bass_utils import run_bass_kernel_spmd

F32 = mybir.dt.float32
BF16 = mybir.dt.bfloat16
ALU = mybir.AluOpType
AF = mybir.ActivationFunctionType
AX = mybir.AxisListType

D = 2048
T = 8192
TO = 2048
NQT = 16
SCALE = 128 ** -0.5
EPS = 1e-5
DN_ALPHA = 2.0 ** 0.25
LAMBDA_INIT = 0.8 - 0.6 * math.exp(0.0)
NEG_BIAS = -30000.0


class Sched:
    ENGS = ("pe", "act", "dve", "pool", "sp")

    def __init__(self, nc):
        self.nc = nc
        self.ops = []
        self.last_writer = {}
        self.readers = {}
        self.dma_sems = {}
        self.last_on_eng = {}

    def _add(self, eng, fn, reads, writes, dma_sem=None, ndma=0, extra_deps=()):
        idx = len(self.ops)
        writes = tuple(writes) + tuple(k for k in reads if isinstance(k, tuple) and k[0] == "ps" and k not in writes)
        deps = set(extra_deps)
        for k in reads:
            w = self.last_writer.get(k)
            if w is not None:
                deps.add(w)
        for k in writes:
            w = self.last_writer.get(k)
            if w is not None:
                deps.add(w)
            lastc = {}
            for rd in self.readers.get(k, ()):
                ro = self.ops[rd]
                if ro["dma_sem"] is not None:
                    deps.add(rd)
                else:
                    lastc[ro["eng"]] = max(lastc.get(ro["eng"], -1), rd)
            deps.update(lastc.values())
        deps.discard(idx)
        for k in writes:
            self.last_writer[k] = idx
            self.readers[k] = []
        for k in reads:
            self.readers.setdefault(k, []).append(idx)
        self.ops.append(dict(eng=eng, fn=fn, deps=deps, dma_sem=dma_sem, ndma=ndma, sig=False))
        if fn is not None:
            self.last_on_eng[eng if dma_sem is None else ("dma", dma_sem)] = idx
        return idx

    def op(self, eng, fn, reads=(), writes=()):
        return self._add(eng, fn, tuple(reads), tuple(writes))

    def dma(self, eng, sem_name, fns, reads=(), writes=()):
        if sem_name not in self.dma_sems:
            self.dma_sems[sem_name] = None
        return self._add(eng, fns, tuple(reads), tuple(writes), dma_sem=sem_name, ndma=len(fns))

    def barrier(self):
        lasts = list(self.last_on_eng.values())
        for e in self.ENGS:
            self._add(e, None, (), (), extra_deps=lasts)

    def emit(self):
        nc = self.nc
        for o in self.ops:
            for d in o["deps"]:
                do = self.ops[d]
                if do["dma_sem"] is None:
                    if not (do["eng"] == "pe" and o["eng"] == "pe" and o["dma_sem"] is None):
                        do["sig"] = True
        cnt = {e: 0 for e in self.ENGS}
        dcnt = {k: 0 for k in self.dma_sems}
        for o in self.ops:
            if o["dma_sem"] is not None:
                dcnt[o["dma_sem"]] += 16 * o["ndma"]
                o["ev"] = ("d", o["dma_sem"], dcnt[o["dma_sem"]])
            elif o["sig"]:
                cnt[o["eng"]] += 1
                o["ev"] = ("e", o["eng"], cnt[o["eng"]])
            else:
                o["ev"] = None
        import contextlib
        with contextlib.ExitStack() as st:
            esem = {e: st.enter_context(nc.semaphore("s_" + e)) for e in self.ENGS}
            dsem = {k: st.enter_context(nc.semaphore("d_" + k)) for k in self.dma_sems}
            block = st.enter_context(nc.Block())
            ops = self.ops

            def run(ename):
                def body(eng):
                    waited = {}
                    for o in ops:
                        if o["eng"] != ename:
                            continue
                        for d in sorted(o["deps"]):
                            do = ops[d]
                            ev = do["ev"]
                            if ev is None:
                                continue
                            if ev[0] == "e" and ev[1] == "pe" and ename == "pe" and o["dma_sem"] is None:
                                continue
                            key = (ev[0], ev[1])
                            if waited.get(key, 0) >= ev[2]:
                                continue
                            waited[key] = ev[2]
                            sem = esem[ev[1]] if ev[0] == "e" else dsem[ev[1]]
                            eng.wait_ge(sem, ev[2])
                        if o["fn"] is None:
                            if o["ev"] is not None:
                                eng.nop().then_inc(esem[ename], 1)
                            continue
                        if o["dma_sem"] is not None:
                            for f in o["fn"]:
                                f(eng).then_inc(dsem[o["dma_sem"]], 16)
                        else:
                            ins = o["fn"](eng)
                            if o["ev"] is not None:
                                ins.then_inc(esem[ename], 1)
                return body

            block.tensor(run("pe"))
            block.scalar(run("act"))
            block.vector(run("dve"))
            block.gpsimd(run("pool"))
            block.sync(run("sp"))


class Arena:
    def __init__(self, nc, nelem_bf16):
        self.ap = nc.alloc_sbuf_tensor("arena", [128, nelem_bf16], BF16).ap()
        self.n = nelem_bf16
        self.off = 0

    def reset(self):
        self.off = 0

    def get(self, shape, dtype, parts=128):
        n = 1
        for s in shape:
            n *= s
        sz = n * (2 if dtype == F32 else 1)
        sz = (sz + 15) // 16 * 16
        assert self.off + sz <= self.n, ("arena overflow", self.off, sz, self.n)
        v = self.ap[0:parts, self.off:self.off + sz]
        self.off += sz
        if dtype == F32:
            v = v.bitcast(F32)
        v = v[:, 0:n]
        if len(shape) == 2:
            v = v.rearrange("p (a b) -> p a b", b=shape[1])
        elif len(shape) == 3:
            v = v.rearrange("p (a b c) -> p a b c", b=shape[1], c=shape[2])
        return v


C_DQ, C_DK, C_DV, C_NQ, C_KC, C_VC, C_KS, C_VS, C_KW, C_VW, C_NG = (
    0, 1024, 2048, 3072, 4096, 4352, 4608, 4864, 5120, 5376, 5632)


def build_nc(stop_after=None, debug=False):
    nc = bass.Bass("TRN2", target_bir_lowering=False)
    S = Sched(nc)

    def din(name, shape, dt=F32):
        return nc.dram_tensor(name, list(shape), dt, kind="ExternalInput").ap()

    xb = din("xb", [T, D]); xo = din("xo", [TO, D])
    w_in = din("w_in", [D, 5656]); w_out = din("w_out", [D, D])
    diff_lambda = din("diff_lambda", [4, 128]); subln_g = din("diff_subln_g", [256])
    cmp_pos = din("cmp_pos", [2, 32, 128]); cmp_w1 = din("cmp_w1", [2, 4096, 256])
    cmp_b1 = din("cmp_b1", [2, 256]); cmp_w2 = din("cmp_w2", [2, 256, 128]); cmp_b2 = din("cmp_b2", [2, 128])
    ln1_g = din("ln1_g", [D]); ln1_b = din("ln1_b", [D]); ln2_g = din("ln2_g", [D]); ln2_b = din("ln2_b", [D])
    r_group = din("router_group", [D, 4]); r_expert = din("router_expert", [D, 32])
    lite = stop_after in ("A1", "A2", "A", "B", "C0", "C", "E1")
    if not lite:
        wg = din("expert_w_gate", [32, D, 512]); wu = din("expert_w_up", [32, D, 512]); wd = din("expert_w_down", [32, 512, D])
    rope_k = din("rope_k", [2, 32, T]); rope_q = din("rope_q", [2, 32, TO])
    c_ident = din("c_ident", [128, 128], BF16); c_rsw = din("c_rsw", [32, 32], BF16)
    c_dmask4 = din("c_dmask4", [128, 4, 512], BF16); c_cmask4 = din("c_cmask4", [128, 5, 512], BF16)
    c_wmask4 = din("c_wmask4", [128, 8, 512], BF16)
    c_selvm = din("c_selvm", [128, 16, 128]); c_selfb = din("c_selfb", [128, 16, 128])
    c_ovl1 = din("c_ovl1", [512, 129], BF16); c_E = din("c_E", [128, T], BF16)
    c_sele = din("c_sele", [32, 32, 128], BF16)
    out = nc.dram_tensor("out", [TO, D], F32, kind="ExternalOutput").ap()
    skind = "ExternalOutput" if debug else "Internal"
    xT_s = nc.dram_tensor("xT_s", [16, 128, 16 * 512], BF16).ap()
    KT_s = nc.dram_tensor("KT_s", [16, 128, T], BF16, kind=skind).ap()
    V_s = nc.dram_tensor("V_s", [T, 1536], BF16, kind=skind).ap()
    QT_s = nc.dram_tensor("QT_s", [24, 128, TO], BF16, kind=skind).ap()
    oT_s = nc.dram_tensor("oT_s", [16, 128, TO], BF16, kind=skind).ap()
    x1_s = nc.dram_tensor("x1_s", [TO, D], F32, kind=skind).ap()
    x1T_s = nc.dram_tensor("x1T_s", [16, 128, TO], BF16).ap()

    def sb(name, shape, dt):
        return nc.alloc_sbuf_tensor(name, list(shape), dt).ap()

    ident = sb("ident", [128, 128], BF16); rsw = sb("rsw", [32, 32], BF16)
    gates = sb("gates", [128, 16, 24], F32)
    ones_bf = sb("ones_bf", [128, 128], BF16)
    kcT = [sb("kcT%d" % g, [128, 512], BF16) for g in range(2)]
    VCB = [sb("VCB%d" % g, [128, 4, 128], BF16) for g in range(2)]
    dwT = sb("dwT", [32, TO], BF16)
    A = Arena(nc, 92 * 1024)
    ps = [nc.alloc_psum_tensor("ps%d" % i, [128, 512], F32).ap() for i in range(8)]
    psb = [p.bitcast(BF16) for p in ps]

    def O(eng, method, reads, writes, *a, **kw):
        return S.op(eng, lambda e: getattr(e, method)(*a, **kw), reads, writes)

    def DM(sem, out_, in_, reads, writes, eng="sp", **kw):
        return S.dma(eng, sem, [lambda e: e.dma_start(out=out_, in_=in_, **kw)], reads, writes)

    def DMS(sem, out_, in_, nsplit, reads, writes):
        nt = out_.shape[1]
        step = nt // nsplit
        fns = [(lambda e, i=i: e.dma_start(out=out_[:, i * step:(i + 1) * step, :], in_=in_[:, i * step:(i + 1) * step, :])) for i in range(nsplit)]
        return S.dma("sp", sem, fns, reads, writes)

    def MM(out_, lhsT, rhs, start, stop, reads, writes):
        return S.op("pe", lambda e: e.matmul(out_, lhsT=lhsT, rhs=rhs, start=start, stop=stop, skip_group_check=True), reads, writes)

    def ps3(bank):
        return ps[bank].rearrange("p (h k) -> p h k", k=128)

    def TR(out_, in_, reads, writes):
        return S.op("pe", lambda e: e.transpose(out=out_, in_=in_, identity=ident), list(reads) + ["ident"], writes)

    rr = {"c": 0}

    def cast_eng(choices=("pool", "dve", "act")):
        rr["c"] += 1
        return choices[rr["c"] % len(choices)]

    def COPY(eng, out_, in_, reads, writes):
        if eng == "act":
            return O("act", "activation", reads, writes, out=out_, in_=in_, func=AF.Copy)
        return O(eng, "tensor_copy", reads, writes, out=out_, in_=in_)

    DM("c0", ident, c_ident, [], ["ident"])
    DM("c1", rsw, c_rsw, [], ["rsw"])
    O("pool", "memset", [], ["ones_bf"], ones_bf, 1.0)

    def load_weight_cols(Wsb, col_ranges, wkey, nm):
        ncols = sum(w for _, w in col_ranges)
        stg = [A.get([ncols], F32) for _ in range(2)]
        for dc in range(16):
            s = dc % 2
            fns = []
            off = 0
            for (c0, w) in col_ranges:
                fns.append(lambda e, c0=c0, w=w, off=off, s=s, dc=dc: e.dma_start(
                    out=stg[s][:, off:off + w], in_=w_in[dc * 128:(dc + 1) * 128, c0:c0 + w]))
                off += w
            S.dma("sp", "wst%s%d" % (nm, s), fns, [], [("wstg", nm, s)])
            COPY(cast_eng(), Wsb[:, dc, :], stg[s], [("wstg", nm, s)], [wkey])

    def x_chunk_to_xT(xsrc, tc, xT, xTkey, bufs, nm):
        xst, xbf = bufs
        for t in range(4):
            s = (tc * 4 + t) % 2
            DM("xst%s%d" % (nm, s), xst[s], xsrc[tc * 512 + t * 128: tc * 512 + (t + 1) * 128, :], [], [("xst", s)])
            COPY(cast_eng(("dve", "act")), xbf[s], xst[s], [("xst", s)], [("xbf", s)])
            for hb in range(2):
                bank = hb
                for j in range(8):
                    dc = hb * 8 + j
                    TR(psb[bank][:, j * 128:(j + 1) * 128], xbf[s][:, dc * 128:(dc + 1) * 128], [("xbf", s)], [("ps", bank)])
                COPY(cast_eng(("act", "dve")), xT[:, hb * 8:(hb + 1) * 8, t * 128:(t + 1) * 128],
                     psb[bank].rearrange("p (j k) -> p j k", k=128), [("ps", bank)], [xTkey])

    def rope_fix(dst32, psacc32, ropet, s, keys_r, keys_w, tmpA, tmpB):
        MM(ps[6][0:32, :], rsw[:, :], dst32, True, True, ["rsw"] + keys_w, [("ps", 6)])
        O("dve", "tensor_tensor", [("ps", 6), ("ropet", s)], ["tmpA"], out=tmpA, in0=ps[6][0:32, :], in1=ropet[:, 1, :], op=ALU.mult)
        O("dve", "tensor_tensor", keys_r + [("ropet", s)], ["tmpB"], out=tmpB, in0=psacc32, in1=ropet[:, 0, :], op=ALU.mult)
        O("dve", "tensor_tensor", ["tmpA", "tmpB"], keys_w, out=dst32, in0=tmpA, in1=tmpB, op=ALU.add)

    A.reset()
    Wk = A.get([16, 2048], BF16)
    load_weight_cols(Wk, [(C_DK, 1024), (C_KC, 256), (C_KS, 256), (C_KW, 256), (C_VC, 256)], "Wk", "k")
    xst = [A.get([2048], F32) for _ in range(2)]
    xbf = [A.get([2048], BF16) for _ in range(2)]
    xTs = [A.get([16, 512], BF16) for _ in range(2)]
    Kst = A.get([16, 512], BF16)
    ropet = [A.get([2, 512], F32, parts=32) for _ in range(2)]
    tmpA = A.get([512], F32, parts=32); tmpB = A.get([512], F32, parts=32)
    rope_tiles_k = set(range(0, 8)) | {10, 11, 12, 13}
    for tc in range(16):
        s = tc % 2
        xT = xTs[s]
        x_chunk_to_xT(xb, tc, xT, ("xT", s), (xst, xbf), "a")
        DM("xTst%d" % s, xT_s[tc].rearrange("p (c t) -> p c t", t=512), xT, [("xT", s)], [("xT_s", tc)])
        DM("rope%d" % s, ropet[s], rope_k[:, :, tc * 512:(tc + 1) * 512].rearrange("a p t -> p a t"), [], [("ropet", s)])
        pend = None
        for ct in range(16):
            bank = 2 + ct % 4
            for dc in range(16):
                MM(ps[bank], Wk[:, dc, ct * 128:(ct + 1) * 128], xT[:, dc, :], dc == 0, dc == 15, ["Wk", ("xT", s)], [("ps", bank)])
            COPY(cast_eng(("act", "dve")), Kst[:, ct, :], ps[bank], [("ps", bank)], [("Kst", ct)])
            if pend is not None:
                pend(); pend = None
            if ct in rope_tiles_k:
                pend = (lambda ct=ct, bank=bank, s=s: rope_fix(Kst[0:32, ct, :], ps[bank][0:32, :], ropet[s], s, [("ps", bank)], [("Kst", ct)], tmpA, tmpB))
        if pend is not None:
            pend(); pend = None
        DM("Kstst", KT_s[:, :, tc * 512:(tc + 1) * 512].rearrange("i p t -> p i t"), Kst,
           [("Kst", ct) for ct in range(16)], [("KT_s", tc)])
    S.barrier()
    if stop_after == "A1":
        S.emit(); return nc

    A.reset()
    Wv = A.get([16, 1536], BF16)
    load_weight_cols(Wv, [(C_DV, 1024), (C_VS, 256), (C_VW, 256)], "Wv", "v")
    xTs = [A.get([16, 512], BF16) for _ in range(2)]
    Vst = [A.get([4, 1536], BF16) for _ in range(2)]
    for tc in range(16):
        s = tc % 2
        xT = xTs[s]
        DM("xTld%d" % s, xT, xT_s[tc].rearrange("p (c t) -> p c t", t=512), [("xT_s", tc)], [("xT", s)])
        for t in range(4):
            for nb in range(3):
                bank = 2 + (t * 3 + nb) % 4
                for dc in range(16):
                    MM(ps[bank], xT[:, dc, t * 128:(t + 1) * 128], Wv[:, dc, nb * 512:(nb + 1) * 512], dc == 0, dc == 15,
                       ["Wv", ("xT", s)], [("ps", bank)])
                COPY(cast_eng(("act", "dve")), Vst[s][:, t, nb * 512:(nb + 1) * 512], ps[bank], [("ps", bank)], [("Vst", s)])
        DM("Vstst%d" % s, V_s[tc * 512:(tc + 1) * 512, :].rearrange("(t p) c -> p t c", p=128), Vst[s], [("Vst", s)], [("V_s", tc)])
    S.barrier()
    if stop_after == "A2":
        S.emit(); return nc

    A.reset()
    Wq = A.get([16, 2048], BF16)
    load_weight_cols(Wq, [(C_DQ, 1024), (C_NQ, 1024)], "Wq", "q")
    Wgt = A.get([16, 24], BF16)
    load_weight_cols(Wgt, [(C_NG, 24)], "Wgt", "g")
    xst = [A.get([2048], F32) for _ in range(2)]
    xbf = [A.get([2048], BF16) for _ in range(2)]
    xTs = [A.get([16, 512], BF16) for _ in range(2)]
    Qst = A.get([24, 512], BF16)
    ropet = [A.get([2, 512], F32, parts=32) for _ in range(2)]
    tmpA = A.get([512], F32, parts=32); tmpB = A.get([512], F32, parts=32)
    for oc in range(4):
        s = oc % 2
        xT = xTs[s]
        x_chunk_to_xT(xo, oc, xT, ("xT", s), (xst, xbf), "q")
        DM("rope%d" % s, ropet[s], rope_q[:, :, oc * 512:(oc + 1) * 512].rearrange("a p t -> p a t"), [], [("ropet", s)])
        pend = None
        for ct in range(16):
            bank = 2 + ct % 4
            for dc in range(16):
                MM(ps[bank], Wq[:, dc, ct * 128:(ct + 1) * 128], xT[:, dc, :], dc == 0, dc == 15, ["Wq", ("xT", s)], [("ps", bank)])
            if ct < 8:
                COPY(cast_eng(("act", "dve")), Qst[:, ct, :], ps[bank], [("ps", bank)], [("Qst", ct)])
                dstt = ct
            else:
                COPY("act", Qst[:, ct, :], ps[bank], [("ps", bank)], [("Qst", ct)])
                COPY("dve", Qst[:, ct + 8, :], ps[bank], [("ps", bank)], [("Qst", ct + 8)])
                dstt = ct + 8
            if pend is not None:
                pend(); pend = None
            pend = (lambda dstt=dstt, bank=bank, s=s: rope_fix(Qst[0:32, dstt, :], ps[bank][0:32, :], ropet[s], s, [("ps", bank)], [("Qst", dstt)], tmpA, tmpB))
        if pend is not None:
            pend(); pend = None
        for t in range(4):
            for dc in range(16):
                MM(ps[7][:, 0:24], xT[:, dc, t * 128:(t + 1) * 128], Wgt[:, dc, :], dc == 0, dc == 15, ["Wgt", ("xT", s)], [("ps", 7)])
            O("act", "activation", [("ps", 7)], ["gates"], out=gates[:, oc * 4 + t, :], in_=ps[7][:, 0:24], func=AF.Sigmoid)
        DM("Qstst", QT_s[:, :, oc * 512:(oc + 1) * 512].rearrange("i p t -> p i t"), Qst,
           [("Qst", ct) for ct in range(24)], [("QT_s", oc)])
    S.barrier()
    if stop_after == "A":
        S.emit(); return nc

    A.reset()
    KT2 = [A.get([2, T], BF16) for _ in range(2)]
    Vh = [A.get([64, 257], BF16) for _ in range(2)]
    QT2 = [A.get([2, TO], BF16) for _ in range(2)]
    oTh = [A.get([2, TO], BF16) for _ in range(2)]
    PT = [A.get([512], BF16) for _ in range(3)]
    dmask4 = A.get([4, 512], BF16)
    lamb = A.get([512], F32); lt = A.get([256], F32); g08 = A.get([256], F32)
    sm = A.get([16], F32)
    od = A.get([256], F32); junk = A.get([256], F32); obf = A.get([256], BF16)
    DM("c2", dmask4, c_dmask4, [], ["dmask4"])
    DM("c3", lamb, diff_lambda.rearrange("a d -> (a d)").partition_broadcast(128), [], ["lamb"])
    DM("c4", g08, subln_g.partition_broadcast(128), [], ["g08"])
    O("dve", "tensor_scalar", ["g08"], ["g08"], out=g08, in0=g08, scalar1=1.0 - LAMBDA_INIT, scalar2=None, op0=ALU.mult)
    for i in range(2):
        O("dve", "tensor_tensor", ["lamb"], ["lt"], out=lt[:, 0:128], in0=lamb[:, i * 256:i * 256 + 128], in1=lamb[:, i * 256 + 128:i * 256 + 256], op=ALU.mult)
        O("dve", "reduce_sum", ["lt"], [("sm", i)], out=sm[:, i:i + 1], in_=lt[:, 0:128], axis=AX.X)
        O("act", "activation", [("sm", i)], [("sm", i)], out=sm[:, i:i + 1], in_=sm[:, i:i + 1], func=AF.Exp)
    O("dve", "tensor_tensor", [("sm", 0), ("sm", 1)], ["nlam"], out=sm[:, 2:3], in0=sm[:, 1:2], in1=sm[:, 0:1], op=ALU.subtract)
    O("dve", "tensor_scalar", ["nlam"], ["nlam"], out=sm[:, 2:3], in0=sm[:, 2:3], scalar1=-LAMBDA_INIT, scalar2=None, op0=ALU.add)
    for s in range(2):
        O("pool", "memset", [], [("Vh1", s)], Vh[s][:, :, 256:257], 1.0)
    O("dve", "memset", [], ["epsb"], sm[:, 8:9], EPS)
    steps = []
    cnt = {"pt": 0, "sb": 0}
    def mk_pre_head(h):
        s = h % 2

        def pre_head():
            DM("kt2_%d" % s, KT2[s], KT_s[2 * h:2 * h + 2].rearrange("i p t -> p i t"), [("KT_s", tc) for tc in range(16)], [("KT2", s)])
            DMS("vh_%d" % s, Vh[s][:, :, 0:256], V_s[:, 256 * h:256 * h + 256].rearrange("(t p) c -> p t c", p=128), 8,
                [("V_s", tc) for tc in range(16)], [("Vh", s)])
            DM("qt2_%d" % s, QT2[s], QT_s[2 * h:2 * h + 2].rearrange("i p t -> p i t"), [("QT_s", oc) for oc in range(4)], [("QT2", s)])
        return pre_head

    for h in range(4):
        s = h % 2
        pre_head = mk_pre_head(h)
        nxt_pre = mk_pre_head(h + 1) if h + 1 < 4 else None

        def _unused(h=h, s=s):
            DM("kt2_%d" % s, KT2[s], KT_s[2 * h:2 * h + 2].rearrange("i p t -> p i t"), [("KT_s", tc) for tc in range(16)], [("KT2", s)])
            DMS("vh_%d" % s, Vh[s][:, :, 0:256], V_s[:, 256 * h:256 * h + 256].rearrange("(t p) c -> p t c", p=128), 8,
                [("V_s", tc) for tc in range(16)], [("Vh", s)])
            DM("qt2_%d" % s, QT2[s], QT_s[2 * h:2 * h + 2].rearrange("i p t -> p i t"), [("QT_s", oc) for oc in range(4)], [("QT2", s)])

        for m in range(NQT):
            ob = (m % 2) * 2
            nkt = 4 * m + 4
            for kp in range(nkt // 2):
                sbank = 4 + cnt["sb"] % 2; cnt["sb"] += 1
                pti = cnt["pt"] % 3; cnt["pt"] += 1
                pt = PT[pti]; ptk = ("PT", pti)
                st = {}
                if h == 0 and m == 0 and kp == 0:
                    st["pre"] = pre_head
                if m == 8 and kp == 0 and nxt_pre is not None:
                    st["pre"] = nxt_pre

                def qk(s=s, m=m, kp=kp, sbank=sbank):
                    for j in range(2):
                        kt = 2 * kp + j
                        for c in range(2):
                            MM(ps[sbank][:, (j * 2 + c) * 128:(j * 2 + c + 1) * 128], KT2[s][:, c, kt * 128:(kt + 1) * 128],
                               QT2[s][:, c, m * 128:(m + 1) * 128], True, True, [("KT2", s), ("QT2", s)], [("ps", sbank)])

                def em(m=m, kp=kp, sbank=sbank, pt=pt, ptk=ptk):
                    O("act", "activation", [("ps", sbank)], [ptk], out=pt, in_=ps[sbank], func=AF.Exp, scale=SCALE)
                    for j in range(2):
                        kt = 2 * kp + j
                        if kt >= 4 * m:
                            i = kt - 4 * m
                            O(cast_eng(("pool", "dve")), "tensor_tensor", [ptk, "dmask4"], [ptk], out=pt[:, j * 256:(j + 1) * 256],
                              in0=pt[:, j * 256:(j + 1) * 256], in1=dmask4[:, i, 0:256], op=ALU.mult)

                def pv(s=s, m=m, kp=kp, pt=pt, ptk=ptk, ob=ob, nkt=nkt):
                    for j in range(2):
                        kt = 2 * kp + j
                        for c in range(2):
                            MM(ps[ob + c][:, 0:257], pt[:, (j * 2 + c) * 128:(j * 2 + c + 1) * 128], Vh[s][:, kt, :], kt == 0, kt == nkt - 1,
                               [ptk, ("Vh", s), ("Vh1", s)], [("ps", ob + c)])

                st["qk"] = qk; st["em"] = em; st["pv"] = pv
                if kp == nkt // 2 - 1:
                    def post(h=h, s=s, m=m, ob=ob):
                        O("dve", "reciprocal", [("ps", ob)], ["rl0"], out=sm[:, 4:5], in_=ps[ob][:, 256:257])
                        O("dve", "reciprocal", [("ps", ob + 1)], ["rl1"], out=sm[:, 5:6], in_=ps[ob + 1][:, 256:257])
                        O("dve", "tensor_tensor", ["rl1", "nlam"], ["rl1"], out=sm[:, 5:6], in0=sm[:, 5:6], in1=sm[:, 2:3], op=ALU.mult)
                        O("dve", "tensor_scalar", [("ps", ob), "rl0"], ["od"], out=od, in0=ps[ob][:, 0:256], scalar1=sm[:, 4:5], scalar2=None, op0=ALU.mult)
                        O("dve", "scalar_tensor_tensor", [("ps", ob + 1), "rl1", "od"], ["od"], out=od, in0=ps[ob + 1][:, 0:256], scalar=sm[:, 5:6], in1=od,
                          op0=ALU.mult, op1=ALU.add)
                        O("act", "activation", ["od"], ["junk", "ss"], out=junk, in_=od, func=AF.Square, accum_out=sm[:, 6:7])
                        O("act", "activation", ["ss", "epsb"], ["ss"], out=sm[:, 6:7], in_=sm[:, 6:7], func=AF.Ln, scale=1.0 / 256.0, bias=sm[:, 8:9])
                        O("act", "activation", ["ss"], ["ss"], out=sm[:, 6:7], in_=sm[:, 6:7], func=AF.Exp, scale=-0.5)
                        O("dve", "scalar_tensor_tensor", ["od", "ss", "g08"], ["obf"], out=obf, in0=od, scalar=sm[:, 6:7], in1=g08, op0=ALU.mult, op1=ALU.mult)

                    def post_pe(h=h, s=s, m=m):
                        for c in range(2):
                            TR(psb[6][:, c * 128:(c + 1) * 128], obf[:, c * 128:(c + 1) * 128], ["obf"], [("ps", 6)])
                        COPY("act", oTh[s][:, :, m * 128:(m + 1) * 128], psb[6][:, 0:256].rearrange("p (c k) -> p c k", k=128), [("ps", 6)], [("oTh", s)])
                        if m == NQT - 1:
                            DM("oTst%d" % s, oT_s[2 * h:2 * h + 2].rearrange("i p t -> p i t"), oTh[s], [("oTh", s)], [("oT_s", "d", h)])
                    st["post"] = post; st["post_pe"] = post_pe
                steps.append(st)

    def run_pipeline(steps, defer=2):
        pending = []
        n = len(steps)
        if "pre" in steps[0]:
            steps[0]["pre"]()
        steps[0]["qk"]()
        for i in range(n):
            if i + 1 < n:
                if "pre" in steps[i + 1]:
                    steps[i + 1]["pre"]()
                steps[i + 1]["qk"]()
            steps[i]["em"]()
            steps[i]["pv"]()
            pending = [(c - 1, f) for (c, f) in pending]
            for (c, f) in [p for p in pending if p[0] <= 0]:
                f()
            pending = [p for p in pending if p[0] > 0]
            if "post" in steps[i]:
                steps[i]["post"]()
            if "post_pe" in steps[i]:
                pending.append((defer, steps[i]["post_pe"]))
        for (c, f) in pending:
            f()

    run_pipeline(steps)
    S.barrier()
    if stop_after == "B":
        S.emit(); return nc

    A.reset()
    XcT = A.get([T], BF16)
    W1 = A.get([32, 256], BF16)
    w1st = [A.get([8, 256], F32) for _ in range(2)]
    posf = A.get([128], F32, parts=32); posb = A.get([128], BF16, parts=32); posT = A.get([32], BF16)
    W2f = A.get([2, 128], F32); W2 = A.get([2, 128], BF16)
    b1 = A.get([2], F32); b2c = A.get([1], F32)
    b2rf = A.get([128], F32, parts=1); b2r = A.get([128], BF16, parts=1)
    hT = A.get([2, 512], BF16)
    u = A.get([512], F32); u2 = A.get([512], F32); sg = A.get([512], F32)
    O("pool", "memset", [], ["hT"], hT, 0.0)
    for g in range(2):
        O("pool", "memset", [], [("kcT", g)], kcT[g], 0.0)
    for j in range(2):
        DM("cp0", posf, cmp_pos[j], [], ["posf"])
        COPY("dve", posb, posf, ["posf"], ["posb"])
        S.op("pe", lambda e: e.transpose(out=psb[2][:, 0:32], in_=posb, identity=ident[0:32, 0:32]), ["posb", "ident"], [("ps", 2)])
        COPY("dve", posT, psb[2][:, 0:32], [("ps", 2)], ["posT"])
        DM("cp1", W2f, cmp_w2[j].rearrange("(c p) d -> p c d", p=128), [], ["W2f"])
        COPY("dve", W2, W2f, ["W2f"], ["W2"])
        DM("cp2", b1, cmp_b1[j].rearrange("(c p) -> p c", p=128), [], ["b1"], allow_slow_non_contiguous=True)
        if j == 0:
            DM("cp3", b2c, cmp_b2[0].rearrange("(p o) -> p o", o=1), [], ["b2c"], allow_slow_non_contiguous=True)
        else:
            DM("cp3", b2rf, cmp_b2[1].rearrange("(o d) -> o d", o=1), [], ["b2rf"])
            COPY("dve", b2r, b2rf, ["b2rf"], ["b2r"])
        for q4 in range(4):
            s = q4 % 2
            DM("w1st%d" % s, w1st[s], cmp_w1[j, q4 * 1024:(q4 + 1) * 1024, :].rearrange("(l d) h -> d l h", d=128), [], [("w1st", s)])
            COPY(cast_eng(("pool", "dve")), W1[:, q4 * 8:(q4 + 1) * 8, :], w1st[s], [("w1st", s)], ["W1"])
        for hc in range(2):
            for l in range(32):
                MM(ps[2][:, hc:hc + 1], W1[:, l, hc * 128:(hc + 1) * 128], posT[:, l:l + 1], l == 0, l == 31, ["W1", "posT"], [("ps", 2)])
        O("dve", "tensor_tensor", [("ps", 2), "b1"], ["b1"], out=b1, in0=ps[2][:, 0:2], in1=b1, op=ALU.add)
        for g in range(2):
            DM("xct", XcT, KT_s[(8 if j == 0 else 14) + g], [("KT_s", tc) for tc in range(16)], ["XcT"])
            for hc in range(2):
                for l in range(32):
                    MM(ps[hc][:, 0:511], W1[:, l, hc * 128:(hc + 1) * 128], XcT[:, l:l + 16 * 510 + 1:16], l == 0, l == 31, ["W1", "XcT"], [("ps", hc)])
                O("act", "activation", [("ps", hc), "b1"], ["u"], out=u[:, 0:511], in_=ps[hc][:, 0:511], func=AF.Identity, bias=b1[:, hc:hc + 1])
                O("dve", "tensor_tensor", ["u"], ["u2"], out=u2[:, 0:511], in0=u[:, 0:511], in1=u[:, 0:511], op=ALU.mult)
                O("dve", "tensor_scalar", ["u2"], ["u2"], out=u2[:, 0:511], in0=u2[:, 0:511], scalar1=0.044715, scalar2=1.0, op0=ALU.mult, op1=ALU.add)
                O("dve", "tensor_tensor", ["u2", "u"], ["u2"], out=u2[:, 0:511], in0=u2[:, 0:511], in1=u[:, 0:511], op=ALU.mult)
                O("act", "activation", ["u2"], ["sg"], out=sg[:, 0:511], in_=u2[:, 0:511], func=AF.Sigmoid, scale=1.5957691216057308)
                O("dve", "tensor_tensor", ["u", "sg"], ["hT"], out=hT[:, hc, 0:511], in0=u[:, 0:511], in1=sg[:, 0:511], op=ALU.mult)
            if j == 0:
                for hc in range(2):
                    MM(ps[3][:, 0:511], W2[:, hc, :], hT[:, hc, 0:511], hc == 0, hc == 1, ["W2", "hT"], [("ps", 3)])
                O("act", "activation", [("ps", 3), "b2c"], [("kcT", g)], out=kcT[g][:, 0:511], in_=ps[3][:, 0:511], func=AF.Identity, bias=b2c[:, 0:1])
            else:
                for ct in range(4):
                    for hc in range(2):
                        MM(ps[3][:, ct * 128:(ct + 1) * 128], hT[:, hc, ct * 128:(ct + 1) * 128], W2[:, hc, :], hc == 0, False, ["W2", "hT"], [("ps", 3)])
                    MM(ps[3][:, ct * 128:(ct + 1) * 128], ones_bf[0:1, :], b2r[0:1, :], False, True, ["ones_bf", "b2r"], [("ps", 3)])
                COPY("act", VCB[g], ps[3].rearrange("p (c k) -> p c k", k=128), [("ps", 3)], [("VCB", g)])
    S.barrier()
    if stop_after == "C0":
        S.emit(); return nc

    A.reset()
    QTg = A.get([4, TO], BF16); QRg = A.get([4, TO], BF16)
    KsT = A.get([T], BF16); KwT = A.get([T], BF16)
    Vs1 = A.get([64, 129], BF16); Vw1 = A.get([64, 129], BF16)
    Eexp = A.get([T], BF16)
    oTg = A.get([4, TO], BF16)
    dmask4 = A.get([4, 512], BF16); cmask4 = A.get([5, 512], BF16); wmask4 = A.get([8, 512], BF16)
    selvm = A.get([16, 128], F32); selfb = A.get([16, 128], F32)
    VCA = A.get([4, 129], BF16)
    PT = [A.get([512], BF16) for _ in range(3)]
    selbT4 = A.get([4, 128], BF16)
    imp = A.get([128], F32); sc2 = A.get([128], F32); selb = A.get([128], BF16)
    mx = A.get([16], F32); sm = A.get([32], F32)
    onsa = A.get([4, 128], F32); onb = A.get([4, 128], BF16)
    DM("c2", dmask4, c_dmask4, [], ["dmask4"]); DM("c5", cmask4, c_cmask4, [], ["cmask4"]); DM("c6", wmask4, c_wmask4, [], ["wmask4"])
    DM("c7", selvm, c_selvm, [], ["selvm"]); DM("c8", selfb, c_selfb, [], ["selfb"])
    DM("c9", VCA, c_ovl1.rearrange("(t p) c -> p t c", p=128), [], ["VCA"])
    DM("c10", Eexp, c_E, [], ["Eexp"])
    O("pool", "memset", [], ["Vs1o"], Vs1[:, :, 128:129], 1.0)
    O("pool", "memset", [], ["Vw1o"], Vw1[:, :, 128:129], 1.0)
    RA = [(0, 0), (0, 129), (0, 258), (1, 0)]
    RB = [(1, 129), (1, 257), (2, 0), (2, 128)]
    RS = [(3, 0), (3, 129), (3, 258), (4, 0)]
    RW = [(4, 129), (4, 258), (5, 0), (5, 129)]
    steps = []
    cnt = {"pt": 0, "sb": 0}

    def nxt_bufs():
        sbank = 6 + cnt["sb"] % 2; cnt["sb"] += 1
        pti = cnt["pt"] % 3; cnt["pt"] += 1
        return sbank, PT[pti], ("PT", pti)

    for g in range(2):
        allK = [("KT_s", tc) for tc in range(16)]; allV = [("V_s", tc) for tc in range(16)]; allQ = [("QT_s", oc) for oc in range(4)]

        def pre_group(g=g, allK=allK, allV=allV, allQ=allQ):
            DM("n0", QTg, QT_s[8 + 4 * g:12 + 4 * g].rearrange("i p t -> p i t"), allQ, ["QTg"])
            DM("n1", QRg, QT_s[16 + 4 * g:20 + 4 * g].rearrange("i p t -> p i t"), allQ, ["QRg"])
            DM("n2", KsT, KT_s[10 + g], allK, ["KsT"]); DM("n3", KwT, KT_s[12 + g], allK, ["KwT"])
            DMS("n4", Vs1[:, :, 0:128], V_s[:, 1024 + 128 * g:1024 + 128 * g + 128].rearrange("(t p) c -> p t c", p=128), 8, allV, ["Vs1"])
            DMS("n5", Vw1[:, :, 0:128], V_s[:, 1280 + 128 * g:1280 + 128 * g + 128].rearrange("(t p) c -> p t c", p=128), 8, allV, ["Vw1"])

        for m in range(NQT):
            started = {}
            qsl = slice(m * 128, (m + 1) * 128)

            def ACC(reg, width, lhsT, rhs, last, reads, started=started):
                bank, off = reg
                st_ = bank not in started
                started[bank] = True
                MM(ps[bank][:, off:off + width], lhsT, rhs, st_, last, reads, [("ps", bank)])

            nct = m // 4 + 1
            for ct in range(nct):
                sbank, pt, ptk = nxt_bufs()
                st = {}
                if m == 0 and ct == 0:
                    st["pre"] = pre_group

                def qk(g=g, ct=ct, sbank=sbank, qsl=qsl):
                    MM(ps3(sbank), kcT[g][:, ct * 128:(ct + 1) * 128], QTg[:, :, qsl], True, True, [("kcT", g), "QTg"], [("ps", sbank)])

                def em(m=m, ct=ct, sbank=sbank, pt=pt, ptk=ptk):
                    O("act", "activation", [("ps", sbank)], [ptk], out=pt, in_=ps[sbank], func=AF.Exp, scale=SCALE)
                    i = m - 4 * ct
                    if i <= 4:
                        O(cast_eng(("pool", "dve")), "tensor_tensor", [ptk, "cmask4"], [ptk], out=pt, in0=pt, in1=cmask4[:, i, :], op=ALU.mult)

                def pv(g=g, ct=ct, nct=nct, pt=pt, ptk=ptk, ACC=ACC):
                    for hh in range(4):
                        ACC(RA[hh], 129, pt[:, hh * 128:(hh + 1) * 128], VCA[:, ct, :], ct == nct - 1, [ptk, "VCA"])
                        ACC(RB[hh], 128, pt[:, hh * 128:(hh + 1) * 128], VCB[g][:, ct, :], ct == nct - 1, [ptk, ("VCB", g)])

                st["qk"] = qk; st["em"] = em; st["pv"] = pv
                if ct == nct - 1:
                    def post(g=g, m=m):
                        for hh in range(4):
                            bk, off = RA[hh]
                            O("dve", "tensor_scalar", [("ps", bk)], [("rc", hh)], out=sm[:, hh:hh + 1], in0=ps[bk][:, off + 128:off + 129], scalar1=1e-30, scalar2=None, op0=ALU.max)
                            O("dve", "reciprocal", [("rc", hh)], [("rc", hh)], out=sm[:, hh:hh + 1], in_=sm[:, hh:hh + 1])
                            if hh == 0:
                                O("dve", "tensor_scalar", [("ps", bk), ("rc", hh)], ["imp"], out=imp, in0=ps[bk][:, off:off + 128], scalar1=sm[:, hh:hh + 1], scalar2=None, op0=ALU.mult)
                            else:
                                O("dve", "scalar_tensor_tensor", [("ps", bk), ("rc", hh), "imp"], ["imp"], out=imp, in0=ps[bk][:, off:off + 128], scalar=sm[:, hh:hh + 1], in1=imp,
                                  op0=ALU.mult, op1=ALU.add)
                        O("dve", "tensor_tensor", ["imp", "selvm"], ["imp"], out=imp, in0=imp, in1=selvm[:, m, :], op=ALU.mult)
                        O("dve", "tensor_tensor", ["imp", "selfb"], ["imp"], out=imp, in0=imp, in1=selfb[:, m, :], op=ALU.add)
                        O("dve", "max", ["imp"], ["mx"], out=mx[:, 0:8], in_=imp)
                        O("dve", "match_replace", ["imp", "mx"], ["sc2"], out=sc2, in_to_replace=mx[:, 0:8], in_values=imp, imm_value=-3.0e38)
                        O("dve", "max", ["sc2"], ["mx2"], out=mx[:, 8:16], in_=sc2)
                        O("dve", "tensor_scalar", ["imp", "mx2", "sc2"], ["sc2"], out=sc2, in0=imp, scalar1=mx[:, 15:16], scalar2=1.0, op0=ALU.is_ge, op1=ALU.subtract)
                        O("dve", "tensor_scalar", ["sc2"], ["selb"], out=selb, in0=sc2, scalar1=-NEG_BIAS, scalar2=None, op0=ALU.mult)
                        for hh in range(4):
                            bk, off = RB[hh]
                            gcol = (4 * g + hh) * 3
                            O("dve", "tensor_tensor", [("rc", hh), "gates"], [("f", hh)], out=sm[:, 8 + hh:9 + hh], in0=sm[:, hh:hh + 1], in1=gates[:, m, gcol:gcol + 1], op=ALU.mult)
                            O("dve", "tensor_scalar", [("ps", bk), ("f", hh)], [("onsa", hh)], out=onsa[:, hh, :], in0=ps[bk][:, off:off + 128], scalar1=sm[:, 8 + hh:9 + hh], scalar2=None, op0=ALU.mult)
                    st["post"] = post
                steps.append(st)
            jl = [j for j in range(8) if 4 * m - 4 + j >= 0]
            for j in jl:
                tt = 4 * m - 4 + j
                sbank, pt, ptk = nxt_bufs()
                st = {}

                def qk(tt=tt, sbank=sbank, qsl=qsl):
                    MM(ps3(sbank), KwT[:, tt * 128:(tt + 1) * 128], QRg[:, :, qsl], True, True, ["KwT", "QRg"], [("ps", sbank)])

                def em(j=j, sbank=sbank, pt=pt, ptk=ptk):
                    O("act", "activation", [("ps", sbank)], [ptk], out=pt, in_=ps[sbank], func=AF.Exp, scale=SCALE)
                    O(cast_eng(("pool", "dve")), "tensor_tensor", [ptk, "wmask4"], [ptk], out=pt, in0=pt, in1=wmask4[:, j, :], op=ALU.mult)

                def pv(j=j, tt=tt, jl=jl, pt=pt, ptk=ptk, ACC=ACC):
                    for hh in range(4):
                        ACC(RW[hh], 129, pt[:, hh * 128:(hh + 1) * 128], Vw1[:, tt, :], j == jl[-1], [ptk, "Vw1", "Vw1o"])

                st["qk"] = qk; st["em"] = em; st["pv"] = pv
                steps.append(st)
            ntt = 4 * m + 4
            for tt in range(ntt):
                sbank, pt, ptk = nxt_bufs()
                st = {}
                if tt == 0:
                    def pre_sel():
                        sbank_t, _, _ = nxt_bufs()
                        TR(psb[sbank_t][:, 0:128], selb, ["selb"], [("ps", sbank_t)])
                        for hh in range(4):
                            COPY("act", selbT4[:, hh, :], psb[sbank_t][:, 0:128], [("ps", sbank_t)], ["selbT4"])
                    st["pre"] = pre_sel

                def qk(tt=tt, sbank=sbank, qsl=qsl):
                    MM(ps3(sbank), KsT[:, tt * 128:(tt + 1) * 128], QRg[:, :, qsl], True, False, ["KsT", "QRg"], [("ps", sbank)])
                    MM(ps3(sbank), Eexp[:, tt * 128:(tt + 1) * 128], selbT4, False, True, ["Eexp", "selbT4"], [("ps", sbank)])

                def em(m=m, tt=tt, sbank=sbank, pt=pt, ptk=ptk):
                    O("act", "activation", [("ps", sbank)], [ptk], out=pt, in_=ps[sbank], func=AF.Exp, scale=SCALE)
                    if tt >= 4 * m:
                        O(cast_eng(("pool", "dve")), "tensor_tensor", [ptk, "dmask4"], [ptk], out=pt, in0=pt, in1=dmask4[:, tt - 4 * m, :], op=ALU.mult)

                def pv(tt=tt, ntt=ntt, pt=pt, ptk=ptk, ACC=ACC):
                    for hh in range(4):
                        ACC(RS[hh], 129, pt[:, hh * 128:(hh + 1) * 128], Vs1[:, tt, :], tt == ntt - 1, [ptk, "Vs1", "Vs1o"])

                st["qk"] = qk; st["em"] = em; st["pv"] = pv
                if tt == ntt - 1:
                    def post(g=g, m=m):
                        for hh in range(4):
                            gcol = (4 * g + hh) * 3
                            for (reg, gi, nm) in ((RS[hh], 1, "fs"), (RW[hh], 2, "fw")):
                                bk, off = reg
                                col = 16 + hh * 2 + (gi - 1)
                                O("dve", "reciprocal", [("ps", bk)], [(nm, hh)], out=sm[:, col:col + 1], in_=ps[bk][:, off + 128:off + 129])
                                O("dve", "tensor_tensor", [(nm, hh), "gates"], [(nm, hh)], out=sm[:, col:col + 1], in0=sm[:, col:col + 1], in1=gates[:, m, gcol + gi:gcol + gi + 1], op=ALU.mult)
                                O("dve", "scalar_tensor_tensor", [("ps", bk), (nm, hh), ("onsa", hh)], [("onsa", hh)], out=onsa[:, hh, :], in0=ps[bk][:, off:off + 128],
                                  scalar=sm[:, col:col + 1], in1=onsa[:, hh, :], op0=ALU.mult, op1=ALU.add)
                        COPY("act", onb, onsa, [("onsa", hh) for hh in range(4)], ["onb"])

                    def post_pe(g=g, m=m, qsl=qsl):
                        tb, _, _ = nxt_bufs()
                        for hh in range(4):
                            TR(psb[tb][:, hh * 128:(hh + 1) * 128], onb[:, hh, :], ["onb"], [("ps", tb)])
                        COPY("dve", oTg[:, :, qsl], psb[tb][:, 0:512].rearrange("p (c k) -> p c k", k=128), [("ps", tb)], ["oTg"])
                        if m == NQT - 1:
                            DM("oTgst", oT_s[8 + 4 * g:12 + 4 * g].rearrange("i p t -> p i t"), oTg, ["oTg"], [("oT_s", "n", g)])
                    st["post"] = post; st["post_pe"] = post_pe
                steps.append(st)
    run_pipeline(steps)
    S.barrier()
    if stop_after == "C":
        S.emit(); return nc

    def layernorm(z, gam, bet, outt, sm_, keyz, keyo, rd):
        O("act", "activation", [keyz], ["lnjunk", "ln_s1"], out=lnjunk, in_=z, func=AF.Copy, accum_out=sm_[:, 0:1])
        O("act", "activation", [keyz], ["lnjunk", "ln_s2"], out=lnjunk, in_=z, func=AF.Square, accum_out=sm_[:, 1:2])
        O("dve", "tensor_scalar", ["ln_s1"], ["ln_mu"], out=sm_[:, 2:3], in0=sm_[:, 0:1], scalar1=1.0 / D, scalar2=None, op0=ALU.mult)
        O("dve", "tensor_tensor", ["ln_mu"], ["ln_m2"], out=sm_[:, 3:4], in0=sm_[:, 2:3], in1=sm_[:, 2:3], op=ALU.mult)
        O("dve", "scalar_tensor_tensor", ["ln_s2", "ln_m2"], ["ln_var"], out=sm_[:, 4:5], in0=sm_[:, 1:2], scalar=1.0 / D, in1=sm_[:, 3:4],
          op0=ALU.mult, op1=ALU.subtract)
        O("act", "activation", ["ln_var", "ln_eps"], ["ln_var"], out=sm_[:, 4:5], in_=sm_[:, 4:5], func=AF.Ln, bias=sm_[:, 7:8])
        O("act", "activation", ["ln_var"], ["ln_rstd"], out=sm_[:, 5:6], in_=sm_[:, 4:5], func=AF.Exp, scale=-0.5)
        O("dve", "tensor_scalar", [keyz, "ln_mu", "ln_rstd"], [keyz], out=z, in0=z, scalar1=sm_[:, 2:3], scalar2=sm_[:, 5:6], op0=ALU.subtract, op1=ALU.mult)
        O("pool", "tensor_tensor", [keyz] + rd, [keyz], out=z, in0=z, in1=gam, op=ALU.mult)
        O("pool", "tensor_tensor", [keyz] + rd, [keyo], out=outt, in0=z, in1=bet, op=ALU.add)

    A.reset()
    Wo = A.get([16, D], BF16)
    wost = [A.get([D], F32) for _ in range(2)]
    for c in range(16):
        s = c % 2
        DM("wost%d" % s, wost[s], w_out[c * 128:(c + 1) * 128, :], [], [("wost", s)])
        COPY(cast_eng(), Wo[:, c, :], wost[s], [("wost", s)], ["Wo"])
    g1 = A.get([D], F32); be1 = A.get([D], F32)
    DM("c11", g1, ln1_g.partition_broadcast(128), [], ["lnp"]); DM("c12", be1, ln1_b.partition_broadcast(128), [], ["lnp"])
    Wrf = A.get([16, 36], F32); Wr = A.get([16, 36], BF16)
    DM("c13", Wrf[:, :, 0:4], r_group.rearrange("(c p) g -> p c g", p=128), [], ["Wrf"], allow_slow_non_contiguous=True)
    DM("c14", Wrf[:, :, 4:36], r_expert.rearrange("(c p) g -> p c g", p=128), [], ["Wrf"], allow_slow_non_contiguous=True)
    COPY("dve", Wr, Wrf, ["Wrf"], ["Wr"])
    oTt = [A.get([16, 128], BF16) for _ in range(2)]
    xt = [A.get([D], F32) for _ in range(2)]
    z = [A.get([D], F32) for _ in range(2)]
    x1 = [A.get([D], F32) for _ in range(2)]
    x1b = A.get([D], BF16)
    x1Tt = [A.get([16, 128], BF16) for _ in range(2)]
    lnjunk = A.get([D], BF16)
    sm = A.get([16], F32)
    lg = A.get([36], F32); rt = A.get([160], F32); dwb = A.get([32], BF16)
    O("dve", "memset", [], ["ln_eps"], sm[:, 7:8], EPS)
    for m in range(NQT):
        s = m % 2
        DM("oTt%d" % s, oTt[s], oT_s[:, :, m * 128:(m + 1) * 128].rearrange("i p t -> p i t"),
           [("oT_s", "d", h) for h in range(4)] + [("oT_s", "n", g) for g in range(2)], [("oTt", s)])
        DM("xt%d" % s, xt[s], xo[m * 128:(m + 1) * 128, :], [], [("xt", s)])
        for nb in range(4):
            for c in range(16):
                MM(ps[nb], oTt[s][:, c, :], Wo[:, c, nb * 512:(nb + 1) * 512], c == 0, c == 15, [("oTt", s), "Wo"], [("ps", nb)])
            O("dve", "scalar_tensor_tensor", [("xt", s), ("ps", nb)], [("z", s)], out=z[s][:, nb * 512:(nb + 1) * 512], in0=xt[s][:, nb * 512:(nb + 1) * 512],
              scalar=DN_ALPHA, in1=ps[nb], op0=ALU.mult, op1=ALU.add)
        layernorm(z[s], g1, be1, x1[s], sm, ("z", s), ("x1", s), ["lnp"])
        DM("x1st%d" % s, x1_s[m * 128:(m + 1) * 128, :], x1[s], [("x1", s)], [("x1_s", m)])
        COPY("act", x1b, x1[s], [("x1", s)], ["x1b"])
        for hb in range(2):
            bank = 4 + hb
            for j in range(8):
                dc = hb * 8 + j
                TR(psb[bank][:, j * 128:(j + 1) * 128], x1b[:, dc * 128:(dc + 1) * 128], ["x1b"], [("ps", bank)])
            COPY("dve" if hb == 0 else "act", x1Tt[s][:, hb * 8:(hb + 1) * 8, :], psb[bank].rearrange("p (j k) -> p j k", k=128), [("ps", bank)], [("x1Tt", s)])
        DM("x1Tst%d" % s, x1T_s[:, :, m * 128:(m + 1) * 128].rearrange("i p t -> p i t"), x1Tt[s], [("x1Tt", s)], [("x1T_s", m)])
        for dc in range(16):
            MM(ps[6][:, 0:36], x1Tt[s][:, dc, :], Wr[:, dc, :], dc == 0, dc == 15, [("x1Tt", s), "Wr"], [("ps", 6)])
        COPY("dve", lg, ps[6][:, 0:36], [("ps", 6)], ["lg"])
        R = ["rt"]
        gmx, ngmx, gsum, pg, gm = rt[:, 0:1], rt[:, 1:2], rt[:, 2:3], rt[:, 4:8], rt[:, 8:12]
        em, ee, mk, em2 = rt[:, 16:48], rt[:, 48:80], rt[:, 80:112], rt[:, 112:144]
        m1, nm1, m2, den, scw = rt[:, 144:145], rt[:, 145:146], rt[:, 146:147], rt[:, 147:148], rt[:, 148:149]
        O("dve", "reduce_max", ["lg"], R, out=gmx, in_=lg[:, 0:4], axis=AX.X)
        O("dve", "tensor_scalar", R, R, out=ngmx, in0=gmx, scalar1=-1.0, scalar2=None, op0=ALU.mult)
        O("dve", "tensor_scalar", ["lg"] + R, R, out=gm, in0=lg[:, 0:4], scalar1=gmx, scalar2=None, op0=ALU.is_ge)
        O("act", "activation", ["lg"] + R, R, out=pg, in_=lg[:, 0:4], func=AF.Exp, bias=ngmx, accum_out=gsum)
        O("dve", "tensor_scalar", R, R, out=pg, in0=gm, scalar1=1.0, scalar2=1e30, op0=ALU.subtract, op1=ALU.mult)
        for gg in range(4):
            O("dve", "tensor_scalar", ["lg"] + R, R, out=em[:, gg * 8:(gg + 1) * 8], in0=lg[:, 4 + gg * 8:12 + gg * 8], scalar1=pg[:, gg:gg + 1], scalar2=None, op0=ALU.add)
        O("dve", "reduce_max", R, R, out=m1, in_=em, axis=AX.X)
        O("dve", "tensor_scalar", R, R, out=nm1, in0=m1, scalar1=-1.0, scalar2=None, op0=ALU.mult)
        O("act", "activation", R, R, out=ee, in_=em, func=AF.Exp, bias=nm1)
        O("dve", "tensor_scalar", R, R, out=mk, in0=em, scalar1=m1, scalar2=None, op0=ALU.is_ge)
        O("dve", "scalar_tensor_tensor", R, R, out=em2, in0=mk, scalar=-1e30, in1=em, op0=ALU.mult, op1=ALU.add)
        O("dve", "reduce_max", R, R, out=m2, in_=em2, axis=AX.X)
        O("dve", "tensor_scalar", R, R, out=mk, in0=em, scalar1=m2, scalar2=None, op0=ALU.is_ge)
        O("dve", "tensor_tensor", R, R, out=ee, in0=ee, in1=mk, op=ALU.mult)
        O("dve", "reduce_sum", R, R, out=den, in_=ee, axis=AX.X)
        O("dve", "tensor_tensor", R, R, out=den, in0=den, in1=gsum, op=ALU.mult)
        O("dve", "reciprocal", R, R, out=scw, in_=den)
        O("dve", "tensor_scalar", R, ["dwb"], out=dwb, in0=ee, scalar1=scw, scalar2=None, op0=ALU.mult)
        TR(psb[7][0:32, 0:128], dwb, ["dwb"], [("ps", 7)])
        COPY("act", dwT[:, m * 128:(m + 1) * 128], psb[7][0:32, 0:128], [("ps", 7)], ["dwT"])
    S.barrier()
    if stop_after == "E1":
        S.emit(); return nc

    A.reset()
    x1T = A.get([16, 1024], BF16)
    yacc = A.get([8, D], F32)
    hTm = A.get([4, 1024], BF16)
    wb = A.get([1024], F32)
    sele = A.get([32, 128], BF16, parts=32)
    sgt = [A.get([512], F32) for _ in range(2)]; tt_ = [A.get([512], F32) for _ in range(2)]
    sm = A.get([16], F32)
    ov0 = A.off
    wgst = [A.get([16, 128], F32)]; wust = [A.get([16, 128], F32)]
    wgb = [A.get([16, 128], BF16) for _ in range(2)]; wub = [A.get([16, 128], BF16) for _ in range(2)]
    wdst = [A.get([4, 512], F32) for _ in range(2)]; wdb = [A.get([4, 512], BF16) for _ in range(2)]
    A.off = ov0
    g2 = A.get([D], F32); be2 = A.get([D], F32)
    x1r = A.get([D], F32); outt = A.get([D], F32)
    lnjunk = A.get([D], BF16)
    DM("c15", sele, c_sele, [], ["sele"])
    O("dve", "memset", [], ["ln_eps"], sm[:, 7:8], EPS)
    wi = 0; di = 0; cntd = {"i": 0}
    for hb in range(2):
        DM("x1Tld", x1T, x1T_s[:, :, hb * 1024:(hb + 1) * 1024].rearrange("i p t -> p i t"), [("x1T_s", m) for m in range(16)], ["x1T"])
        chunks = []
        for e in range(32):
            for fc in range(4):
                s_ = wi % 2; wi += 1

                def load(e=e, fc=fc):
                    DM("wgst0", wgst[0], wg[e, :, fc * 128:(fc + 1) * 128].rearrange("(c p) f -> p c f", p=128), [], [("wgst", 0)])
                    DM("wust0", wust[0], wu[e, :, fc * 128:(fc + 1) * 128].rearrange("(c p) f -> p c f", p=128), [], [("wust", 0)])

                def cast(s_=s_):
                    COPY("dve", wgb[s_], wgst[0], [("wgst", 0)], [("wgb", s_)])
                    COPY("act", wub[s_], wust[0], [("wust", 0)], [("wub", s_)])

                def compute(e=e, fc=fc, s_=s_, hb=hb):
                    if fc == 0:
                        for blk in range(2):
                            MM(ps[4 + blk], sele[:, e, :], dwT[:, hb * 1024 + blk * 512: hb * 1024 + (blk + 1) * 512], True, True, ["sele", "dwT"], [("ps", 4 + blk)])
                            COPY("act", wb[:, blk * 512:(blk + 1) * 512], ps[4 + blk], [("ps", 4 + blk)], ["wb"])
                    for blk in range(2):
                        for dc in range(16):
                            MM(ps[blk], wgb[s_][:, dc, :], x1T[:, dc, blk * 512:(blk + 1) * 512], dc == 0, dc == 15, [("wgb", s_), "x1T"], [("ps", blk)])
                        for dc in range(16):
                            MM(ps[2 + blk], wub[s_][:, dc, :], x1T[:, dc, blk * 512:(blk + 1) * 512], dc == 0, dc == 15, [("wub", s_), "x1T"], [("ps", 2 + blk)])
                        O("act", "activation", [("ps", blk)], [("sgt", blk)], out=sgt[blk], in_=ps[blk], func=AF.Silu)
                        O("dve", "tensor_tensor", [("ps", 2 + blk), "wb"], [("tt_", blk)], out=tt_[blk], in0=ps[2 + blk], in1=wb[:, blk * 512:(blk + 1) * 512], op=ALU.mult)
                        O("pool", "tensor_tensor", [("sgt", blk), ("tt_", blk)], ["hTm"], out=hTm[:, fc, blk * 512:(blk + 1) * 512], in0=sgt[blk], in1=tt_[blk], op=ALU.mult)
                chunks.append((load, cast, compute))
            for dbk in range(4):
                s_ = di % 2; di += 1

                def load(e=e, dbk=dbk, s_=s_):
                    DM("wdst%d" % s_, wdst[s_], wd[e, :, dbk * 512:(dbk + 1) * 512].rearrange("(c p) d -> p c d", p=128), [], [("wdst", s_)])

                def cast(dbk=dbk, s_=s_):
                    COPY("act" if dbk % 2 == 0 else "dve", wdb[s_], wdst[s_], [("wdst", s_)], [("wdb", s_)])

                def compute(e=e, dbk=dbk, s_=s_):
                    for t8 in range(8):
                        bank = 4 + cntd["i"] % 4; cntd["i"] += 1
                        for fc in range(4):
                            MM(ps[bank], hTm[:, fc, t8 * 128:(t8 + 1) * 128], wdb[s_][:, fc, :], fc == 0, fc == 3, ["hTm", ("wdb", s_)], [("ps", bank)])
                        ysl = yacc[:, t8, dbk * 512:(dbk + 1) * 512]
                        if e == 0:
                            COPY("dve", ysl, ps[bank], [("ps", bank)], [("yacc", t8)])
                        else:
                            O("dve", "tensor_tensor", [("ps", bank), ("yacc", t8)], [("yacc", t8)], out=ysl, in0=ps[bank], in1=ysl, op=ALU.add)
                chunks.append((load, cast, compute))
        chunks[0][0](); chunks[0][1]()
        for k in range(len(chunks)):
            if k + 1 < len(chunks):
                chunks[k + 1][0](); chunks[k + 1][1]()
            chunks[k][2]()
        S.barrier()
        DM("c11", g2, ln2_g.partition_broadcast(128), [], ["lnp"]); DM("c12", be2, ln2_b.partition_broadcast(128), [], ["lnp"])
        for t8 in range(8):
            m = hb * 8 + t8
            DM("x1r", x1r, x1_s[m * 128:(m + 1) * 128, :], [("x1_s", m)], ["x1r"])
            O("dve", "scalar_tensor_tensor", ["x1r", ("yacc", t8)], [("yacc", t8)], out=yacc[:, t8, :], in0=x1r, scalar=DN_ALPHA, in1=yacc[:, t8, :], op0=ALU.mult, op1=ALU.add)
            layernorm(yacc[:, t8, :], g2, be2, outt, sm, ("yacc", t8), "outt", ["lnp"])
            DM("outst", out[m * 128:(m + 1) * 128, :], outt, ["outt"], [("out", m)])
        S.barrier()
    S.emit()
    return nc


def _constants(r):
    bf = ml_dtypes.bfloat16
    c = {}
    inv_freq = (np.float32(500000.0) ** (-np.arange(0, 32, 2, dtype=np.float32) / np.float32(32))).astype(np.float32)
    ang = (np.arange(T, dtype=np.float32)[:, None] * inv_freq[None, :]).astype(np.float32)
    cs, sn = np.cos(ang).astype(np.float32), np.sin(ang).astype(np.float32)
    C = np.concatenate([cs.T, cs.T], axis=0)
    Sg = np.concatenate([-sn.T, sn.T], axis=0)
    rope_k = np.stack([C, Sg], axis=0).astype(np.float32)
    own = (np.arange(16)[:, None] * 4 + r) * 128 + np.arange(128)[None, :]
    own = own.reshape(-1)
    c["rope_k"] = np.ascontiguousarray(rope_k)
    c["rope_q"] = np.ascontiguousarray(rope_k[:, :, own])
    c["c_ident"] = np.eye(128, dtype=np.float32).astype(bf)
    rsw = np.zeros((32, 32), np.float32)
    for m in range(32):
        rsw[(m + 16) % 32, m] = 1.0
    c["c_rsw"] = rsw.astype(bf)
    ki = np.arange(128)[:, None]; qi = np.arange(128)[None, :]
    dm = np.zeros((128, 4, 128), np.float32)
    for i in range(4):
        dm[:, i, :] = 1.0 if i < r else ((ki <= qi).astype(np.float32) if i == r else 0.0)
    c["c_dmask4"] = np.tile(dm, (1, 1, 4)).astype(bf)
    cm = np.zeros((128, 5, 128), np.float32)
    for i in range(5):
        cm[:, i, :] = (16 * ki + 31 <= 128 * (4 * i + r) + qi).astype(np.float32)
    c["c_cmask4"] = np.tile(cm, (1, 1, 4)).astype(bf)
    wm = np.zeros((128, 8, 128), np.float32)
    for j in range(8):
        d = r + 4 - j
        if d == 4:
            wm[:, j, :] = (ki > qi)
        elif 1 <= d <= 3:
            wm[:, j, :] = 1.0
        elif d == 0:
            wm[:, j, :] = (ki <= qi)
    c["c_wmask4"] = np.tile(wm, (1, 1, 4)).astype(bf)
    svm = np.zeros((128, 16, 128), np.float32); sfb = np.zeros((128, 16, 128), np.float32)
    s_idx = np.arange(128)[None, :]
    for m in range(16):
        qpos = (4 * m + r) * 128 + np.arange(128)[:, None]
        cur = qpos // 64
        valid = s_idx <= cur
        forced = (s_idx == 0) | (s_idx == cur) | (s_idx == cur - 1)
        svm[:, m, :] = (valid & ~forced)
        sfb[:, m, :] = np.where(valid & forced, 1e30, np.where(valid, 0.0, -1e30))
    c["c_selvm"] = svm; c["c_selfb"] = sfb
    ov = np.zeros((512, 129), np.float32)
    ci = np.arange(511)[:, None] * 16; sj = np.arange(128)[None, :] * 64
    ov[:511, :128] = ((ci < sj + 64) & (ci + 32 > sj))
    ov[:, 128] = 1.0
    c["c_ovl1"] = ov.astype(bf)
    E = (np.arange(T)[None, :] // 64 == np.arange(128)[:, None]).astype(np.float32)
    c["c_E"] = E.astype(bf)
    se = np.zeros((32, 32, 128), np.float32)
    for e in range(32):
        se[e, e, :] = 1.0
    c["c_sele"] = se.astype(bf)
    return c


_NC_CACHE = {}


def kernel(x, w_in, diff_lambda, diff_subln_g, cmp_pos, cmp_w1, cmp_b1, cmp_w2, cmp_b2, w_out,
           ln1_g, ln1_b, router_group, router_expert, expert_w_gate, expert_w_up, expert_w_down,
           ln2_g, ln2_b, _stop_after=None, _debug=False):
    f = lambda a: np.ascontiguousarray(np.asarray(a, dtype=np.float32))
    x = f(x)
    shared = dict(
        w_in=f(w_in)[0], w_out=f(w_out)[0], diff_lambda=f(diff_lambda)[0], diff_subln_g=f(diff_subln_g)[0],
        cmp_pos=f(cmp_pos)[0], cmp_w1=f(cmp_w1)[0], cmp_b1=f(cmp_b1)[0], cmp_w2=f(cmp_w2)[0], cmp_b2=f(cmp_b2)[0],
        ln1_g=f(ln1_g)[0], ln1_b=f(ln1_b)[0], ln2_g=f(ln2_g)[0], ln2_b=f(ln2_b)[0],
        router_group=f(router_group)[0], router_expert=f(router_expert)[0],
        expert_w_gate=f(expert_w_gate)[0], expert_w_up=f(expert_w_up)[0], expert_w_down=f(expert_w_down)[0])
    consts = [_constants(r) for r in range(4)]
    in_maps = []
    for c in range(8):
        b, r = c // 4, c % 4
        xbb = x[b]
        xo = np.ascontiguousarray(xbb.reshape(64, 128, D)[r::4].reshape(TO, D))
        d = dict(shared)
        if _stop_after in ("A1", "A2", "A", "B", "C0", "C", "E1"):
            for kk in ("expert_w_gate", "expert_w_up", "expert_w_down"):
                d.pop(kk)
        d.update(consts[r])
        d["xb"] = xbb
        d["xo"] = xo
        in_maps.append(d)
    key = (_stop_after, _debug)
    nc = build_nc(stop_after=_stop_after, debug=_debug)
    res = run_bass_kernel_spmd(nc, in_maps, core_ids=list(range(8)))
    if _debug:
        return res
    outp = np.zeros((2, T, D), np.float32)
    for c in range(8):
        b, r = c // 4, c % 4
        outp[b].reshape(64, 128, D)[r::4] = res.results[c]["out"].reshape(16, 128, D)
    return outp
```

```python
import math
import numpy as np
import ml_dtypes
import concourse.bass as bass
import concourse.mybir as mybir
from concourse.bass_utils import run_bass_kernel_spmd

F32 = mybir.dt.float32
BF16 = mybir.dt.bfloat16
ALU = mybir.AluOpType
AF = mybir.ActivationFunctionType
AX = mybir.AxisListType

D = 2048
T = 8192
TO = 2048
NQT = 16
SCALE = 128 ** -0.5
EPS = 1e-5
DN_ALPHA = 2.0 ** 0.25
LAMBDA_INIT = 0.8 - 0.6 * math.exp(0.0)
NEG_BIAS = -30000.0


class Sched:
    ENGS = ("pe", "act", "dve", "pool", "sp")

    def __init__(self, nc):
        self.nc = nc
        self.ops = []
        self.last_writer = {}
        self.readers = {}
        self.dma_sems = {}
        self.last_on_eng = {}

    def _add(self, eng, fn, reads, writes, dma_sem=None, ndma=0, extra_deps=()):
        idx = len(self.ops)
        writes = tuple(writes) + tuple(k for k in reads if isinstance(k, tuple) and k[0] == "ps" and k not in writes)
        deps = set(extra_deps)
        for k in reads:
            w = self.last_writer.get(k)
            if w is not None:
                deps.add(w)
        for k in writes:
            w = self.last_writer.get(k)
            if w is not None:
                deps.add(w)
            lastc = {}
            for rd in self.readers.get(k, ()):
                ro = self.ops[rd]
                if ro["dma_sem"] is not None:
                    deps.add(rd)
                else:
                    lastc[ro["eng"]] = max(lastc.get(ro["eng"], -1), rd)
            deps.update(lastc.values())
        deps.discard(idx)
        for k in writes:
            self.last_writer[k] = idx
            self.readers[k] = []
        for k in reads:
            self.readers.setdefault(k, []).append(idx)
        self.ops.append(dict(eng=eng, fn=fn, deps=deps, dma_sem=dma_sem, ndma=ndma, sig=False))
        if fn is not None:
            self.last_on_eng[eng if dma_sem is None else ("dma", dma_sem)] = idx
        return idx

    def op(self, eng, fn, reads=(), writes=()):
        return self._add(eng, fn, tuple(reads), tuple(writes))

    def dma(self, eng, sem_name, fns, reads=(), writes=()):
        if sem_name not in self.dma_sems:
            self.dma_sems[sem_name] = None
        return self._add(eng, fns, tuple(reads), tuple(writes), dma_sem=sem_name, ndma=len(fns))

    def barrier(self):
        lasts = list(self.last_on_eng.values())
        for e in self.ENGS:
            self._add(e, None, (), (), extra_deps=lasts)

    def emit(self):
        nc = self.nc
        for o in self.ops:
            for d in o["deps"]:
                do = self.ops[d]
                if do["dma_sem"] is None:
                    if not (do["eng"] == "pe" and o["eng"] == "pe" and o["dma_sem"] is None):
                        do["sig"] = True
        cnt = {e: 0 for e in self.ENGS}
        dcnt = {k: 0 for k in self.dma_sems}
        for o in self.ops:
            if o["dma_sem"] is not None:
                dcnt[o["dma_sem"]] += 16 * o["ndma"]
                o["ev"] = ("d", o["dma_sem"], dcnt[o["dma_sem"]])
            elif o["sig"]:
                cnt[o["eng"]] += 1
                o["ev"] = ("e", o["eng"], cnt[o["eng"]])
            else:
                o["ev"] = None
        import contextlib
        with contextlib.ExitStack() as st:
            esem = {e: st.enter_context(nc.semaphore("s_" + e)) for e in self.ENGS}
            dsem = {k: st.enter_context(nc.semaphore("d_" + k)) for k in self.dma_sems}
            block = st.enter_context(nc.Block())
            ops = self.ops

            def run(ename):
                def body(eng):
                    waited = {}
                    for o in ops:
                        if o["eng"] != ename:
                            continue
                        for d in sorted(o["deps"]):
                            do = ops[d]
                            ev = do["ev"]
                            if ev is None:
                                continue
                            if ev[0] == "e" and ev[1] == "pe" and ename == "pe" and o["dma_sem"] is None:
                                continue
                            key = (ev[0], ev[1])
                            if waited.get(key, 0) >= ev[2]:
                                continue
                            waited[key] = ev[2]
                            sem = esem[ev[1]] if ev[0] == "e" else dsem[ev[1]]
                            eng.wait_ge(sem, ev[2])
                        if o["fn"] is None:
                            if o["ev"] is not None:
                                eng.nop().then_inc(esem[ename], 1)
                            continue
                        if o["dma_sem"] is not None:
                            for f in o["fn"]:
                                f(eng).then_inc(dsem[o["dma_sem"]], 16)
                        else:
                            ins = o["fn"](eng)
                            if o["ev"] is not None:
                                ins.then_inc(esem[ename], 1)
                return body

            block.tensor(run("pe"))
            block.scalar(run("act"))
            block.vector(run("dve"))
            block.gpsimd(run("pool"))
            block.sync(run("sp"))


class Arena:
    def __init__(self, nc, nelem_bf16):
        self.ap = nc.alloc_sbuf_tensor("arena", [128, nelem_bf16], BF16).ap()
        self.n = nelem_bf16
        self.off = 0

    def reset(self):
        self.off = 0

    def get(self, shape, dtype, parts=128):
        n = 1
        for s in shape:
            n *= s
        sz = n * (2 if dtype == F32 else 1)
        sz = (sz + 15) // 16 * 16
        assert self.off + sz <= self.n, ("arena overflow", self.off, sz, self.n)
        v = self.ap[0:parts, self.off:self.off + sz]
        self.off += sz
        if dtype == F32:
            v = v.bitcast(F32)
        v = v[:, 0:n]
        if len(shape) == 2:
            v = v.rearrange("p (a b) -> p a b", b=shape[1])
        elif len(shape) == 3:
            v = v.rearrange("p (a b c) -> p a b c", b=shape[1], c=shape[2])
        return v


C_DQ, C_DK, C_DV, C_NQ, C_KC, C_VC, C_KS, C_VS, C_KW, C_VW, C_NG = (
    0, 1024, 2048, 3072, 4096, 4352, 4608, 4864, 5120, 5376, 5632)


def build_nc(stop_after=None, debug=False):
    nc = bass.Bass("TRN2", target_bir_lowering=False)
    S = Sched(nc)

    def din(name, shape, dt=F32):
        return nc.dram_tensor(name, list(shape), dt, kind="ExternalInput").ap()

    xb = din("xb", [T, D]); xo = din("xo", [TO, D])
    w_in = din("w_in", [D, 5656]); w_out = din("w_out", [D, D])
    diff_lambda = din("diff_lambda", [4, 128]); subln_g = din("diff_subln_g", [256])
    cmp_pos = din("cmp_pos", [2, 32, 128]); cmp_w1 = din("cmp_w1", [2, 4096, 256])
    cmp_b1 = din("cmp_b1", [2, 256]); cmp_w2 = din("cmp_w2", [2, 256, 128]); cmp_b2 = din("cmp_b2", [2, 128])
    ln1_g = din("ln1_g", [D]); ln1_b = din("ln1_b", [D]); ln2_g = din("ln2_g", [D]); ln2_b = din("ln2_b", [D])
    r_group = din("router_group", [D, 4]); r_expert = din("router_expert", [D, 32])
    lite = stop_after in ("A1", "A2", "A", "B", "C0", "C", "E1")
    if not lite:
        wg = din("expert_w_gate", [32, D, 512]); wu = din("expert_w_up", [32, D, 512]); wd = din("expert_w_down", [32, 512, D])
    rope_k = din("rope_k", [2, 32, T]); rope_q = din("rope_q", [2, 32, TO])
    c_ident = din("c_ident", [128, 128], BF16); c_rsw = din("c_rsw", [32, 32], BF16)
    c_dmask4 = din("c_dmask4", [128, 4, 512], BF16); c_cmask4 = din("c_cmask4", [128, 5, 512], BF16)
    c_wmask4 = din("c_wmask4", [128, 8, 512], BF16)
    c_selvm = din("c_selvm", [128, 16, 128]); c_selfb = din("c_selfb", [128, 16, 128])
    c_ovl1 = din("c_ovl1", [512, 129], BF16); c_E = din("c_E", [128, T], BF16)
    c_sele = din("c_sele", [32, 32, 128], BF16)
    out = nc.dram_tensor("out", [TO, D], F32, kind="ExternalOutput").ap()
    skind = "ExternalOutput" if debug else "Internal"
    xT_s = nc.dram_tensor("xT_s", [16, 128, 16 * 512], BF16).ap()
    KT_s = nc.dram_tensor("KT_s", [16, 128, T], BF16, kind=skind).ap()
    V_s = nc.dram_tensor("V_s", [T, 1536], BF16, kind=skind).ap()
    QT_s = nc.dram_tensor("QT_s", [24, 128, TO], BF16, kind=skind).ap()
    oT_s = nc.dram_tensor("oT_s", [16, 128, TO], BF16, kind=skind).ap()
    x1_s = nc.dram_tensor("x1_s", [TO, D], F32, kind=skind).ap()
    x1T_s = nc.dram_tensor("x1T_s", [16, 128, TO], BF16).ap()

    def sb(name, shape, dt):
        return nc.alloc_sbuf_tensor(name, list(shape), dt).ap()

    ident = sb("ident", [128, 128], BF16); rsw = sb("rsw", [32, 32], BF16)
    gates = sb("gates", [128, 16, 24], F32)
    ones_bf = sb("ones_bf", [128, 128], BF16)
    kcT = [sb("kcT%d" % g, [128, 512], BF16) for g in range(2)]
    VCB = [sb("VCB%d" % g, [128, 4, 128], BF16) for g in range(2)]
    dwT = sb("dwT", [32, TO], BF16)
    A = Arena(nc, 92 * 1024)
    ps = [nc.alloc_psum_tensor("ps%d" % i, [128, 512], F32).ap() for i in range(8)]
    psb = [p.bitcast(BF16) for p in ps]

    def O(eng, method, reads, writes, *a, **kw):
        return S.op(eng, lambda e: getattr(e, method)(*a, **kw), reads, writes)

    def DM(sem, out_, in_, reads, writes, eng="sp", **kw):
        return S.dma(eng, sem, [lambda e: e.dma_start(out=out_, in_=in_, **kw)], reads, writes)

    def DMS(sem, out_, in_, nsplit, reads, writes):
        nt = out_.shape[1]
        step = nt // nsplit
        fns = [(lambda e, i=i: e.dma_start(out=out_[:, i * step:(i + 1) * step, :], in_=in_[:, i * step:(i + 1) * step, :])) for i in range(nsplit)]
        return S.dma("sp", sem, fns, reads, writes)

    def MM(out_, lhsT, rhs, start, stop, reads, writes):
        return S.op("pe", lambda e: e.matmul(out_, lhsT=lhsT, rhs=rhs, start=start, stop=stop, skip_group_check=True), reads, writes)

    def ps3(bank):
        return ps[bank].rearrange("p (h k) -> p h k", k=128)

    def TR(out_, in_, reads, writes):
        return S.op("pe", lambda e: e.transpose(out=out_, in_=in_, identity=ident), list(reads) + ["ident"], writes)

    rr = {"c": 0}

    def cast_eng(choices=("pool", "dve", "act")):
        rr["c"] += 1
        return choices[rr["c"] % len(choices)]

    def COPY(eng, out_, in_, reads, writes):
        if eng == "act":
            return O("act", "activation", reads, writes, out=out_, in_=in_, func=AF.Copy)
        return O(eng, "tensor_copy", reads, writes, out=out_, in_=in_)

    DM("c0", ident, c_ident, [], ["ident"])
    DM("c1", rsw, c_rsw, [], ["rsw"])
    O("pool", "memset", [], ["ones_bf"], ones_bf, 1.0)

    def load_weight_cols(Wsb, col_ranges, wkey, nm):
        ncols = sum(w for _, w in col_ranges)
        stg = [A.get([ncols], F32) for _ in range(2)]
        for dc in range(16):
            s = dc % 2
            fns = []
            off = 0
            for (c0, w) in col_ranges:
                fns.append(lambda e, c0=c0, w=w, off=off, s=s, dc=dc: e.dma_start(
                    out=stg[s][:, off:off + w], in_=w_in[dc * 128:(dc + 1) * 128, c0:c0 + w]))
                off += w
            S.dma("sp", "wst%s%d" % (nm, s), fns, [], [("wstg", nm, s)])
            COPY(cast_eng(), Wsb[:, dc, :], stg[s], [("wstg", nm, s)], [wkey])

    def x_chunk_to_xT(xsrc, tc, xT, xTkey, bufs, nm):
        xst, xbf = bufs
        for t in range(4):
            s = (tc * 4 + t) % 2
            DM("xst%s%d" % (nm, s), xst[s], xsrc[tc * 512 + t * 128: tc * 512 + (t + 1) * 128, :], [], [("xst", s)])
            COPY(cast_eng(("dve", "act")), xbf[s], xst[s], [("xst", s)], [("xbf", s)])
            for hb in range(2):
                bank = hb
                for j in range(8):
                    dc = hb * 8 + j
                    TR(psb[bank][:, j * 128:(j + 1) * 128], xbf[s][:, dc * 128:(dc + 1) * 128], [("xbf", s)], [("ps", bank)])
                COPY(cast_eng(("act", "dve")), xT[:, hb * 8:(hb + 1) * 8, t * 128:(t + 1) * 128],
                     psb[bank].rearrange("p (j k) -> p j k", k=128), [("ps", bank)], [xTkey])

    def rope_fix(dst32, psacc32, ropet, s, keys_r, keys_w, tmpA, tmpB):
        MM(ps[6][0:32, :], rsw[:, :], dst32, True, True, ["rsw"] + keys_w, [("ps", 6)])
        O("dve", "tensor_tensor", [("ps", 6), ("ropet", s)], ["tmpA"], out=tmpA, in0=ps[6][0:32, :], in1=ropet[:, 1, :], op=ALU.mult)
        O("dve", "tensor_tensor", keys_r + [("ropet", s)], ["tmpB"], out=tmpB, in0=psacc32, in1=ropet[:, 0, :], op=ALU.mult)
        O("dve", "tensor_tensor", ["tmpA", "tmpB"], keys_w, out=dst32, in0=tmpA, in1=tmpB, op=ALU.add)

    A.reset()
    Wk = A.get([16, 2048], BF16)
    load_weight_cols(Wk, [(C_DK, 1024), (C_KC, 256), (C_KS, 256), (C_KW, 256), (C_VC, 256)], "Wk", "k")
    xst = [A.get([2048], F32) for _ in range(2)]
    xbf = [A.get([2048], BF16) for _ in range(2)]
    xTs = [A.get([16, 512], BF16) for _ in range(2)]
    Kst = A.get([16, 512], BF16)
    ropet = [A.get([2, 512], F32, parts=32) for _ in range(2)]
    tmpA = A.get([512], F32, parts=32); tmpB = A.get([512], F32, parts=32)
    rope_tiles_k = set(range(0, 8)) | {10, 11, 12, 13}
    for tc in range(16):
        s = tc % 2
        xT = xTs[s]
        x_chunk_to_xT(xb, tc, xT, ("xT", s), (xst, xbf), "a")
        DM("xTst%d" % s, xT_s[tc].rearrange("p (c t) -> p c t", t=512), xT, [("xT", s)], [("xT_s", tc)])
        DM("rope%d" % s, ropet[s], rope_k[:, :, tc * 512:(tc + 1) * 512].rearrange("a p t -> p a t"), [], [("ropet", s)])
        pend = None
        for ct in range(16):
            bank = 2 + ct % 4
            for dc in range(16):
                MM(ps[bank], Wk[:, dc, ct * 128:(ct + 1) * 128], xT[:, dc, :], dc == 0, dc == 15, ["Wk", ("xT", s)], [("ps", bank)])
            COPY(cast_eng(("act", "dve")), Kst[:, ct, :], ps[bank], [("ps", bank)], [("Kst", ct)])
            if pend is not None:
                pend(); pend = None
            if ct in rope_tiles_k:
                pend = (lambda ct=ct, bank=bank, s=s: rope_fix(Kst[0:32, ct, :], ps[bank][0:32, :], ropet[s], s, [("ps", bank)], [("Kst", ct)], tmpA, tmpB))
        if pend is not None:
            pend(); pend = None
        DM("Kstst", KT_s[:, :, tc * 512:(tc + 1) * 512].rearrange("i p t -> p i t"), Kst,
           [("Kst", ct) for ct in range(16)], [("KT_s", tc)])
    S.barrier()
    if stop_after == "A1":
        S.emit(); return nc

    A.reset()
    Wv = A.get([16, 1536], BF16)
    load_weight_cols(Wv, [(C_DV, 1024), (C_VS, 256), (C_VW, 256)], "Wv", "v")
    xTs = [A.get([16, 512], BF16) for _ in range(2)]
    Vst = [A.get([4, 1536], BF16) for _ in range(2)]
    for tc in range(16):
        s = tc % 2
        xT = xTs[s]
        DM("xTld%d" % s, xT, xT_s[tc].rearrange("p (c t) -> p c t", t=512), [("xT_s", tc)], [("xT", s)])
        for t in range(4):
            for nb in range(3):
                bank = 2 + (t * 3 + nb) % 4
                for dc in range(16):
                    MM(ps[bank], xT[:, dc, t * 128:(t + 1) * 128], Wv[:, dc, nb * 512:(nb + 1) * 512], dc == 0, dc == 15,
                       ["Wv", ("xT", s)], [("ps", bank)])
                COPY(cast_eng(("act", "dve")), Vst[s][:, t, nb * 512:(nb + 1) * 512], ps[bank], [("ps", bank)], [("Vst", s)])
        DM("Vstst%d" % s, V_s[tc * 512:(tc + 1) * 512, :].rearrange("(t p) c -> p t c", p=128), Vst[s], [("Vst", s)], [("V_s", tc)])
    S.barrier()
    if stop_after == "A2":
        S.emit(); return nc

    A.reset()
    Wq = A.get([16, 2048], BF16)
    load_weight_cols(Wq, [(C_DQ, 1024), (C_NQ, 1024)], "Wq", "q")
    Wgt = A.get([16, 24], BF16)
    load_weight_cols(Wgt, [(C_NG, 24)], "Wgt", "g")
    xst = [A.get([2048], F32) for _ in range(2)]
    xbf = [A.get([2048], BF16) for _ in range(2)]
    xTs = [A.get([16, 512], BF16) for _ in range(2)]
    Qst = A.get([24, 512], BF16)
    ropet = [A.get([2, 512], F32, parts=32) for _ in range(2)]
    tmpA = A.get([512], F32, parts=32); tmpB = A.get([512], F32, parts=32)
    for oc in range(4):
        s = oc % 2
        xT = xTs[s]
        x_chunk_to_xT(xo, oc, xT, ("xT", s), (xst, xbf), "q")
        DM("rope%d" % s, ropet[s], rope_q[:, :, oc * 512:(oc + 1) * 512].rearrange("a p t -> p a t"), [], [("ropet", s)])
        pend = None
        for ct in range(16):
            bank = 2 + ct % 4
            for dc in range(16):
                MM(ps[bank], Wq[:, dc, ct * 128:(ct + 1) * 128], xT[:, dc, :], dc == 0, dc == 15, ["Wq", ("xT", s)], [("ps", bank)])
            if ct < 8:
                COPY(cast_eng(("act", "dve")), Qst[:, ct, :], ps[bank], [("ps", bank)], [("Qst", ct)])
                dstt = ct
            else:
                COPY("act", Qst[:, ct, :], ps[bank], [("ps", bank)], [("Qst", ct)])
                COPY("dve", Qst[:, ct + 8, :], ps[bank], [("ps", bank)], [("Qst", ct + 8)])
                dstt = ct + 8
            if pend is not None:
                pend(); pend = None
            pend = (lambda dstt=dstt, bank=bank, s=s: rope_fix(Qst[0:32, dstt, :], ps[bank][0:32, :], ropet[s], s, [("ps", bank)], [("Qst", dstt)], tmpA, tmpB))
        if pend is not None:
            pend(); pend = None
        for t in range(4):
            for dc in range(16):
                MM(ps[7][:, 0:24], xT[:, dc, t * 128:(t + 1) * 128], Wgt[:, dc, :], dc == 0, dc == 15, ["Wgt", ("xT", s)], [("ps", 7)])
            O("act", "activation", [("ps", 7)], ["gates"], out=gates[:, oc * 4 + t, :], in_=ps[7][:, 0:24], func=AF.Sigmoid)
        DM("Qstst", QT_s[:, :, oc * 512:(oc + 1) * 512].rearrange("i p t -> p i t"), Qst,
           [("Qst", ct) for ct in range(24)], [("QT_s", oc)])
    S.barrier()
    if stop_after == "A":
        S.emit(); return nc

    A.reset()
    KT2 = [A.get([2, T], BF16) for _ in range(2)]
    Vh = [A.get([64, 257], BF16) for _ in range(2)]
    QT2 = [A.get([2, TO], BF16) for _ in range(2)]
    oTh = [A.get([2, TO], BF16) for _ in range(2)]
    PT = [A.get([512], BF16) for _ in range(3)]
    dmask4 = A.get([4, 512], BF16)
    lamb = A.get([512], F32); lt = A.get([256], F32); g08 = A.get([256], F32)
    sm = A.get([16], F32)
    od = A.get([256], F32); junk = A.get([256], F32); obf = A.get([256], BF16)
    DM("c2", dmask4, c_dmask4, [], ["dmask4"])
    DM("c3", lamb, diff_lambda.rearrange("a d -> (a d)").partition_broadcast(128), [], ["lamb"])
    DM("c4", g08, subln_g.partition_broadcast(128), [], ["g08"])
    O("dve", "tensor_scalar", ["g08"], ["g08"], out=g08, in0=g08, scalar1=1.0 - LAMBDA_INIT, scalar2=None, op0=ALU.mult)
    for i in range(2):
        O("dve", "tensor_tensor", ["lamb"], ["lt"], out=lt[:, 0:128], in0=lamb[:, i * 256:i * 256 + 128], in1=lamb[:, i * 256 + 128:i * 256 + 256], op=ALU.mult)
        O("dve", "reduce_sum", ["lt"], [("sm", i)], out=sm[:, i:i + 1], in_=lt[:, 0:128], axis=AX.X)
        O("act", "activation", [("sm", i)], [("sm", i)], out=sm[:, i:i + 1], in_=sm[:, i:i + 1], func=AF.Exp)
    O("dve", "tensor_tensor", [("sm", 0), ("sm", 1)], ["nlam"], out=sm[:, 2:3], in0=sm[:, 1:2], in1=sm[:, 0:1], op=ALU.subtract)
    O("dve", "tensor_scalar", ["nlam"], ["nlam"], out=sm[:, 2:3], in0=sm[:, 2:3], scalar1=-LAMBDA_INIT, scalar2=None, op0=ALU.add)
    for s in range(2):
        O("pool", "memset", [], [("Vh1", s)], Vh[s][:, :, 256:257], 1.0)
    O("dve", "memset", [], ["epsb"], sm[:, 8:9], EPS)
    steps = []
    cnt = {"pt": 0, "sb": 0}
    def mk_pre_head(h):
        s = h % 2

        def pre_head():
            DM("kt2_%d" % s, KT2[s], KT_s[2 * h:2 * h + 2].rearrange("i p t -> p i t"), [("KT_s", tc) for tc in range(16)], [("KT2", s)])
            DMS("vh_%d" % s, Vh[s][:, :, 0:256], V_s[:, 256 * h:256 * h + 256].rearrange("(t p) c -> p t c", p=128), 8,
                [("V_s", tc) for tc in range(16)], [("Vh", s)])
            DM("qt2_%d" % s, QT2[s], QT_s[2 * h:2 * h + 2].rearrange("i p t -> p i t"), [("QT_s", oc) for oc in range(4)], [("QT2", s)])
        return pre_head

    for h in range(4):
        s = h % 2
        pre_head = mk_pre_head(h)
        nxt_pre = mk_pre_head(h + 1) if h + 1 < 4 else None

        def _unused(h=h, s=s):
            DM("kt2_%d" % s, KT2[s], KT_s[2 * h:2 * h + 2].rearrange("i p t -> p i t"), [("KT_s", tc) for tc in range(16)], [("KT2", s)])
            DMS("vh_%d" % s, Vh[s][:, :, 0:256], V_s[:, 256 * h:256 * h + 256].rearrange("(t p) c -> p t c", p=128), 8,
                [("V_s", tc) for tc in range(16)], [("Vh", s)])
            DM("qt2_%d" % s, QT2[s], QT_s[2 * h:2 * h + 2].rearrange("i p t -> p i t"), [("QT_s", oc) for oc in range(4)], [("QT2", s)])

        for m in range(NQT):
            ob = (m % 2) * 2
            nkt = 4 * m + 4
            for kp in range(nkt // 2):
                sbank = 4 + cnt["sb"] % 2; cnt["sb"] += 1
                pti = cnt["pt"] % 3; cnt["pt"] += 1
                pt = PT[pti]; ptk = ("PT", pti)
                st = {}
                if h == 0 and m == 0 and kp == 0:
                    st["pre"] = pre_head
                if m == 8 and kp == 0 and nxt_pre is not None:
                    st["pre"] = nxt_pre

                def qk(s=s, m=m, kp=kp, sbank=sbank):
                    for j in range(2):
                        kt = 2 * kp + j
                        for c in range(2):
                            MM(ps[sbank][:, (j * 2 + c) * 128:(j * 2 + c + 1) * 128], KT2[s][:, c, kt * 128:(kt + 1) * 128],
                               QT2[s][:, c, m * 128:(m + 1) * 128], True, True, [("KT2", s), ("QT2", s)], [("ps", sbank)])

                def em(m=m, kp=kp, sbank=sbank, pt=pt, ptk=ptk):
                    O("act", "activation", [("ps", sbank)], [ptk], out=pt, in_=ps[sbank], func=AF.Exp, scale=SCALE)
                    for j in range(2):
                        kt = 2 * kp + j
                        if kt >= 4 * m:
                            i = kt - 4 * m
                            O("dve", "tensor_tensor", [ptk, "dmask4"], [ptk], out=pt[:, j * 256:(j + 1) * 256],
                              in0=pt[:, j * 256:(j + 1) * 256], in1=dmask4[:, i, 0:256], op=ALU.mult)

                def pv(s=s, m=m, kp=kp, pt=pt, ptk=ptk, ob=ob, nkt=nkt):
                    for j in range(2):
                        kt = 2 * kp + j
                        for c in range(2):
                            MM(ps[ob + c][:, 0:257], pt[:, (j * 2 + c) * 128:(j * 2 + c + 1) * 128], Vh[s][:, kt, :], kt == 0, kt == nkt - 1,
                               [ptk, ("Vh", s), ("Vh1", s)], [("ps", ob + c)])

                st["qk"] = qk; st["em"] = em; st["pv"] = pv
                if kp == nkt // 2 - 1:
                    def post(h=h, s=s, m=m, ob=ob):
                        O("dve", "reciprocal", [("ps", ob)], ["rl0"], out=sm[:, 4:5], in_=ps[ob][:, 256:257])
                        O("dve", "reciprocal", [("ps", ob + 1)], ["rl1"], out=sm[:, 5:6], in_=ps[ob + 1][:, 256:257])
                        O("dve", "tensor_tensor", ["rl1", "nlam"], ["rl1"], out=sm[:, 5:6], in0=sm[:, 5:6], in1=sm[:, 2:3], op=ALU.mult)
                        O("dve", "tensor_scalar", [("ps", ob), "rl0"], ["od"], out=od, in0=ps[ob][:, 0:256], scalar1=sm[:, 4:5], scalar2=None, op0=ALU.mult)
                        O("dve", "scalar_tensor_tensor", [("ps", ob + 1), "rl1", "od"], ["od"], out=od, in0=ps[ob + 1][:, 0:256], scalar=sm[:, 5:6], in1=od,
                          op0=ALU.mult, op1=ALU.add)
                        O("act", "activation", ["od"], ["junk", "ss"], out=junk, in_=od, func=AF.Square, accum_out=sm[:, 6:7])
                        O("act", "activation", ["ss", "epsb"], ["ss"], out=sm[:, 6:7], in_=sm[:, 6:7], func=AF.Ln, scale=1.0 / 256.0, bias=sm[:, 8:9])
                        O("act", "activation", ["ss"], ["ss"], out=sm[:, 6:7], in_=sm[:, 6:7], func=AF.Exp, scale=-0.5)
                        O("dve", "scalar_tensor_tensor", ["od", "ss", "g08"], ["obf"], out=obf, in0=od, scalar=sm[:, 6:7], in1=g08, op0=ALU.mult, op1=ALU.mult)

                    def post_pe(h=h, s=s, m=m):
                        for c in range(2):
                            TR(psb[6][:, c * 128:(c + 1) * 128], obf[:, c * 128:(c + 1) * 128], ["obf"], [("ps", 6)])
                        COPY("act", oTh[s][:, :, m * 128:(m + 1) * 128], psb[6][:, 0:256].rearrange("p (c k) -> p c k", k=128), [("ps", 6)], [("oTh", s)])
                        if m == NQT - 1:
                            DM("oTst%d" % s, oT_s[2 * h:2 * h + 2].rearrange("i p t -> p i t"), oTh[s], [("oTh", s)], [("oT_s", "d", h)])
                    st["post"] = post; st["post_pe"] = post_pe
                steps.append(st)

    def run_pipeline(steps, defer=2):
        pending = []
        n = len(steps)

        def finish(i):
            nonlocal pending
            steps[i]["pv"]()
            pending = [(c - 1, f) for (c, f) in pending]
            for (c, f) in [p for p in pending if p[0] <= 0]:
                f()
            pending = [p for p in pending if p[0] > 0]
            if "post" in steps[i]:
                steps[i]["post"]()
            if "post_pe" in steps[i]:
                pending.append((defer, steps[i]["post_pe"]))

        if "pre" in steps[0]:
            steps[0]["pre"]()
        steps[0]["qk"]()
        for i in range(n):
            steps[i]["em"]()
            if i + 1 < n:
                if "pre" in steps[i + 1]:
                    steps[i + 1]["pre"]()
                steps[i + 1]["qk"]()
            if i >= 1:
                finish(i - 1)
        finish(n - 1)
        for (c, f) in pending:
            f()

    run_pipeline(steps)
    S.barrier()
    if stop_after == "B":
        S.emit(); return nc

    A.reset()
    XcT = A.get([T], BF16)
    W1 = A.get([32, 256], BF16)
    w1st = [A.get([8, 256], F32) for _ in range(2)]
    posf = A.get([128], F32, parts=32); posb = A.get([128], BF16, parts=32); posT = A.get([32], BF16)
    W2f = A.get([2, 128], F32); W2 = A.get([2, 128], BF16)
    b1 = A.get([2], F32); b2c = A.get([1], F32)
    b2rf = A.get([128], F32, parts=1); b2r = A.get([128], BF16, parts=1)
    hT = A.get([2, 512], BF16)
    u = A.get([512], F32); u2 = A.get([512], F32); sg = A.get([512], F32)
    O("pool", "memset", [], ["hT"], hT, 0.0)
    for g in range(2):
        O("pool", "memset", [], [("kcT", g)], kcT[g], 0.0)
    for j in range(2):
        DM("cp0", posf, cmp_pos[j], [], ["posf"])
        COPY("dve", posb, posf, ["posf"], ["posb"])
        S.op("pe", lambda e: e.transpose(out=psb[2][:, 0:32], in_=posb, identity=ident[0:32, 0:32]), ["posb", "ident"], [("ps", 2)])
        COPY("dve", posT, psb[2][:, 0:32], [("ps", 2)], ["posT"])
        DM("cp1", W2f, cmp_w2[j].rearrange("(c p) d -> p c d", p=128), [], ["W2f"])
        COPY("dve", W2, W2f, ["W2f"], ["W2"])
        DM("cp2", b1, cmp_b1[j].rearrange("(c p) -> p c", p=128), [], ["b1"], allow_slow_non_contiguous=True)
        if j == 0:
            DM("cp3", b2c, cmp_b2[0].rearrange("(p o) -> p o", o=1), [], ["b2c"], allow_slow_non_contiguous=True)
        else:
            DM("cp3", b2rf, cmp_b2[1].rearrange("(o d) -> o d", o=1), [], ["b2rf"])
            COPY("dve", b2r, b2rf, ["b2rf"], ["b2r"])
        for q4 in range(4):
            s = q4 % 2
            DM("w1st%d" % s, w1st[s], cmp_w1[j, q4 * 1024:(q4 + 1) * 1024, :].rearrange("(l d) h -> d l h", d=128), [], [("w1st", s)])
            COPY(cast_eng(("pool", "dve")), W1[:, q4 * 8:(q4 + 1) * 8, :], w1st[s], [("w1st", s)], ["W1"])
        for hc in range(2):
            for l in range(32):
                MM(ps[2][:, hc:hc + 1], W1[:, l, hc * 128:(hc + 1) * 128], posT[:, l:l + 1], l == 0, l == 31, ["W1", "posT"], [("ps", 2)])
        O("dve", "tensor_tensor", [("ps", 2), "b1"], ["b1"], out=b1, in0=ps[2][:, 0:2], in1=b1, op=ALU.add)
        for g in range(2):
            DM("xct", XcT, KT_s[(8 if j == 0 else 14) + g], [("KT_s", tc) for tc in range(16)], ["XcT"])
            for hc in range(2):
                for l in range(32):
                    MM(ps[hc][:, 0:511], W1[:, l, hc * 128:(hc + 1) * 128], XcT[:, l:l + 16 * 510 + 1:16], l == 0, l == 31, ["W1", "XcT"], [("ps", hc)])
                O("act", "activation", [("ps", hc), "b1"], ["u"], out=u[:, 0:511], in_=ps[hc][:, 0:511], func=AF.Identity, bias=b1[:, hc:hc + 1])
                O("dve", "tensor_tensor", ["u"], ["u2"], out=u2[:, 0:511], in0=u[:, 0:511], in1=u[:, 0:511], op=ALU.mult)
                O("dve", "tensor_scalar", ["u2"], ["u2"], out=u2[:, 0:511], in0=u2[:, 0:511], scalar1=0.044715, scalar2=1.0, op0=ALU.mult, op1=ALU.add)
                O("dve", "tensor_tensor", ["u2", "u"], ["u2"], out=u2[:, 0:511], in0=u2[:, 0:511], in1=u[:, 0:511], op=ALU.mult)
                O("act", "activation", ["u2"], ["sg"], out=sg[:, 0:511], in_=u2[:, 0:511], func=AF.Sigmoid, scale=1.5957691216057308)
                O("dve", "tensor_tensor", ["u", "sg"], ["hT"], out=hT[:, hc, 0:511], in0=u[:, 0:511], in1=sg[:, 0:511], op=ALU.mult)
            if j == 0:
                for hc in range(2):
                    MM(ps[3][:, 0:511], W2[:, hc, :], hT[:, hc, 0:511], hc == 0, hc == 1, ["W2", "hT"], [("ps", 3)])
                O("act", "activation", [("ps", 3), "b2c"], [("kcT", g)], out=kcT[g][:, 0:511], in_=ps[3][:, 0:511], func=AF.Identity, bias=b2c[:, 0:1])
            else:
                for ct in range(4):
                    for hc in range(2):
                        MM(ps[3][:, ct * 128:(ct + 1) * 128], hT[:, hc, ct * 128:(ct + 1) * 128], W2[:, hc, :], hc == 0, False, ["W2", "hT"], [("ps", 3)])
                    MM(ps[3][:, ct * 128:(ct + 1) * 128], ones_bf[0:1, :], b2r[0:1, :], False, True, ["ones_bf", "b2r"], [("ps", 3)])
                COPY("act", VCB[g], ps[3].rearrange("p (c k) -> p c k", k=128), [("ps", 3)], [("VCB", g)])
    S.barrier()
    if stop_after == "C0":
        S.emit(); return nc

    A.reset()
    QTg = A.get([4, TO], BF16); QRg = A.get([4, TO], BF16)
    KsT = A.get([T], BF16); KwT = A.get([T], BF16)
    Vs1 = A.get([64, 129], BF16); Vw1 = A.get([64, 129], BF16)
    Eexp = A.get([T], BF16)
    oTg = A.get([4, TO], BF16)
    dmask4 = A.get([4, 512], BF16); cmask4 = A.get([5, 512], BF16); wmask4 = A.get([8, 512], BF16)
    selvm = A.get([16, 128], F32); selfb = A.get([16, 128], F32)
    VCA = A.get([4, 129], BF16)
    PT = [A.get([512], BF16) for _ in range(3)]
    selbT4 = A.get([4, 128], BF16)
    imp = A.get([128], F32); sc2 = A.get([128], F32); selb = A.get([128], BF16)
    mx = A.get([16], F32); sm = A.get([32], F32)
    onsa = A.get([4, 128], F32); onb = A.get([4, 128], BF16)
    DM("c2", dmask4, c_dmask4, [], ["dmask4"]); DM("c5", cmask4, c_cmask4, [], ["cmask4"]); DM("c6", wmask4, c_wmask4, [], ["wmask4"])
    DM("c7", selvm, c_selvm, [], ["selvm"]); DM("c8", selfb, c_selfb, [], ["selfb"])
    DM("c9", VCA, c_ovl1.rearrange("(t p) c -> p t c", p=128), [], ["VCA"])
    DM("c10", Eexp, c_E, [], ["Eexp"])
    O("pool", "memset", [], ["Vs1o"], Vs1[:, :, 128:129], 1.0)
    O("pool", "memset", [], ["Vw1o"], Vw1[:, :, 128:129], 1.0)
    RA = [(0, 0), (0, 129), (0, 258), (1, 0)]
    RB = [(1, 129), (1, 257), (2, 0), (2, 128)]
    RS = [(3, 0), (3, 129), (3, 258), (4, 0)]
    RW = [(4, 129), (4, 258), (5, 0), (5, 129)]
    steps = []
    cnt = {"pt": 0, "sb": 0}

    def nxt_bufs():
        sbank = 6 + cnt["sb"] % 2; cnt["sb"] += 1
        pti = cnt["pt"] % 3; cnt["pt"] += 1
        return sbank, PT[pti], ("PT", pti)

    for g in range(2):
        allK = [("KT_s", tc) for tc in range(16)]; allV = [("V_s", tc) for tc in range(16)]; allQ = [("QT_s", oc) for oc in range(4)]

        def pre_group(g=g, allK=allK, allV=allV, allQ=allQ):
            DM("n0", QTg, QT_s[8 + 4 * g:12 + 4 * g].rearrange("i p t -> p i t"), allQ, ["QTg"])
            DM("n1", QRg, QT_s[16 + 4 * g:20 + 4 * g].rearrange("i p t -> p i t"), allQ, ["QRg"])
            DM("n2", KsT, KT_s[10 + g], allK, ["KsT"]); DM("n3", KwT, KT_s[12 + g], allK, ["KwT"])
            DMS("n4", Vs1[:, :, 0:128], V_s[:, 1024 + 128 * g:1024 + 128 * g + 128].rearrange("(t p) c -> p t c", p=128), 8, allV, ["Vs1"])
            DMS("n5", Vw1[:, :, 0:128], V_s[:, 1280 + 128 * g:1280 + 128 * g + 128].rearrange("(t p) c -> p t c", p=128), 8, allV, ["Vw1"])

        for m in range(NQT):
            started = {}
            qsl = slice(m * 128, (m + 1) * 128)

            def ACC(reg, width, lhsT, rhs, last, reads, started=started):
                bank, off = reg
                st_ = bank not in started
                started[bank] = True
                MM(ps[bank][:, off:off + width], lhsT, rhs, st_, last, reads, [("ps", bank)])

            nct = m // 4 + 1
            for ct in range(nct):
                sbank, pt, ptk = nxt_bufs()
                st = {}
                if m == 0 and ct == 0:
                    st["pre"] = pre_group

                def qk(g=g, ct=ct, sbank=sbank, qsl=qsl):
                    MM(ps3(sbank), kcT[g][:, ct * 128:(ct + 1) * 128], QTg[:, :, qsl], True, True, [("kcT", g), "QTg"], [("ps", sbank)])

                def em(m=m, ct=ct, sbank=sbank, pt=pt, ptk=ptk):
                    O("act", "activation", [("ps", sbank)], [ptk], out=pt, in_=ps[sbank], func=AF.Exp, scale=SCALE)
                    i = m - 4 * ct
                    if i <= 4:
                        O("dve", "tensor_tensor", [ptk, "cmask4"], [ptk], out=pt, in0=pt, in1=cmask4[:, i, :], op=ALU.mult)

                def pv(g=g, ct=ct, nct=nct, pt=pt, ptk=ptk, ACC=ACC):
                    for hh in range(4):
                        ACC(RA[hh], 129, pt[:, hh * 128:(hh + 1) * 128], VCA[:, ct, :], ct == nct - 1, [ptk, "VCA"])
                        ACC(RB[hh], 128, pt[:, hh * 128:(hh + 1) * 128], VCB[g][:, ct, :], ct == nct - 1, [ptk, ("VCB", g)])

                st["qk"] = qk; st["em"] = em; st["pv"] = pv
                if ct == nct - 1:
                    def post(g=g, m=m):
                        for hh in range(4):
                            bk, off = RA[hh]
                            O("dve", "tensor_scalar", [("ps", bk)], [("rc", hh)], out=sm[:, hh:hh + 1], in0=ps[bk][:, off + 128:off + 129], scalar1=1e-30, scalar2=None, op0=ALU.max)
                            O("dve", "reciprocal", [("rc", hh)], [("rc", hh)], out=sm[:, hh:hh + 1], in_=sm[:, hh:hh + 1])
                            if hh == 0:
                                O("dve", "tensor_scalar", [("ps", bk), ("rc", hh)], ["imp"], out=imp, in0=ps[bk][:, off:off + 128], scalar1=sm[:, hh:hh + 1], scalar2=None, op0=ALU.mult)
                            else:
                                O("dve", "scalar_tensor_tensor", [("ps", bk), ("rc", hh), "imp"], ["imp"], out=imp, in0=ps[bk][:, off:off + 128], scalar=sm[:, hh:hh + 1], in1=imp,
                                  op0=ALU.mult, op1=ALU.add)
                        O("dve", "tensor_tensor", ["imp", "selvm"], ["imp"], out=imp, in0=imp, in1=selvm[:, m, :], op=ALU.mult)
                        O("dve", "tensor_tensor", ["imp", "selfb"], ["imp"], out=imp, in0=imp, in1=selfb[:, m, :], op=ALU.add)
                        O("dve", "max", ["imp"], ["mx"], out=mx[:, 0:8], in_=imp)
                        O("dve", "match_replace", ["imp", "mx"], ["sc2"], out=sc2, in_to_replace=mx[:, 0:8], in_values=imp, imm_value=-3.0e38)
                        O("dve", "max", ["sc2"], ["mx2"], out=mx[:, 8:16], in_=sc2)
                        O("dve", "tensor_scalar", ["imp", "mx2", "sc2"], ["sc2"], out=sc2, in0=imp, scalar1=mx[:, 15:16], scalar2=1.0, op0=ALU.is_ge, op1=ALU.subtract)
                        O("dve", "tensor_scalar", ["sc2"], ["selb"], out=selb, in0=sc2, scalar1=-NEG_BIAS, scalar2=None, op0=ALU.mult)
                        for hh in range(4):
                            bk, off = RB[hh]
                            gcol = (4 * g + hh) * 3
                            O("dve", "tensor_tensor", [("rc", hh), "gates"], [("f", hh)], out=sm[:, 8 + hh:9 + hh], in0=sm[:, hh:hh + 1], in1=gates[:, m, gcol:gcol + 1], op=ALU.mult)
                            O("dve", "tensor_scalar", [("ps", bk), ("f", hh)], [("onsa", hh)], out=onsa[:, hh, :], in0=ps[bk][:, off:off + 128], scalar1=sm[:, 8 + hh:9 + hh], scalar2=None, op0=ALU.mult)
                    st["post"] = post
                steps.append(st)
            jl = [j for j in range(8) if 4 * m - 4 + j >= 0]
            for j in jl:
                tt = 4 * m - 4 + j
                sbank, pt, ptk = nxt_bufs()
                st = {}

                def qk(tt=tt, sbank=sbank, qsl=qsl):
                    MM(ps3(sbank), KwT[:, tt * 128:(tt + 1) * 128], QRg[:, :, qsl], True, True, ["KwT", "QRg"], [("ps", sbank)])

                def em(j=j, sbank=sbank, pt=pt, ptk=ptk):
                    O("act", "activation", [("ps", sbank)], [ptk], out=pt, in_=ps[sbank], func=AF.Exp, scale=SCALE)
                    O("dve", "tensor_tensor", [ptk, "wmask4"], [ptk], out=pt, in0=pt, in1=wmask4[:, j, :], op=ALU.mult)

                def pv(j=j, tt=tt, jl=jl, pt=pt, ptk=ptk, ACC=ACC):
                    for hh in range(4):
                        ACC(RW[hh], 129, pt[:, hh * 128:(hh + 1) * 128], Vw1[:, tt, :], j == jl[-1], [ptk, "Vw1", "Vw1o"])

                st["qk"] = qk; st["em"] = em; st["pv"] = pv
                steps.append(st)
            ntt = 4 * m + 4
            for tt in range(ntt):
                sbank, pt, ptk = nxt_bufs()
                st = {}
                if tt == 0:
                    def pre_sel():
                        sbank_t, _, _ = nxt_bufs()
                        TR(psb[sbank_t][:, 0:128], selb, ["selb"], [("ps", sbank_t)])
                        for hh in range(4):
                            COPY("act", selbT4[:, hh, :], psb[sbank_t][:, 0:128], [("ps", sbank_t)], ["selbT4"])
                    st["pre"] = pre_sel

                def qk(tt=tt, sbank=sbank, qsl=qsl):
                    MM(ps3(sbank), KsT[:, tt * 128:(tt + 1) * 128], QRg[:, :, qsl], True, False, ["KsT", "QRg"], [("ps", sbank)])
                    MM(ps3(sbank), Eexp[:, tt * 128:(tt + 1) * 128], selbT4, False, True, ["Eexp", "selbT4"], [("ps", sbank)])

                def em(m=m, tt=tt, sbank=sbank, pt=pt, ptk=ptk):
                    O("act", "activation", [("ps", sbank)], [ptk], out=pt, in_=ps[sbank], func=AF.Exp, scale=SCALE)
                    if tt >= 4 * m:
                        O("dve", "tensor_tensor", [ptk, "dmask4"], [ptk], out=pt, in0=pt, in1=dmask4[:, tt - 4 * m, :], op=ALU.mult)

                def pv(tt=tt, ntt=ntt, pt=pt, ptk=ptk, ACC=ACC):
                    for hh in range(4):
                        ACC(RS[hh], 129, pt[:, hh * 128:(hh + 1) * 128], Vs1[:, tt, :], tt == ntt - 1, [ptk, "Vs1", "Vs1o"])

                st["qk"] = qk; st["em"] = em; st["pv"] = pv
                if tt == ntt - 1:
                    def post(g=g, m=m):
                        for hh in range(4):
                            gcol = (4 * g + hh) * 3
                            for (reg, gi, nm) in ((RS[hh], 1, "fs"), (RW[hh], 2, "fw")):
                                bk, off = reg
                                col = 16 + hh * 2 + (gi - 1)
                                O("dve", "reciprocal", [("ps", bk)], [(nm, hh)], out=sm[:, col:col + 1], in_=ps[bk][:, off + 128:off + 129])
                                O("dve", "tensor_tensor", [(nm, hh), "gates"], [(nm, hh)], out=sm[:, col:col + 1], in0=sm[:, col:col + 1], in1=gates[:, m, gcol + gi:gcol + gi + 1], op=ALU.mult)
                                O("dve", "scalar_tensor_tensor", [("ps", bk), (nm, hh), ("onsa", hh)], [("onsa", hh)], out=onsa[:, hh, :], in0=ps[bk][:, off:off + 128],
                                  scalar=sm[:, col:col + 1], in1=onsa[:, hh, :], op0=ALU.mult, op1=ALU.add)
                        COPY("act", onb, onsa, [("onsa", hh) for hh in range(4)], ["onb"])

                    def post_pe(g=g, m=m, qsl=qsl):
                        tb, _, _ = nxt_bufs()
                        for hh in range(4):
                            TR(psb[tb][:, hh * 128:(hh + 1) * 128], onb[:, hh, :], ["onb"], [("ps", tb)])
                        COPY("dve", oTg[:, :, qsl], psb[tb][:, 0:512].rearrange("p (c k) -> p c k", k=128), [("ps", tb)], ["oTg"])
                        if m == NQT - 1:
                            DM("oTgst", oT_s[8 + 4 * g:12 + 4 * g].rearrange("i p t -> p i t"), oTg, ["oTg"], [("oT_s", "n", g)])
                    st["post"] = post; st["post_pe"] = post_pe
                steps.append(st)
    run_pipeline(steps)
    S.barrier()
    if stop_after == "C":
        S.emit(); return nc

    def layernorm(z, gam, bet, outt, sm_, keyz, keyo, rd):
        O("act", "activation", [keyz], ["lnjunk", "ln_s1"], out=lnjunk, in_=z, func=AF.Copy, accum_out=sm_[:, 0:1])
        O("act", "activation", [keyz], ["lnjunk", "ln_s2"], out=lnjunk, in_=z, func=AF.Square, accum_out=sm_[:, 1:2])
        O("dve", "tensor_scalar", ["ln_s1"], ["ln_mu"], out=sm_[:, 2:3], in0=sm_[:, 0:1], scalar1=1.0 / D, scalar2=None, op0=ALU.mult)
        O("dve", "tensor_tensor", ["ln_mu"], ["ln_m2"], out=sm_[:, 3:4], in0=sm_[:, 2:3], in1=sm_[:, 2:3], op=ALU.mult)
        O("dve", "scalar_tensor_tensor", ["ln_s2", "ln_m2"], ["ln_var"], out=sm_[:, 4:5], in0=sm_[:, 1:2], scalar=1.0 / D, in1=sm_[:, 3:4],
          op0=ALU.mult, op1=ALU.subtract)
        O("act", "activation", ["ln_var", "ln_eps"], ["ln_var"], out=sm_[:, 4:5], in_=sm_[:, 4:5], func=AF.Ln, bias=sm_[:, 7:8])
        O("act", "activation", ["ln_var"], ["ln_rstd"], out=sm_[:, 5:6], in_=sm_[:, 4:5], func=AF.Exp, scale=-0.5)
        O("dve", "tensor_scalar", [keyz, "ln_mu", "ln_rstd"], [keyz], out=z, in0=z, scalar1=sm_[:, 2:3], scalar2=sm_[:, 5:6], op0=ALU.subtract, op1=ALU.mult)
        O("dve", "tensor_tensor", [keyz] + rd, [keyz], out=z, in0=z, in1=gam, op=ALU.mult)
        O("dve", "tensor_tensor", [keyz] + rd, [keyo], out=outt, in0=z, in1=bet, op=ALU.add)

    A.reset()
    Wo = A.get([16, D], BF16)
    wost = [A.get([D], F32) for _ in range(2)]
    for c in range(16):
        s = c % 2
        DM("wost%d" % s, wost[s], w_out[c * 128:(c + 1) * 128, :], [], [("wost", s)])
        COPY(cast_eng(), Wo[:, c, :], wost[s], [("wost", s)], ["Wo"])
    g1 = A.get([D], F32); be1 = A.get([D], F32)
    DM("c11", g1, ln1_g.partition_broadcast(128), [], ["lnp"]); DM("c12", be1, ln1_b.partition_broadcast(128), [], ["lnp"])
    Wrf = A.get([16, 36], F32); Wr = A.get([16, 36], BF16)
    DM("c13", Wrf[:, :, 0:4], r_group.rearrange("(c p) g -> p c g", p=128), [], ["Wrf"], allow_slow_non_contiguous=True)
    DM("c14", Wrf[:, :, 4:36], r_expert.rearrange("(c p) g -> p c g", p=128), [], ["Wrf"], allow_slow_non_contiguous=True)
    COPY("dve", Wr, Wrf, ["Wrf"], ["Wr"])
    oTt = [A.get([16, 128], BF16) for _ in range(2)]
    xt = [A.get([D], F32) for _ in range(2)]
    z = [A.get([D], F32) for _ in range(2)]
    x1 = [A.get([D], F32) for _ in range(2)]
    x1b = A.get([D], BF16)
    x1Tt = [A.get([16, 128], BF16) for _ in range(2)]
    lnjunk = A.get([D], BF16)
    sm = A.get([16], F32)
    lg = A.get([36], F32); rt = A.get([160], F32); dwb = A.get([32], BF16)
    O("dve", "memset", [], ["ln_eps"], sm[:, 7:8], EPS)
    for m in range(NQT):
        s = m % 2
        DM("oTt%d" % s, oTt[s], oT_s[:, :, m * 128:(m + 1) * 128].rearrange("i p t -> p i t"),
           [("oT_s", "d", h) for h in range(4)] + [("oT_s", "n", g) for g in range(2)], [("oTt", s)])
        DM("xt%d" % s, xt[s], xo[m * 128:(m + 1) * 128, :], [], [("xt", s)])
        for nb in range(4):
            for c in range(16):
                MM(ps[nb], oTt[s][:, c, :], Wo[:, c, nb * 512:(nb + 1) * 512], c == 0, c == 15, [("oTt", s), "Wo"], [("ps", nb)])
            O("dve", "scalar_tensor_tensor", [("xt", s), ("ps", nb)], [("z", s)], out=z[s][:, nb * 512:(nb + 1) * 512], in0=xt[s][:, nb * 512:(nb + 1) * 512],
              scalar=DN_ALPHA, in1=ps[nb], op0=ALU.mult, op1=ALU.add)
        layernorm(z[s], g1, be1, x1[s], sm, ("z", s), ("x1", s), ["lnp"])
        DM("x1st%d" % s, x1_s[m * 128:(m + 1) * 128, :], x1[s], [("x1", s)], [("x1_s", m)])
        COPY("act", x1b, x1[s], [("x1", s)], ["x1b"])
        for hb in range(2):
            bank = 4 + hb
            for j in range(8):
                dc = hb * 8 + j
                TR(psb[bank][:, j * 128:(j + 1) * 128], x1b[:, dc * 128:(dc + 1) * 128], ["x1b"], [("ps", bank)])
            COPY("dve" if hb == 0 else "act", x1Tt[s][:, hb * 8:(hb + 1) * 8, :], psb[bank].rearrange("p (j k) -> p j k", k=128), [("ps", bank)], [("x1Tt", s)])
        DM("x1Tst%d" % s, x1T_s[:, :, m * 128:(m + 1) * 128].rearrange("i p t -> p i t"), x1Tt[s], [("x1Tt", s)], [("x1T_s", m)])
        for dc in range(16):
            MM(ps[6][:, 0:36], x1Tt[s][:, dc, :], Wr[:, dc, :], dc == 0, dc == 15, [("x1Tt", s), "Wr"], [("ps", 6)])
        COPY("dve", lg, ps[6][:, 0:36], [("ps", 6)], ["lg"])
        R = ["rt"]
        gmx, ngmx, gsum, pg, gm = rt[:, 0:1], rt[:, 1:2], rt[:, 2:3], rt[:, 4:8], rt[:, 8:12]
        em, ee, mk, em2 = rt[:, 16:48], rt[:, 48:80], rt[:, 80:112], rt[:, 112:144]
        m1, nm1, m2, den, scw = rt[:, 144:145], rt[:, 145:146], rt[:, 146:147], rt[:, 147:148], rt[:, 148:149]
        O("dve", "reduce_max", ["lg"], R, out=gmx, in_=lg[:, 0:4], axis=AX.X)
        O("dve", "tensor_scalar", R, R, out=ngmx, in0=gmx, scalar1=-1.0, scalar2=None, op0=ALU.mult)
        O("dve", "tensor_scalar", ["lg"] + R, R, out=gm, in0=lg[:, 0:4], scalar1=gmx, scalar2=None, op0=ALU.is_ge)
        O("act", "activation", ["lg"] + R, R, out=pg, in_=lg[:, 0:4], func=AF.Exp, bias=ngmx, accum_out=gsum)
        O("dve", "tensor_scalar", R, R, out=pg, in0=gm, scalar1=1.0, scalar2=1e30, op0=ALU.subtract, op1=ALU.mult)
        for gg in range(4):
            O("dve", "tensor_scalar", ["lg"] + R, R, out=em[:, gg * 8:(gg + 1) * 8], in0=lg[:, 4 + gg * 8:12 + gg * 8], scalar1=pg[:, gg:gg + 1], scalar2=None, op0=ALU.add)
        O("dve", "reduce_max", R, R, out=m1, in_=em, axis=AX.X)
        O("dve", "tensor_scalar", R, R, out=nm1, in0=m1, scalar1=-1.0, scalar2=None, op0=ALU.mult)
        O("act", "activation", R, R, out=ee, in_=em, func=AF.Exp, bias=nm1)
        O("dve", "tensor_scalar", R, R, out=mk, in0=em, scalar1=m1, scalar2=None, op0=ALU.is_ge)
        O("dve", "scalar_tensor_tensor", R, R, out=em2, in0=mk, scalar=-1e30, in1=em, op0=ALU.mult, op1=ALU.add)
        O("dve", "reduce_max", R, R, out=m2, in_=em2, axis=AX.X)
        O("dve", "tensor_scalar", R, R, out=mk, in0=em, scalar1=m2, scalar2=None, op0=ALU.is_ge)
        O("dve", "tensor_tensor", R, R, out=ee, in0=ee, in1=mk, op=ALU.mult)
        O("dve", "reduce_sum", R, R, out=den, in_=ee, axis=AX.X)
        O("dve", "tensor_tensor", R, R, out=den, in0=den, in1=gsum, op=ALU.mult)
        O("dve", "reciprocal", R, R, out=scw, in_=den)
        O("dve", "tensor_scalar", R, ["dwb"], out=dwb, in0=ee, scalar1=scw, scalar2=None, op0=ALU.mult)
        TR(psb[7][0:32, 0:128], dwb, ["dwb"], [("ps", 7)])
        COPY("act", dwT[:, m * 128:(m + 1) * 128], psb[7][0:32, 0:128], [("ps", 7)], ["dwT"])
    S.barrier()
    if stop_after == "E1":
        S.emit(); return nc

    A.reset()
    x1T = A.get([16, 1024], BF16)
    yacc = A.get([8, D], F32)
    hTm = A.get([4, 1024], BF16)
    wb = A.get([1024], F32)
    sele = A.get([32, 128], BF16, parts=32)
    sgt = [A.get([512], F32) for _ in range(2)]; tt_ = [A.get([512], F32) for _ in range(2)]
    sm = A.get([16], F32)
    ov0 = A.off
    wgst = [A.get([16, 128], F32)]; wust = [A.get([16, 128], F32)]
    wgb = [A.get([16, 128], BF16) for _ in range(2)]; wub = [A.get([16, 128], BF16) for _ in range(2)]
    wdst = [A.get([4, 512], F32) for _ in range(2)]; wdb = [A.get([4, 512], BF16) for _ in range(2)]
    A.off = ov0
    g2 = A.get([D], F32); be2 = A.get([D], F32)
    x1r = A.get([D], F32); outt = A.get([D], F32)
    lnjunk = A.get([D], BF16)
    DM("c15", sele, c_sele, [], ["sele"])
    O("dve", "memset", [], ["ln_eps"], sm[:, 7:8], EPS)
    wi = 0; di = 0; cntd = {"i": 0}
    for hb in range(2):
        DM("x1Tld", x1T, x1T_s[:, :, hb * 1024:(hb + 1) * 1024].rearrange("i p t -> p i t"), [("x1T_s", m) for m in range(16)], ["x1T"])
        chunks = []
        for e in range(32):
            for fc in range(4):
                s_ = wi % 2; wi += 1

                def load(e=e, fc=fc):
                    DM("wgst0", wgst[0], wg[e, :, fc * 128:(fc + 1) * 128].rearrange("(c p) f -> p c f", p=128), [], [("wgst", 0)])
                    DM("wust0", wust[0], wu[e, :, fc * 128:(fc + 1) * 128].rearrange("(c p) f -> p c f", p=128), [], [("wust", 0)])

                def cast(s_=s_):
                    COPY("dve", wgb[s_], wgst[0], [("wgst", 0)], [("wgb", s_)])
                    COPY("act", wub[s_], wust[0], [("wust", 0)], [("wub", s_)])

                def compute(e=e, fc=fc, s_=s_, hb=hb):
                    if fc == 0:
                        for blk in range(2):
                            MM(ps[4 + blk], sele[:, e, :], dwT[:, hb * 1024 + blk * 512: hb * 1024 + (blk + 1) * 512], True, True, ["sele", "dwT"], [("ps", 4 + blk)])
                            COPY("act", wb[:, blk * 512:(blk + 1) * 512], ps[4 + blk], [("ps", 4 + blk)], ["wb"])
                    for blk in range(2):
                        for dc in range(16):
                            MM(ps[blk], wgb[s_][:, dc, :], x1T[:, dc, blk * 512:(blk + 1) * 512], dc == 0, dc == 15, [("wgb", s_), "x1T"], [("ps", blk)])
                        for dc in range(16):
                            MM(ps[2 + blk], wub[s_][:, dc, :], x1T[:, dc, blk * 512:(blk + 1) * 512], dc == 0, dc == 15, [("wub", s_), "x1T"], [("ps", 2 + blk)])
                        O("act", "activation", [("ps", blk)], [("sgt", blk)], out=sgt[blk], in_=ps[blk], func=AF.Silu)
                        O("dve", "tensor_tensor", [("ps", 2 + blk), "wb"], [("tt_", blk)], out=tt_[blk], in0=ps[2 + blk], in1=wb[:, blk * 512:(blk + 1) * 512], op=ALU.mult)
                        O("pool", "tensor_tensor", [("sgt", blk), ("tt_", blk)], ["hTm"], out=hTm[:, fc, blk * 512:(blk + 1) * 512], in0=sgt[blk], in1=tt_[blk], op=ALU.mult)
                chunks.append((load, cast, compute))
            for dbk in range(4):
                s_ = di % 2; di += 1

                def load(e=e, dbk=dbk, s_=s_):
                    DM("wdst%d" % s_, wdst[s_], wd[e, :, dbk * 512:(dbk + 1) * 512].rearrange("(c p) d -> p c d", p=128), [], [("wdst", s_)])

                def cast(dbk=dbk, s_=s_):
                    COPY("act" if dbk % 2 == 0 else "dve", wdb[s_], wdst[s_], [("wdst", s_)], [("wdb", s_)])

                def compute(e=e, dbk=dbk, s_=s_):
                    for t8 in range(8):
                        bank = 4 + cntd["i"] % 4; cntd["i"] += 1
                        for fc in range(4):
                            MM(ps[bank], hTm[:, fc, t8 * 128:(t8 + 1) * 128], wdb[s_][:, fc, :], fc == 0, fc == 3, ["hTm", ("wdb", s_)], [("ps", bank)])
                        ysl = yacc[:, t8, dbk * 512:(dbk + 1) * 512]
                        if e == 0:
                            COPY("dve", ysl, ps[bank], [("ps", bank)], [("yacc", t8)])
                        else:
                            O("dve", "tensor_tensor", [("ps", bank), ("yacc", t8)], [("yacc", t8)], out=ysl, in0=ps[bank], in1=ysl, op=ALU.add)
                chunks.append((load, cast, compute))
        chunks[0][0](); chunks[0][1]()
        for k in range(len(chunks)):
            if k + 1 < len(chunks):
                chunks[k + 1][0](); chunks[k + 1][1]()
            chunks[k][2]()
        S.barrier()
        DM("c11", g2, ln2_g.partition_broadcast(128), [], ["lnp"]); DM("c12", be2, ln2_b.partition_broadcast(128), [], ["lnp"])
        for t8 in range(8):
            m = hb * 8 + t8
            DM("x1r", x1r, x1_s[m * 128:(m + 1) * 128, :], [("x1_s", m)], ["x1r"])
            O("dve", "scalar_tensor_tensor", ["x1r", ("yacc", t8)], [("yacc", t8)], out=yacc[:, t8, :], in0=x1r, scalar=DN_ALPHA, in1=yacc[:, t8, :], op0=ALU.mult, op1=ALU.add)
            layernorm(yacc[:, t8, :], g2, be2, outt, sm, ("yacc", t8), "outt", ["lnp"])
            DM("outst", out[m * 128:(m + 1) * 128, :], outt, ["outt"], [("out", m)])
        S.barrier()
    S.emit()
    return nc


def _constants(r):
    bf = ml_dtypes.bfloat16
    c = {}
    inv_freq = (np.float32(500000.0) ** (-np.arange(0, 32, 2, dtype=np.float32) / np.float32(32))).astype(np.float32)
    ang = (np.arange(T, dtype=np.float32)[:, None] * inv_freq[None, :]).astype(np.float32)
    cs, sn = np.cos(ang).astype(np.float32), np.sin(ang).astype(np.float32)
    C = np.concatenate([cs.T, cs.T], axis=0)
    Sg = np.concatenate([-sn.T, sn.T], axis=0)
    rope_k = np.stack([C, Sg], axis=0).astype(np.float32)
    own = (np.arange(16)[:, None] * 4 + r) * 128 + np.arange(128)[None, :]
    own = own.reshape(-1)
    c["rope_k"] = np.ascontiguousarray(rope_k)
    c["rope_q"] = np.ascontiguousarray(rope_k[:, :, own])
    c["c_ident"] = np.eye(128, dtype=np.float32).astype(bf)
    rsw = np.zeros((32, 32), np.float32)
    for m in range(32):
        rsw[(m + 16) % 32, m] = 1.0
    c["c_rsw"] = rsw.astype(bf)
    ki = np.arange(128)[:, None]; qi = np.arange(128)[None, :]
    dm = np.zeros((128, 4, 128), np.float32)
    for i in range(4):
        dm[:, i, :] = 1.0 if i < r else ((ki <= qi).astype(np.float32) if i == r else 0.0)
    c["c_dmask4"] = np.tile(dm, (1, 1, 4)).astype(bf)
    cm = np.zeros((128, 5, 128), np.float32)
    for i in range(5):
        cm[:, i, :] = (16 * ki + 31 <= 128 * (4 * i + r) + qi).astype(np.float32)
    c["c_cmask4"] = np.tile(cm, (1, 1, 4)).astype(bf)
    wm = np.zeros((128, 8, 128), np.float32)
    for j in range(8):
        d = r + 4 - j
        if d == 4:
            wm[:, j, :] = (ki > qi)
        elif 1 <= d <= 3:
            wm[:, j, :] = 1.0
        elif d == 0:
            wm[:, j, :] = (ki <= qi)
    c["c_wmask4"] = np.tile(wm, (1, 1, 4)).astype(bf)
    svm = np.zeros((128, 16, 128), np.float32); sfb = np.zeros((128, 16, 128), np.float32)
    s_idx = np.arange(128)[None, :]
    for m in range(16):
        qpos = (4 * m + r) * 128 + np.arange(128)[:, None]
        cur = qpos // 64
        valid = s_idx <= cur
        forced = (s_idx == 0) | (s_idx == cur) | (s_idx == cur - 1)
        svm[:, m, :] = (valid & ~forced)
        sfb[:, m, :] = np.where(valid & forced, 1e30, np.where(valid, 0.0, -1e30))
    c["c_selvm"] = svm; c["c_selfb"] = sfb
    ov = np.zeros((512, 129), np.float32)
    ci = np.arange(511)[:, None] * 16; sj = np.arange(128)[None, :] * 64
    ov[:511, :128] = ((ci < sj + 64) & (ci + 32 > sj))
    ov[:, 128] = 1.0
    c["c_ovl1"] = ov.astype(bf)
    E = (np.arange(T)[None, :] // 64 == np.arange(128)[:, None]).astype(np.float32)
    c["c_E"] = E.astype(bf)
    se = np.zeros((32, 32, 128), np.float32)
    for e in range(32):
        se[e, e, :] = 1.0
    c["c_sele"] = se.astype(bf)
    return c


_NC_CACHE = {}


def kernel(x, w_in, diff_lambda, diff_subln_g, cmp_pos, cmp_w1, cmp_b1, cmp_w2, cmp_b2, w_out,
           ln1_g, ln1_b, router_group, router_expert, expert_w_gate, expert_w_up, expert_w_down,
           ln2_g, ln2_b, _stop_after=None, _debug=False):
    f = lambda a: np.ascontiguousarray(np.asarray(a, dtype=np.float32))
    x = f(x)
    shared = dict(
        w_in=f(w_in)[0], w_out=f(w_out)[0], diff_lambda=f(diff_lambda)[0], diff_subln_g=f(diff_subln_g)[0],
        cmp_pos=f(cmp_pos)[0], cmp_w1=f(cmp_w1)[0], cmp_b1=f(cmp_b1)[0], cmp_w2=f(cmp_w2)[0], cmp_b2=f(cmp_b2)[0],
        ln1_g=f(ln1_g)[0], ln1_b=f(ln1_b)[0], ln2_g=f(ln2_g)[0], ln2_b=f(ln2_b)[0],
        router_group=f(router_group)[0], router_expert=f(router_expert)[0],
        expert_w_gate=f(expert_w_gate)[0], expert_w_up=f(expert_w_up)[0], expert_w_down=f(expert_w_down)[0])
    consts = [_constants(r) for r in range(4)]
    in_maps = []
    for c in range(8):
        b, r = c // 4, c % 4
        xbb = x[b]
        xo = np.ascontiguousarray(xbb.reshape(64, 128, D)[r::4].reshape(TO, D))
        d = dict(shared)
        if _stop_after in ("A1", "A2", "A", "B", "C0", "C", "E1"):
            for kk in ("expert_w_gate", "expert_w_up", "expert_w_down"):
                d.pop(kk)
        d.update(consts[r])
        d["xb"] = xbb
        d["xo"] = xo
        in_maps.append(d)
    key = (_stop_after, _debug)
    nc = build_nc(stop_after=_stop_after, debug=_debug)
    res = run_bass_kernel_spmd(nc, in_maps, core_ids=list(range(8)))
    if _debug:
        return res
    outp = np.zeros((2, T, D), np.float32)
    for c in range(8):
        b, r = c // 4, c % 4
        outp[b].reshape(64, 128, D)[r::4] = res.results[c]["out"].reshape(16, 128, D)
    return outp
```

```python
import math
import numpy as np
import ml_dtypes
import concourse.bass as bass
import concourse.mybir as mybir
from concourse.bass_utils import run_bass_kernel_spmd

F32 = mybir.dt.float32
BF16 = mybir.dt.bfloat16
ALU = mybir.AluOpType
AF = mybir.ActivationFunctionType
AX = mybir.AxisListType

D = 2048
T = 8192
TO = 2048
NQT = 16
SCALE = 128 ** -0.5
EPS = 1e-5
DN_ALPHA = 2.0 ** 0.25
LAMBDA_INIT = 0.8 - 0.6 * math.exp(0.0)
NEG_BIAS = -30000.0


class Sched:
    ENGS = ("pe", "act", "dve", "pool", "sp")

    def __init__(self, nc):
        self.nc = nc
        self.ops = []
        self.last_writer = {}
        self.readers = {}
        self.dma_sems = {}
        self.last_on_eng = {}

    def _add(self, eng, fn, reads, writes, dma_sem=None, ndma=0, extra_deps=()):
        idx = len(self.ops)
        writes = tuple(writes) + tuple(k for k in reads if isinstance(k, tuple) and k[0] == "ps" and k not in writes)
        deps = set(extra_deps)
        for k in reads:
            w = self.last_writer.get(k)
            if w is not None:
                deps.add(w)
        for k in writes:
            w = self.last_writer.get(k)
            if w is not None:
                deps.add(w)
            lastc = {}
            for rd in self.readers.get(k, ()):
                ro = self.ops[rd]
                if ro["dma_sem"] is not None:
                    deps.add(rd)
                else:
                    lastc[ro["eng"]] = max(lastc.get(ro["eng"], -1), rd)
            deps.update(lastc.values())
        deps.discard(idx)
        for k in writes:
            self.last_writer[k] = idx
            self.readers[k] = []
        for k in reads:
            self.readers.setdefault(k, []).append(idx)
        self.ops.append(dict(eng=eng, fn=fn, deps=deps, dma_sem=dma_sem, ndma=ndma, sig=False))
        if fn is not None:
            self.last_on_eng[eng if dma_sem is None else ("dma", dma_sem)] = idx
        return idx

    def op(self, eng, fn, reads=(), writes=()):
        return self._add(eng, fn, tuple(reads), tuple(writes))

    def dma(self, eng, sem_name, fns, reads=(), writes=()):
        if sem_name not in self.dma_sems:
            self.dma_sems[sem_name] = None
        return self._add(eng, fns, tuple(reads), tuple(writes), dma_sem=sem_name, ndma=len(fns))

    def barrier(self):
        lasts = list(self.last_on_eng.values())
        for e in self.ENGS:
            self._add(e, None, (), (), extra_deps=lasts)

    def emit(self):
        nc = self.nc
        for o in self.ops:
            for d in o["deps"]:
                do = self.ops[d]
                if do["dma_sem"] is None:
                    if not (do["eng"] == "pe" and o["eng"] == "pe" and o["dma_sem"] is None):
                        do["sig"] = True
        cnt = {e: 0 for e in self.ENGS}
        dcnt = {k: 0 for k in self.dma_sems}
        for o in self.ops:
            if o["dma_sem"] is not None:
                dcnt[o["dma_sem"]] += 16 * o["ndma"]
                o["ev"] = ("d", o["dma_sem"], dcnt[o["dma_sem"]])
            elif o["sig"]:
                cnt[o["eng"]] += 1
                o["ev"] = ("e", o["eng"], cnt[o["eng"]])
            else:
                o["ev"] = None
        import contextlib
        with contextlib.ExitStack() as st:
            esem = {e: st.enter_context(nc.semaphore("s_" + e)) for e in self.ENGS}
            dsem = {k: st.enter_context(nc.semaphore("d_" + k)) for k in self.dma_sems}
            block = st.enter_context(nc.Block())
            ops = self.ops

            def run(ename):
                def body(eng):
                    waited = {}
                    for o in ops:
                        if o["eng"] != ename:
                            continue
                        for d in sorted(o["deps"]):
                            do = ops[d]
                            ev = do["ev"]
                            if ev is None:
                                continue
                            if ev[0] == "e" and ev[1] == "pe" and ename == "pe" and o["dma_sem"] is None:
                                continue
                            key = (ev[0], ev[1])
                            if waited.get(key, 0) >= ev[2]:
                                continue
                            waited[key] = ev[2]
                            sem = esem[ev[1]] if ev[0] == "e" else dsem[ev[1]]
                            eng.wait_ge(sem, ev[2])
                        if o["fn"] is None:
                            if o["ev"] is not None:
                                eng.nop().then_inc(esem[ename], 1)
                            continue
                        if o["dma_sem"] is not None:
                            for f in o["fn"]:
                                f(eng).then_inc(dsem[o["dma_sem"]], 16)
                        else:
                            ins = o["fn"](eng)
                            if o["ev"] is not None:
                                ins.then_inc(esem[ename], 1)
                return body

            block.tensor(run("pe"))
            block.scalar(run("act"))
            block.vector(run("dve"))
            block.gpsimd(run("pool"))
            block.sync(run("sp"))


class Arena:
    def __init__(self, nc, nelem_bf16):
        self.ap = nc.alloc_sbuf_tensor("arena", [128, nelem_bf16], BF16).ap()
        self.n = nelem_bf16
        self.off = 0

    def reset(self):
        self.off = 0

    def get(self, shape, dtype, parts=128):
        n = 1
        for s in shape:
            n *= s
        sz = n * (2 if dtype == F32 else 1)
        sz = (sz + 15) // 16 * 16
        assert self.off + sz <= self.n, ("arena overflow", self.off, sz, self.n)
        v = self.ap[0:parts, self.off:self.off + sz]
        self.off += sz
        if dtype == F32:
            v = v.bitcast(F32)
        v = v[:, 0:n]
        if len(shape) == 2:
            v = v.rearrange("p (a b) -> p a b", b=shape[1])
        elif len(shape) == 3:
            v = v.rearrange("p (a b c) -> p a b c", b=shape[1], c=shape[2])
        return v


C_DQ, C_DK, C_DV, C_NQ, C_KC, C_VC, C_KS, C_VS, C_KW, C_VW, C_NG = (
    0, 1024, 2048, 3072, 4096, 4352, 4608, 4864, 5120, 5376, 5632)


def build_nc(stop_after=None, debug=False):
    nc = bass.Bass("TRN2", target_bir_lowering=False)
    S = Sched(nc)

    def din(name, shape, dt=F32):
        return nc.dram_tensor(name, list(shape), dt, kind="ExternalInput").ap()

    xb = din("xb", [T, D]); xo = din("xo", [TO, D])
    w_in = din("w_in", [D, 5656]); w_out = din("w_out", [D, D])
    diff_lambda = din("diff_lambda", [4, 128]); subln_g = din("diff_subln_g", [256])
    cmp_pos = din("cmp_pos", [2, 32, 128]); cmp_w1 = din("cmp_w1", [2, 4096, 256])
    cmp_b1 = din("cmp_b1", [2, 256]); cmp_w2 = din("cmp_w2", [2, 256, 128]); cmp_b2 = din("cmp_b2", [2, 128])
    ln1_g = din("ln1_g", [D]); ln1_b = din("ln1_b", [D]); ln2_g = din("ln2_g", [D]); ln2_b = din("ln2_b", [D])
    r_group = din("router_group", [D, 4]); r_expert = din("router_expert", [D, 32])
    lite = stop_after in ("A1", "A2", "A", "B", "C0", "C", "E1")
    if not lite:
        wg = din("expert_w_gate", [32, D, 512]); wu = din("expert_w_up", [32, D, 512]); wd = din("expert_w_down", [32, 512, D])
    rope_k = din("rope_k", [2, 32, T]); rope_q = din("rope_q", [2, 32, TO])
    c_ident = din("c_ident", [128, 128], BF16); c_rsw = din("c_rsw", [32, 32], BF16)
    c_dmask4 = din("c_dmask4", [128, 4, 512], BF16); c_cmask4 = din("c_cmask4", [128, 5, 512], BF16)
    c_wmask4 = din("c_wmask4", [128, 8, 512], BF16)
    c_selvm = din("c_selvm", [128, 16, 128]); c_selfb = din("c_selfb", [128, 16, 128])
    c_ovl1 = din("c_ovl1", [512, 129], BF16); c_E = din("c_E", [128, T], BF16)
    c_sele = din("c_sele", [32, 32, 128], BF16)
    out = nc.dram_tensor("out", [TO, D], F32, kind="ExternalOutput").ap()
    skind = "ExternalOutput" if debug else "Internal"
    xT_s = nc.dram_tensor("xT_s", [16, 128, 16 * 512], BF16).ap()
    KT_s = nc.dram_tensor("KT_s", [16, 128, T], BF16, kind=skind).ap()
    V_s = nc.dram_tensor("V_s", [T, 1536], BF16, kind=skind).ap()
    QT_s = nc.dram_tensor("QT_s", [24, 128, TO], BF16, kind=skind).ap()
    oT_s = nc.dram_tensor("oT_s", [16, 128, TO], BF16, kind=skind).ap()
    x1_s = nc.dram_tensor("x1_s", [TO, D], F32, kind=skind).ap()
    x1T_s = nc.dram_tensor("x1T_s", [16, 128, TO], BF16).ap()

    def sb(name, shape, dt):
        return nc.alloc_sbuf_tensor(name, list(shape), dt).ap()

    ident = sb("ident", [128, 128], BF16); rsw = sb("rsw", [32, 32], BF16)
    gates = sb("gates", [128, 16, 24], F32)
    ones_bf = sb("ones_bf", [128, 128], BF16)
    kcT = [sb("kcT%d" % g, [128, 512], BF16) for g in range(2)]
    VCB = [sb("VCB%d" % g, [128, 4, 128], BF16) for g in range(2)]
    dwT = sb("dwT", [32, TO], BF16)
    A = Arena(nc, 92 * 1024)
    ps = [nc.alloc_psum_tensor("ps%d" % i, [128, 512], F32).ap() for i in range(8)]
    psb = [p.bitcast(BF16) for p in ps]

    def O(eng, method, reads, writes, *a, **kw):
        return S.op(eng, lambda e: getattr(e, method)(*a, **kw), reads, writes)

    def DM(sem, out_, in_, reads, writes, eng="sp", **kw):
        return S.dma(eng, sem, [lambda e: e.dma_start(out=out_, in_=in_, **kw)], reads, writes)

    def DMS(sem, out_, in_, nsplit, reads, writes):
        nt = out_.shape[1]
        step = nt // nsplit
        fns = [(lambda e, i=i: e.dma_start(out=out_[:, i * step:(i + 1) * step, :], in_=in_[:, i * step:(i + 1) * step, :])) for i in range(nsplit)]
        return S.dma("sp", sem, fns, reads, writes)

    def MM(out_, lhsT, rhs, start, stop, reads, writes):
        return S.op("pe", lambda e: e.matmul(out_, lhsT=lhsT, rhs=rhs, start=start, stop=stop, skip_group_check=True), reads, writes)

    def ps3(bank):
        return ps[bank].rearrange("p (h k) -> p h k", k=128)

    def TR(out_, in_, reads, writes):
        return S.op("pe", lambda e: e.transpose(out=out_, in_=in_, identity=ident), list(reads) + ["ident"], writes)

    rr = {"c": 0}

    def cast_eng(choices=("pool", "dve", "act")):
        rr["c"] += 1
        return choices[rr["c"] % len(choices)]

    def COPY(eng, out_, in_, reads, writes):
        if eng == "act":
            return O("act", "activation", reads, writes, out=out_, in_=in_, func=AF.Copy)
        return O(eng, "tensor_copy", reads, writes, out=out_, in_=in_)

    DM("c0", ident, c_ident, [], ["ident"])
    DM("c1", rsw, c_rsw, [], ["rsw"])
    O("pool", "memset", [], ["ones_bf"], ones_bf, 1.0)

    def load_weight_cols(Wsb, col_ranges, wkey, nm):
        ncols = sum(w for _, w in col_ranges)
        stg = [A.get([ncols], F32) for _ in range(2)]
        for dc in range(16):
            s = dc % 2
            fns = []
            off = 0
            for (c0, w) in col_ranges:
                fns.append(lambda e, c0=c0, w=w, off=off, s=s, dc=dc: e.dma_start(
                    out=stg[s][:, off:off + w], in_=w_in[dc * 128:(dc + 1) * 128, c0:c0 + w]))
                off += w
            S.dma("sp", "wst%s%d" % (nm, s), fns, [], [("wstg", nm, s)])
            COPY(cast_eng(), Wsb[:, dc, :], stg[s], [("wstg", nm, s)], [wkey])

    def x_tile_to_xT(xsrc, tc, t, xT, xTkey, bufs, nm):
        xst, xbf = bufs
        s = (tc * 4 + t) % 2
        DM("xst%s%d" % (nm, s), xst[s], xsrc[tc * 512 + t * 128: tc * 512 + (t + 1) * 128, :], [], [("xst", s)])
        COPY(cast_eng(("dve", "act")), xbf[s], xst[s], [("xst", s)], [("xbf", s)])
        for hb in range(2):
            bank = hb
            for j in range(8):
                dc = hb * 8 + j
                TR(psb[bank][:, j * 128:(j + 1) * 128], xbf[s][:, dc * 128:(dc + 1) * 128], [("xbf", s)], [("ps", bank)])
            COPY(cast_eng(("act", "dve")), xT[:, hb * 8:(hb + 1) * 8, t * 128:(t + 1) * 128],
                 psb[bank].rearrange("p (j k) -> p j k", k=128), [("ps", bank)], [xTkey])

    def rope_fix(dst32, psacc32, ropet, s, keys_r, keys_w, tmpA, tmpB):
        MM(ps[6][0:32, :], rsw[:, :], dst32, True, True, ["rsw"] + keys_w, [("ps", 6)])
        O("dve", "tensor_tensor", [("ps", 6), ("ropet", s)], ["tmpA"], out=tmpA, in0=ps[6][0:32, :], in1=ropet[:, 1, :], op=ALU.mult)
        O("dve", "tensor_tensor", keys_r + [("ropet", s)], ["tmpB"], out=tmpB, in0=psacc32, in1=ropet[:, 0, :], op=ALU.mult)
        O("dve", "tensor_tensor", ["tmpA", "tmpB"], keys_w, out=dst32, in0=tmpA, in1=tmpB, op=ALU.add)

    A.reset()
    Wk = A.get([16, 2048], BF16)
    load_weight_cols(Wk, [(C_DK, 1024), (C_KC, 256), (C_KS, 256), (C_KW, 256), (C_VC, 256)], "Wk", "k")
    xst = [A.get([2048], F32) for _ in range(2)]
    xbf = [A.get([2048], BF16) for _ in range(2)]
    xTs = [A.get([16, 512], BF16) for _ in range(2)]
    Kst = A.get([16, 512], BF16)
    ropet = [A.get([2, 512], F32, parts=32) for _ in range(2)]
    tmpA = A.get([512], F32, parts=32); tmpB = A.get([512], F32, parts=32)
    rope_tiles_k = set(range(0, 8)) | {10, 11, 12, 13}
    for tc in range(16):
        s = tc % 2
        xT = xTs[s]
        if tc == 0:
            for t in range(4):
                x_tile_to_xT(xb, 0, t, xT, ("xT", s), (xst, xbf), "a")
        DM("xTst%d" % s, xT_s[tc].rearrange("p (c t) -> p c t", t=512), xT, [("xT", s)], [("xT_s", tc)])
        DM("rope%d" % s, ropet[s], rope_k[:, :, tc * 512:(tc + 1) * 512].rearrange("a p t -> p a t"), [], [("ropet", s)])
        pend = None
        for ct in range(16):
            bank = 2 + ct % 4
            for dc in range(16):
                MM(ps[bank], Wk[:, dc, ct * 128:(ct + 1) * 128], xT[:, dc, :], dc == 0, dc == 15, ["Wk", ("xT", s)], [("ps", bank)])
            COPY(cast_eng(("act", "dve")), Kst[:, ct, :], ps[bank], [("ps", bank)], [("Kst", ct)])
            if pend is not None:
                pend(); pend = None
            if ct in rope_tiles_k:
                pend = (lambda ct=ct, bank=bank, s=s: rope_fix(Kst[0:32, ct, :], ps[bank][0:32, :], ropet[s], s, [("ps", bank)], [("Kst", ct)], tmpA, tmpB))
            if ct in (2, 5, 8, 11) and tc + 1 < 16:
                x_tile_to_xT(xb, tc + 1, (ct - 2) // 3, xTs[1 - s], ("xT", 1 - s), (xst, xbf), "a")
        if pend is not None:
            pend(); pend = None
        DM("Kstst", KT_s[:, :, tc * 512:(tc + 1) * 512].rearrange("i p t -> p i t"), Kst,
           [("Kst", ct) for ct in range(16)], [("KT_s", tc)])
    S.barrier()
    if stop_after == "A1":
        S.emit(); return nc

    A.reset()
    Wv = A.get([16, 1536], BF16)
    load_weight_cols(Wv, [(C_DV, 1024), (C_VS, 256), (C_VW, 256)], "Wv", "v")
    xTs = [A.get([16, 512], BF16) for _ in range(2)]
    Vst = [A.get([4, 1536], BF16) for _ in range(2)]
    for tc in range(16):
        s = tc % 2
        xT = xTs[s]
        DM("xTld%d" % s, xT, xT_s[tc].rearrange("p (c t) -> p c t", t=512), [("xT_s", tc)], [("xT", s)])
        for t in range(4):
            for nb in range(3):
                bank = 2 + (t * 3 + nb) % 4
                for dc in range(16):
                    MM(ps[bank], xT[:, dc, t * 128:(t + 1) * 128], Wv[:, dc, nb * 512:(nb + 1) * 512], dc == 0, dc == 15,
                       ["Wv", ("xT", s)], [("ps", bank)])
                COPY(cast_eng(("act", "dve")), Vst[s][:, t, nb * 512:(nb + 1) * 512], ps[bank], [("ps", bank)], [("Vst", s)])
        DM("Vstst%d" % s, V_s[tc * 512:(tc + 1) * 512, :].rearrange("(t p) c -> p t c", p=128), Vst[s], [("Vst", s)], [("V_s", tc)])
    S.barrier()
    if stop_after == "A2":
        S.emit(); return nc

    A.reset()
    Wq = A.get([16, 2048], BF16)
    load_weight_cols(Wq, [(C_DQ, 1024), (C_NQ, 1024)], "Wq", "q")
    Wgt = A.get([16, 24], BF16)
    load_weight_cols(Wgt, [(C_NG, 24)], "Wgt", "g")
    xst = [A.get([2048], F32) for _ in range(2)]
    xbf = [A.get([2048], BF16) for _ in range(2)]
    xTs = [A.get([16, 512], BF16) for _ in range(2)]
    Qst = A.get([24, 512], BF16)
    ropet = [A.get([2, 512], F32, parts=32) for _ in range(2)]
    tmpA = A.get([512], F32, parts=32); tmpB = A.get([512], F32, parts=32)
    for oc in range(4):
        s = oc % 2
        xT = xTs[s]
        if oc == 0:
            for t in range(4):
                x_tile_to_xT(xo, 0, t, xT, ("xT", s), (xst, xbf), "q")
        DM("rope%d" % s, ropet[s], rope_q[:, :, oc * 512:(oc + 1) * 512].rearrange("a p t -> p a t"), [], [("ropet", s)])
        pend = None
        for ct in range(16):
            bank = 2 + ct % 4
            for dc in range(16):
                MM(ps[bank], Wq[:, dc, ct * 128:(ct + 1) * 128], xT[:, dc, :], dc == 0, dc == 15, ["Wq", ("xT", s)], [("ps", bank)])
            if ct < 8:
                COPY(cast_eng(("act", "dve")), Qst[:, ct, :], ps[bank], [("ps", bank)], [("Qst", ct)])
                dstt = ct
            else:
                COPY("act", Qst[:, ct, :], ps[bank], [("ps", bank)], [("Qst", ct)])
                COPY("dve", Qst[:, ct + 8, :], ps[bank], [("ps", bank)], [("Qst", ct + 8)])
                dstt = ct + 8
            if pend is not None:
                pend(); pend = None
            pend = (lambda dstt=dstt, bank=bank, s=s: rope_fix(Qst[0:32, dstt, :], ps[bank][0:32, :], ropet[s], s, [("ps", bank)], [("Qst", dstt)], tmpA, tmpB))
            if ct in (2, 5, 8, 11) and oc + 1 < 4:
                x_tile_to_xT(xo, oc + 1, (ct - 2) // 3, xTs[1 - s], ("xT", 1 - s), (xst, xbf), "q")
        if pend is not None:
            pend(); pend = None
        for t in range(4):
            for dc in range(16):
                MM(ps[7][:, 0:24], xT[:, dc, t * 128:(t + 1) * 128], Wgt[:, dc, :], dc == 0, dc == 15, ["Wgt", ("xT", s)], [("ps", 7)])
            O("act", "activation", [("ps", 7)], ["gates"], out=gates[:, oc * 4 + t, :], in_=ps[7][:, 0:24], func=AF.Sigmoid)
        DM("Qstst", QT_s[:, :, oc * 512:(oc + 1) * 512].rearrange("i p t -> p i t"), Qst,
           [("Qst", ct) for ct in range(24)], [("QT_s", oc)])
    S.barrier()
    if stop_after == "A":
        S.emit(); return nc

    A.reset()
    KT2 = [A.get([2, T], BF16) for _ in range(2)]
    Vh = [A.get([64, 257], BF16) for _ in range(2)]
    QT2 = [A.get([2, TO], BF16) for _ in range(2)]
    oTh = [A.get([2, TO], BF16) for _ in range(2)]
    PT = [A.get([512], BF16) for _ in range(3)]
    dmask4 = A.get([4, 512], BF16)
    lamb = A.get([512], F32); lt = A.get([256], F32); g08 = A.get([256], F32)
    sm = A.get([16], F32)
    od = A.get([256], F32); junk = A.get([256], F32); obf = A.get([256], BF16)
    DM("c2", dmask4, c_dmask4, [], ["dmask4"])
    DM("c3", lamb, diff_lambda.rearrange("a d -> (a d)").partition_broadcast(128), [], ["lamb"])
    DM("c4", g08, subln_g.partition_broadcast(128), [], ["g08"])
    O("dve", "tensor_scalar", ["g08"], ["g08"], out=g08, in0=g08, scalar1=1.0 - LAMBDA_INIT, scalar2=None, op0=ALU.mult)
    for i in range(2):
        O("dve", "tensor_tensor", ["lamb"], ["lt"], out=lt[:, 0:128], in0=lamb[:, i * 256:i * 256 + 128], in1=lamb[:, i * 256 + 128:i * 256 + 256], op=ALU.mult)
        O("dve", "reduce_sum", ["lt"], [("sm", i)], out=sm[:, i:i + 1], in_=lt[:, 0:128], axis=AX.X)
        O("act", "activation", [("sm", i)], [("sm", i)], out=sm[:, i:i + 1], in_=sm[:, i:i + 1], func=AF.Exp)
    O("dve", "tensor_tensor", [("sm", 0), ("sm", 1)], ["nlam"], out=sm[:, 2:3], in0=sm[:, 1:2], in1=sm[:, 0:1], op=ALU.subtract)
    O("dve", "tensor_scalar", ["nlam"], ["nlam"], out=sm[:, 2:3], in0=sm[:, 2:3], scalar1=-LAMBDA_INIT, scalar2=None, op0=ALU.add)
    for s in range(2):
        O("pool", "memset", [], [("Vh1", s)], Vh[s][:, :, 256:257], 1.0)
    O("dve", "memset", [], ["epsb"], sm[:, 8:9], EPS)
    steps = []
    cnt = {"pt": 0, "sb": 0}
    def mk_pre_head(h):
        s = h % 2

        def pre_head():
            DM("kt2_%d" % s, KT2[s], KT_s[2 * h:2 * h + 2].rearrange("i p t -> p i t"), [("KT_s", tc) for tc in range(16)], [("KT2", s)])
            DMS("vh_%d" % s, Vh[s][:, :, 0:256], V_s[:, 256 * h:256 * h + 256].rearrange("(t p) c -> p t c", p=128), 8,
                [("V_s", tc) for tc in range(16)], [("Vh", s)])
            DM("qt2_%d" % s, QT2[s], QT_s[2 * h:2 * h + 2].rearrange("i p t -> p i t"), [("QT_s", oc) for oc in range(4)], [("QT2", s)])
        return pre_head

    for h in range(4):
        s = h % 2
        pre_head = mk_pre_head(h)
        nxt_pre = mk_pre_head(h + 1) if h + 1 < 4 else None

        def _unused(h=h, s=s):
            DM("kt2_%d" % s, KT2[s], KT_s[2 * h:2 * h + 2].rearrange("i p t -> p i t"), [("KT_s", tc) for tc in range(16)], [("KT2", s)])
            DMS("vh_%d" % s, Vh[s][:, :, 0:256], V_s[:, 256 * h:256 * h + 256].rearrange("(t p) c -> p t c", p=128), 8,
                [("V_s", tc) for tc in range(16)], [("Vh", s)])
            DM("qt2_%d" % s, QT2[s], QT_s[2 * h:2 * h + 2].rearrange("i p t -> p i t"), [("QT_s", oc) for oc in range(4)], [("QT2", s)])

        for m in range(NQT):
            ob = (m % 2) * 2
            nkt = 4 * m + 4
            for kp in range(nkt // 2):
                sbank = 4 + cnt["sb"] % 2; cnt["sb"] += 1
                pti = cnt["pt"] % 3; cnt["pt"] += 1
                pt = PT[pti]; ptk = ("PT", pti)
                st = {}
                if h == 0 and m == 0 and kp == 0:
                    st["pre"] = pre_head
                if m == 8 and kp == 0 and nxt_pre is not None:
                    st["pre"] = nxt_pre

                def qk(s=s, m=m, kp=kp, sbank=sbank):
                    for j in range(2):
                        kt = 2 * kp + j
                        for c in range(2):
                            MM(ps[sbank][:, (j * 2 + c) * 128:(j * 2 + c + 1) * 128], KT2[s][:, c, kt * 128:(kt + 1) * 128],
                               QT2[s][:, c, m * 128:(m + 1) * 128], True, True, [("KT2", s), ("QT2", s)], [("ps", sbank)])

                def em(m=m, kp=kp, sbank=sbank, pt=pt, ptk=ptk):
                    O("act", "activation", [("ps", sbank)], [ptk], out=pt, in_=ps[sbank], func=AF.Exp, scale=SCALE)
                    for j in range(2):
                        kt = 2 * kp + j
                        if kt >= 4 * m:
                            i = kt - 4 * m
                            O("dve", "tensor_tensor", [ptk, "dmask4"], [ptk], out=pt[:, j * 256:(j + 1) * 256],
                              in0=pt[:, j * 256:(j + 1) * 256], in1=dmask4[:, i, 0:256], op=ALU.mult)

                def pv(s=s, m=m, kp=kp, pt=pt, ptk=ptk, ob=ob, nkt=nkt):
                    for j in range(2):
                        kt = 2 * kp + j
                        for c in range(2):
                            MM(ps[ob + c][:, 0:257], pt[:, (j * 2 + c) * 128:(j * 2 + c + 1) * 128], Vh[s][:, kt, :], kt == 0, kt == nkt - 1,
                               [ptk, ("Vh", s), ("Vh1", s)], [("ps", ob + c)])

                st["qk"] = qk; st["em"] = em; st["pv"] = pv
                if kp == nkt // 2 - 1:
                    def post(h=h, s=s, m=m, ob=ob):
                        O("dve", "reciprocal", [("ps", ob)], ["rl0"], out=sm[:, 4:5], in_=ps[ob][:, 256:257])
                        O("dve", "reciprocal", [("ps", ob + 1)], ["rl1"], out=sm[:, 5:6], in_=ps[ob + 1][:, 256:257])
                        O("dve", "tensor_tensor", ["rl1", "nlam"], ["rl1"], out=sm[:, 5:6], in0=sm[:, 5:6], in1=sm[:, 2:3], op=ALU.mult)
                        O("dve", "tensor_scalar", [("ps", ob), "rl0"], ["od"], out=od, in0=ps[ob][:, 0:256], scalar1=sm[:, 4:5], scalar2=None, op0=ALU.mult)
                        O("dve", "scalar_tensor_tensor", [("ps", ob + 1), "rl1", "od"], ["od"], out=od, in0=ps[ob + 1][:, 0:256], scalar=sm[:, 5:6], in1=od,
                          op0=ALU.mult, op1=ALU.add)
                        O("act", "activation", ["od"], ["junk", "ss"], out=junk, in_=od, func=AF.Square, accum_out=sm[:, 6:7])
                        O("act", "activation", ["ss", "epsb"], ["ss"], out=sm[:, 6:7], in_=sm[:, 6:7], func=AF.Ln, scale=1.0 / 256.0, bias=sm[:, 8:9])
                        O("act", "activation", ["ss"], ["ss"], out=sm[:, 6:7], in_=sm[:, 6:7], func=AF.Exp, scale=-0.5)
                        O("dve", "scalar_tensor_tensor", ["od", "ss", "g08"], ["obf"], out=obf, in0=od, scalar=sm[:, 6:7], in1=g08, op0=ALU.mult, op1=ALU.mult)

                    def post_pe(h=h, s=s, m=m):
                        for c in range(2):
                            TR(psb[6][:, c * 128:(c + 1) * 128], obf[:, c * 128:(c + 1) * 128], ["obf"], [("ps", 6)])
                        COPY("act", oTh[s][:, :, m * 128:(m + 1) * 128], psb[6][:, 0:256].rearrange("p (c k) -> p c k", k=128), [("ps", 6)], [("oTh", s)])
                        if m == NQT - 1:
                            DM("oTst%d" % s, oT_s[2 * h:2 * h + 2].rearrange("i p t -> p i t"), oTh[s], [("oTh", s)], [("oT_s", "d", h)])
                    st["post"] = post; st["post_pe"] = post_pe
                steps.append(st)

    def run_pipeline(steps, defer=2):
        pending = []
        n = len(steps)

        def finish(i):
            nonlocal pending
            steps[i]["pv"]()
            pending = [(c - 1, f) for (c, f) in pending]
            for (c, f) in [p for p in pending if p[0] <= 0]:
                f()
            pending = [p for p in pending if p[0] > 0]
            if "post" in steps[i]:
                steps[i]["post"]()
            if "post_pe" in steps[i]:
                pending.append((defer, steps[i]["post_pe"]))

        if "pre" in steps[0]:
            steps[0]["pre"]()
        steps[0]["qk"]()
        for i in range(n):
            steps[i]["em"]()
            if i + 1 < n:
                if "pre" in steps[i + 1]:
                    steps[i + 1]["pre"]()
                steps[i + 1]["qk"]()
            if i >= 1:
                finish(i - 1)
        finish(n - 1)
        for (c, f) in pending:
            f()

    run_pipeline(steps)
    S.barrier()
    if stop_after == "B":
        S.emit(); return nc

    A.reset()
    XcT = A.get([T], BF16)
    W1 = A.get([32, 256], BF16)
    w1st = [A.get([8, 256], F32) for _ in range(2)]
    posf = A.get([128], F32, parts=32); posb = A.get([128], BF16, parts=32); posT = A.get([32], BF16)
    W2f = A.get([2, 128], F32); W2 = A.get([2, 128], BF16)
    b1 = A.get([2], F32); b2c = A.get([1], F32)
    b2rf = A.get([128], F32, parts=1); b2r = A.get([128], BF16, parts=1)
    hT = A.get([2, 512], BF16)
    u = A.get([512], F32); u2 = A.get([512], F32); sg = A.get([512], F32)
    O("pool", "memset", [], ["hT"], hT, 0.0)
    for g in range(2):
        O("pool", "memset", [], [("kcT", g)], kcT[g], 0.0)
    for j in range(2):
        DM("cp0", posf, cmp_pos[j], [], ["posf"])
        COPY("dve", posb, posf, ["posf"], ["posb"])
        S.op("pe", lambda e: e.transpose(out=psb[2][:, 0:32], in_=posb, identity=ident[0:32, 0:32]), ["posb", "ident"], [("ps", 2)])
        COPY("dve", posT, psb[2][:, 0:32], [("ps", 2)], ["posT"])
        DM("cp1", W2f, cmp_w2[j].rearrange("(c p) d -> p c d", p=128), [], ["W2f"])
        COPY("dve", W2, W2f, ["W2f"], ["W2"])
        DM("cp2", b1, cmp_b1[j].rearrange("(c p) -> p c", p=128), [], ["b1"], allow_slow_non_contiguous=True)
        if j == 0:
            DM("cp3", b2c, cmp_b2[0].rearrange("(p o) -> p o", o=1), [], ["b2c"], allow_slow_non_contiguous=True)
        else:
            DM("cp3", b2rf, cmp_b2[1].rearrange("(o d) -> o d", o=1), [], ["b2rf"])
            COPY("dve", b2r, b2rf, ["b2rf"], ["b2r"])
        for q4 in range(4):
            s = q4 % 2
            DM("w1st%d" % s, w1st[s], cmp_w1[j, q4 * 1024:(q4 + 1) * 1024, :].rearrange("(l d) h -> d l h", d=128), [], [("w1st", s)])
            COPY(cast_eng(("pool", "dve")), W1[:, q4 * 8:(q4 + 1) * 8, :], w1st[s], [("w1st", s)], ["W1"])
        for hc in range(2):
            for l in range(32):
                MM(ps[2][:, hc:hc + 1], W1[:, l, hc * 128:(hc + 1) * 128], posT[:, l:l + 1], l == 0, l == 31, ["W1", "posT"], [("ps", 2)])
        O("dve", "tensor_tensor", [("ps", 2), "b1"], ["b1"], out=b1, in0=ps[2][:, 0:2], in1=b1, op=ALU.add)
        for g in range(2):
            DM("xct", XcT, KT_s[(8 if j == 0 else 14) + g], [("KT_s", tc) for tc in range(16)], ["XcT"])
            for hc in range(2):
                for l in range(32):
                    MM(ps[hc][:, 0:511], W1[:, l, hc * 128:(hc + 1) * 128], XcT[:, l:l + 16 * 510 + 1:16], l == 0, l == 31, ["W1", "XcT"], [("ps", hc)])
                O("act", "activation", [("ps", hc), "b1"], ["u"], out=u[:, 0:511], in_=ps[hc][:, 0:511], func=AF.Identity, bias=b1[:, hc:hc + 1])
                O("dve", "tensor_tensor", ["u"], ["u2"], out=u2[:, 0:511], in0=u[:, 0:511], in1=u[:, 0:511], op=ALU.mult)
                O("dve", "tensor_scalar", ["u2"], ["u2"], out=u2[:, 0:511], in0=u2[:, 0:511], scalar1=0.044715, scalar2=1.0, op0=ALU.mult, op1=ALU.add)
                O("dve", "tensor_tensor", ["u2", "u"], ["u2"], out=u2[:, 0:511], in0=u2[:, 0:511], in1=u[:, 0:511], op=ALU.mult)
                O("act", "activation", ["u2"], ["sg"], out=sg[:, 0:511], in_=u2[:, 0:511], func=AF.Sigmoid, scale=1.5957691216057308)
                O("dve", "tensor_tensor", ["u", "sg"], ["hT"], out=hT[:, hc, 0:511], in0=u[:, 0:511], in1=sg[:, 0:511], op=ALU.mult)
            if j == 0:
                for hc in range(2):
                    MM(ps[3][:, 0:511], W2[:, hc, :], hT[:, hc, 0:511], hc == 0, hc == 1, ["W2", "hT"], [("ps", 3)])
                O("act", "activation", [("ps", 3), "b2c"], [("kcT", g)], out=kcT[g][:, 0:511], in_=ps[3][:, 0:511], func=AF.Identity, bias=b2c[:, 0:1])
            else:
                for ct in range(4):
                    for hc in range(2):
                        MM(ps[3][:, ct * 128:(ct + 1) * 128], hT[:, hc, ct * 128:(ct + 1) * 128], W2[:, hc, :], hc == 0, False, ["W2", "hT"], [("ps", 3)])
                    MM(ps[3][:, ct * 128:(ct + 1) * 128], ones_bf[0:1, :], b2r[0:1, :], False, True, ["ones_bf", "b2r"], [("ps", 3)])
                COPY("act", VCB[g], ps[3].rearrange("p (c k) -> p c k", k=128), [("ps", 3)], [("VCB", g)])
    S.barrier()
    if stop_after == "C0":
        S.emit(); return nc

    A.reset()
    QTg = A.get([4, TO], BF16); QRg = A.get([4, TO], BF16)
    KsT = A.get([T], BF16); KwT = A.get([T], BF16)
    Vs1 = A.get([64, 129], BF16); Vw1 = A.get([64, 129], BF16)
    Eexp = A.get([T], BF16)
    oTg = A.get([4, TO], BF16)
    dmask4 = A.get([4, 512], BF16); cmask4 = A.get([5, 512], BF16); wmask4 = A.get([8, 512], BF16)
    selvm = A.get([16, 128], F32); selfb = A.get([16, 128], F32)
    VCA = A.get([4, 129], BF16)
    PT = [A.get([512], BF16) for _ in range(3)]
    selbT4 = A.get([4, 128], BF16)
    imp = A.get([128], F32); sc2 = A.get([128], F32); selb = A.get([128], BF16)
    mx = A.get([16], F32); sm = A.get([32], F32)
    onsa = A.get([4, 128], F32); onb = A.get([4, 128], BF16)
    DM("c2", dmask4, c_dmask4, [], ["dmask4"]); DM("c5", cmask4, c_cmask4, [], ["cmask4"]); DM("c6", wmask4, c_wmask4, [], ["wmask4"])
    DM("c7", selvm, c_selvm, [], ["selvm"]); DM("c8", selfb, c_selfb, [], ["selfb"])
    DM("c9", VCA, c_ovl1.rearrange("(t p) c -> p t c", p=128), [], ["VCA"])
    DM("c10", Eexp, c_E, [], ["Eexp"])
    O("pool", "memset", [], ["Vs1o"], Vs1[:, :, 128:129], 1.0)
    O("pool", "memset", [], ["Vw1o"], Vw1[:, :, 128:129], 1.0)
    RA = [(0, 0), (0, 129), (0, 258), (1, 0)]
    RB = [(1, 129), (1, 257), (2, 0), (2, 128)]
    RS = [(3, 0), (3, 129), (3, 258), (4, 0)]
    RW = [(4, 129), (4, 258), (5, 0), (5, 129)]
    steps = []
    cnt = {"pt": 0, "sb": 0}

    def nxt_bufs():
        sbank = 6 + cnt["sb"] % 2; cnt["sb"] += 1
        pti = cnt["pt"] % 3; cnt["pt"] += 1
        return sbank, PT[pti], ("PT", pti)

    for g in range(2):
        allK = [("KT_s", tc) for tc in range(16)]; allV = [("V_s", tc) for tc in range(16)]; allQ = [("QT_s", oc) for oc in range(4)]

        def pre_group(g=g, allK=allK, allV=allV, allQ=allQ):
            DM("n0", QTg, QT_s[8 + 4 * g:12 + 4 * g].rearrange("i p t -> p i t"), allQ, ["QTg"])
            DM("n1", QRg, QT_s[16 + 4 * g:20 + 4 * g].rearrange("i p t -> p i t"), allQ, ["QRg"])
            DM("n2", KsT, KT_s[10 + g], allK, ["KsT"]); DM("n3", KwT, KT_s[12 + g], allK, ["KwT"])
            DMS("n4", Vs1[:, :, 0:128], V_s[:, 1024 + 128 * g:1024 + 128 * g + 128].rearrange("(t p) c -> p t c", p=128), 8, allV, ["Vs1"])
            DMS("n5", Vw1[:, :, 0:128], V_s[:, 1280 + 128 * g:1280 + 128 * g + 128].rearrange("(t p) c -> p t c", p=128), 8, allV, ["Vw1"])

        for m in range(NQT):
            started = {}
            qsl = slice(m * 128, (m + 1) * 128)

            def ACC(reg, width, lhsT, rhs, last, reads, started=started):
                bank, off = reg
                st_ = bank not in started
                started[bank] = True
                MM(ps[bank][:, off:off + width], lhsT, rhs, st_, last, reads, [("ps", bank)])

            nct = m // 4 + 1
            for ct in range(nct):
                sbank, pt, ptk = nxt_bufs()
                st = {}
                if m == 0 and ct == 0:
                    st["pre"] = pre_group

                def qk(g=g, ct=ct, sbank=sbank, qsl=qsl):
                    MM(ps3(sbank), kcT[g][:, ct * 128:(ct + 1) * 128], QTg[:, :, qsl], True, True, [("kcT", g), "QTg"], [("ps", sbank)])

                def em(m=m, ct=ct, sbank=sbank, pt=pt, ptk=ptk):
                    O("act", "activation", [("ps", sbank)], [ptk], out=pt, in_=ps[sbank], func=AF.Exp, scale=SCALE)
                    i = m - 4 * ct
                    if i <= 4:
                        O("dve", "tensor_tensor", [ptk, "cmask4"], [ptk], out=pt, in0=pt, in1=cmask4[:, i, :], op=ALU.mult)

                def pv(g=g, ct=ct, nct=nct, pt=pt, ptk=ptk, ACC=ACC):
                    for hh in range(4):
                        ACC(RA[hh], 129, pt[:, hh * 128:(hh + 1) * 128], VCA[:, ct, :], ct == nct - 1, [ptk, "VCA"])
                        ACC(RB[hh], 128, pt[:, hh * 128:(hh + 1) * 128], VCB[g][:, ct, :], ct == nct - 1, [ptk, ("VCB", g)])

                st["qk"] = qk; st["em"] = em; st["pv"] = pv
                if ct == nct - 1:
                    def post(g=g, m=m):
                        for hh in range(4):
                            bk, off = RA[hh]
                            O("dve", "tensor_scalar", [("ps", bk)], [("rc", hh)], out=sm[:, hh:hh + 1], in0=ps[bk][:, off + 128:off + 129], scalar1=1e-30, scalar2=None, op0=ALU.max)
                            O("dve", "reciprocal", [("rc", hh)], [("rc", hh)], out=sm[:, hh:hh + 1], in_=sm[:, hh:hh + 1])
                            if hh == 0:
                                O("dve", "tensor_scalar", [("ps", bk), ("rc", hh)], ["imp"], out=imp, in0=ps[bk][:, off:off + 128], scalar1=sm[:, hh:hh + 1], scalar2=None, op0=ALU.mult)
                            else:
                                O("dve", "scalar_tensor_tensor", [("ps", bk), ("rc", hh), "imp"], ["imp"], out=imp, in0=ps[bk][:, off:off + 128], scalar=sm[:, hh:hh + 1], in1=imp,
                                  op0=ALU.mult, op1=ALU.add)
                        O("dve", "tensor_tensor", ["imp", "selvm"], ["imp"], out=imp, in0=imp, in1=selvm[:, m, :], op=ALU.mult)
                        O("dve", "tensor_tensor", ["imp", "selfb"], ["imp"], out=imp, in0=imp, in1=selfb[:, m, :], op=ALU.add)
                        O("dve", "max", ["imp"], ["mx"], out=mx[:, 0:8], in_=imp)
                        O("dve", "match_replace", ["imp", "mx"], ["sc2"], out=sc2, in_to_replace=mx[:, 0:8], in_values=imp, imm_value=-3.0e38)
                        O("dve", "max", ["sc2"], ["mx2"], out=mx[:, 8:16], in_=sc2)
                        O("dve", "tensor_scalar", ["imp", "mx2", "sc2"], ["sc2"], out=sc2, in0=imp, scalar1=mx[:, 15:16], scalar2=1.0, op0=ALU.is_ge, op1=ALU.subtract)
                        O("dve", "tensor_scalar", ["sc2"], ["selb"], out=selb, in0=sc2, scalar1=-NEG_BIAS, scalar2=None, op0=ALU.mult)
                        for hh in range(4):
                            bk, off = RB[hh]
                            gcol = (4 * g + hh) * 3
                            O("dve", "tensor_tensor", [("rc", hh), "gates"], [("f", hh)], out=sm[:, 8 + hh:9 + hh], in0=sm[:, hh:hh + 1], in1=gates[:, m, gcol:gcol + 1], op=ALU.mult)
                            O("dve", "tensor_scalar", [("ps", bk), ("f", hh)], [("onsa", hh)], out=onsa[:, hh, :], in0=ps[bk][:, off:off + 128], scalar1=sm[:, 8 + hh:9 + hh], scalar2=None, op0=ALU.mult)
                    st["post"] = post
                steps.append(st)
            jl = [j for j in range(8) if 4 * m - 4 + j >= 0]
            for j in jl:
                tt = 4 * m - 4 + j
                sbank, pt, ptk = nxt_bufs()
                st = {}

                def qk(tt=tt, sbank=sbank, qsl=qsl):
                    MM(ps3(sbank), KwT[:, tt * 128:(tt + 1) * 128], QRg[:, :, qsl], True, True, ["KwT", "QRg"], [("ps", sbank)])

                def em(j=j, sbank=sbank, pt=pt, ptk=ptk):
                    O("act", "activation", [("ps", sbank)], [ptk], out=pt, in_=ps[sbank], func=AF.Exp, scale=SCALE)
                    O("dve", "tensor_tensor", [ptk, "wmask4"], [ptk], out=pt, in0=pt, in1=wmask4[:, j, :], op=ALU.mult)

                def pv(j=j, tt=tt, jl=jl, pt=pt, ptk=ptk, ACC=ACC):
                    for hh in range(4):
                        ACC(RW[hh], 129, pt[:, hh * 128:(hh + 1) * 128], Vw1[:, tt, :], j == jl[-1], [ptk, "Vw1", "Vw1o"])

                st["qk"] = qk; st["em"] = em; st["pv"] = pv
                steps.append(st)
            ntt = 4 * m + 4
            for tt in range(ntt):
                sbank, pt, ptk = nxt_bufs()
                st = {}
                if tt == 0:
                    def pre_sel():
                        sbank_t, _, _ = nxt_bufs()
                        TR(psb[sbank_t][:, 0:128], selb, ["selb"], [("ps", sbank_t)])
                        for hh in range(4):
                            COPY("act", selbT4[:, hh, :], psb[sbank_t][:, 0:128], [("ps", sbank_t)], ["selbT4"])
                    st["pre"] = pre_sel

                def qk(tt=tt, sbank=sbank, qsl=qsl):
                    MM(ps3(sbank), KsT[:, tt * 128:(tt + 1) * 128], QRg[:, :, qsl], True, False, ["KsT", "QRg"], [("ps", sbank)])
                    MM(ps3(sbank), Eexp[:, tt * 128:(tt + 1) * 128], selbT4, False, True, ["Eexp", "selbT4"], [("ps", sbank)])

                def em(m=m, tt=tt, sbank=sbank, pt=pt, ptk=ptk):
                    O("act", "activation", [("ps", sbank)], [ptk], out=pt, in_=ps[sbank], func=AF.Exp, scale=SCALE)
                    if tt >= 4 * m:
                        O("dve", "tensor_tensor", [ptk, "dmask4"], [ptk], out=pt, in0=pt, in1=dmask4[:, tt - 4 * m, :], op=ALU.mult)

                def pv(tt=tt, ntt=ntt, pt=pt, ptk=ptk, ACC=ACC):
                    for hh in range(4):
                        ACC(RS[hh], 129, pt[:, hh * 128:(hh + 1) * 128], Vs1[:, tt, :], tt == ntt - 1, [ptk, "Vs1", "Vs1o"])

                st["qk"] = qk; st["em"] = em; st["pv"] = pv
                if tt == ntt - 1:
                    def post(g=g, m=m):
                        for hh in range(4):
                            gcol = (4 * g + hh) * 3
                            for (reg, gi, nm) in ((RS[hh], 1, "fs"), (RW[hh], 2, "fw")):
                                bk, off = reg
                                col = 16 + hh * 2 + (gi - 1)
                                O("dve", "reciprocal", [("ps", bk)], [(nm, hh)], out=sm[:, col:col + 1], in_=ps[bk][:, off + 128:off + 129])
                                O("dve", "tensor_tensor", [(nm, hh), "gates"], [(nm, hh)], out=sm[:, col:col + 1], in0=sm[:, col:col + 1], in1=gates[:, m, gcol + gi:gcol + gi + 1], op=ALU.mult)
                                O("dve", "scalar_tensor_tensor", [("ps", bk), (nm, hh), ("onsa", hh)], [("onsa", hh)], out=onsa[:, hh, :], in0=ps[bk][:, off:off + 128],
                                  scalar=sm[:, col:col + 1], in1=onsa[:, hh, :], op0=ALU.mult, op1=ALU.add)
                        COPY("act", onb, onsa, [("onsa", hh) for hh in range(4)], ["onb"])

                    def post_pe(g=g, m=m, qsl=qsl):
                        tb, _, _ = nxt_bufs()
                        for hh in range(4):
                            TR(psb[tb][:, hh * 128:(hh + 1) * 128], onb[:, hh, :], ["onb"], [("ps", tb)])
                        COPY("dve", oTg[:, :, qsl], psb[tb][:, 0:512].rearrange("p (c k) -> p c k", k=128), [("ps", tb)], ["oTg"])
                        if m == NQT - 1:
                            DM("oTgst", oT_s[8 + 4 * g:12 + 4 * g].rearrange("i p t -> p i t"), oTg, ["oTg"], [("oT_s", "n", g)])
                    st["post"] = post; st["post_pe"] = post_pe
                steps.append(st)
    run_pipeline(steps)
    S.barrier()
    if stop_after == "C":
        S.emit(); return nc

    def layernorm(z, gam, bet, outt, sm_, keyz, keyo, rd, tag=0):
        o = 8 * tag
        k = lambda n: (n, tag)
        jk = lnjunk[tag] if isinstance(lnjunk, list) else lnjunk
        O("act", "activation", [keyz], [k("lnjunk"), k("ln_s1")], out=jk, in_=z, func=AF.Copy, accum_out=sm_[:, o + 0:o + 1])
        O("act", "activation", [keyz], [k("lnjunk"), k("ln_s2")], out=jk, in_=z, func=AF.Square, accum_out=sm_[:, o + 1:o + 2])
        O("dve", "tensor_scalar", [k("ln_s1")], [k("ln_mu")], out=sm_[:, o + 2:o + 3], in0=sm_[:, o + 0:o + 1], scalar1=1.0 / D, scalar2=None, op0=ALU.mult)
        O("dve", "tensor_tensor", [k("ln_mu")], [k("ln_m2")], out=sm_[:, o + 3:o + 4], in0=sm_[:, o + 2:o + 3], in1=sm_[:, o + 2:o + 3], op=ALU.mult)
        O("dve", "scalar_tensor_tensor", [k("ln_s2"), k("ln_m2")], [k("ln_var")], out=sm_[:, o + 4:o + 5], in0=sm_[:, o + 1:o + 2], scalar=1.0 / D, in1=sm_[:, o + 3:o + 4],
          op0=ALU.mult, op1=ALU.subtract)
        O("act", "activation", [k("ln_var"), "ln_eps"], [k("ln_var")], out=sm_[:, o + 4:o + 5], in_=sm_[:, o + 4:o + 5], func=AF.Ln, bias=sm_[:, o + 7:o + 8])
        O("act", "activation", [k("ln_var")], [k("ln_rstd")], out=sm_[:, o + 5:o + 6], in_=sm_[:, o + 4:o + 5], func=AF.Exp, scale=-0.5)
        O("dve", "tensor_scalar", [keyz, k("ln_mu"), k("ln_rstd")], [keyz], out=z, in0=z, scalar1=sm_[:, o + 2:o + 3], scalar2=sm_[:, o + 5:o + 6], op0=ALU.subtract, op1=ALU.mult)
        O("dve", "tensor_tensor", [keyz] + rd, [keyz], out=z, in0=z, in1=gam, op=ALU.mult)
        O("dve", "tensor_tensor", [keyz] + rd, [keyo], out=outt, in0=z, in1=bet, op=ALU.add)

    A.reset()
    Wo = A.get([16, D], BF16)
    wost = [A.get([D], F32) for _ in range(2)]
    for c in range(16):
        s = c % 2
        DM("wost%d" % s, wost[s], w_out[c * 128:(c + 1) * 128, :], [], [("wost", s)])
        COPY(cast_eng(), Wo[:, c, :], wost[s], [("wost", s)], ["Wo"])
    g1 = A.get([D], F32); be1 = A.get([D], F32)
    DM("c11", g1, ln1_g.partition_broadcast(128), [], ["lnp"]); DM("c12", be1, ln1_b.partition_broadcast(128), [], ["lnp"])
    Wrf = A.get([16, 36], F32); Wr = A.get([16, 36], BF16)
    DM("c13", Wrf[:, :, 0:4], r_group.rearrange("(c p) g -> p c g", p=128), [], ["Wrf"], allow_slow_non_contiguous=True)
    DM("c14", Wrf[:, :, 4:36], r_expert.rearrange("(c p) g -> p c g", p=128), [], ["Wrf"], allow_slow_non_contiguous=True)
    COPY("dve", Wr, Wrf, ["Wrf"], ["Wr"])
    oTt = [A.get([16, 128], BF16) for _ in range(2)]
    xt = [A.get([D], F32) for _ in range(2)]
    z = [A.get([D], F32) for _ in range(2)]
    x1 = [A.get([D], F32) for _ in range(2)]
    x1b = A.get([D], BF16)
    x1Tt = [A.get([16, 128], BF16) for _ in range(2)]
    lnjunk = A.get([D], BF16)
    sm = A.get([16], F32)
    lg = A.get([36], F32); rt = A.get([160], F32); dwb = A.get([32], BF16)
    O("dve", "memset", [], ["ln_eps"], sm[:, 7:8], EPS)
    O("dve", "memset", [], ["ln_eps"], sm[:, 15:16], EPS)
    def e1_stage1(m):
        s = m % 2
        DM("oTt%d" % s, oTt[s], oT_s[:, :, m * 128:(m + 1) * 128].rearrange("i p t -> p i t"),
           [("oT_s", "d", h) for h in range(4)] + [("oT_s", "n", g) for g in range(2)], [("oTt", s)])
        DM("xt%d" % s, xt[s], xo[m * 128:(m + 1) * 128, :], [], [("xt", s)])
        for nb in range(4):
            for c in range(16):
                MM(ps[nb], oTt[s][:, c, :], Wo[:, c, nb * 512:(nb + 1) * 512], c == 0, c == 15, [("oTt", s), "Wo"], [("ps", nb)])
            O("dve", "scalar_tensor_tensor", [("xt", s), ("ps", nb)], [("z", s)], out=z[s][:, nb * 512:(nb + 1) * 512], in0=xt[s][:, nb * 512:(nb + 1) * 512],
              scalar=DN_ALPHA, in1=ps[nb], op0=ALU.mult, op1=ALU.add)
    def e1_stage2(m):
        s = m % 2
        layernorm(z[s], g1, be1, x1[s], sm, ("z", s), ("x1", s), ["lnp"], tag=s)
        DM("x1st%d" % s, x1_s[m * 128:(m + 1) * 128, :], x1[s], [("x1", s)], [("x1_s", m)])
        COPY("act", x1b, x1[s], [("x1", s)], ["x1b"])
        for hb in range(2):
            bank = 4 + hb
            for j in range(8):
                dc = hb * 8 + j
                TR(psb[bank][:, j * 128:(j + 1) * 128], x1b[:, dc * 128:(dc + 1) * 128], ["x1b"], [("ps", bank)])
            COPY("dve" if hb == 0 else "act", x1Tt[s][:, hb * 8:(hb + 1) * 8, :], psb[bank].rearrange("p (j k) -> p j k", k=128), [("ps", bank)], [("x1Tt", s)])
        DM("x1Tst%d" % s, x1T_s[:, :, m * 128:(m + 1) * 128].rearrange("i p t -> p i t"), x1Tt[s], [("x1Tt", s)], [("x1T_s", m)])
        for dc in range(16):
            MM(ps[6][:, 0:36], x1Tt[s][:, dc, :], Wr[:, dc, :], dc == 0, dc == 15, [("x1Tt", s), "Wr"], [("ps", 6)])
        COPY("dve", lg, ps[6][:, 0:36], [("ps", 6)], ["lg"])
        R = ["rt"]
        gmx, ngmx, gsum, pg, gm = rt[:, 0:1], rt[:, 1:2], rt[:, 2:3], rt[:, 4:8], rt[:, 8:12]
        em, ee, mk, em2 = rt[:, 16:48], rt[:, 48:80], rt[:, 80:112], rt[:, 112:144]
        m1, nm1, m2, den, scw = rt[:, 144:145], rt[:, 145:146], rt[:, 146:147], rt[:, 147:148], rt[:, 148:149]
        O("dve", "reduce_max", ["lg"], R, out=gmx, in_=lg[:, 0:4], axis=AX.X)
        O("dve", "tensor_scalar", R, R, out=ngmx, in0=gmx, scalar1=-1.0, scalar2=None, op0=ALU.mult)
        O("dve", "tensor_scalar", ["lg"] + R, R, out=gm, in0=lg[:, 0:4], scalar1=gmx, scalar2=None, op0=ALU.is_ge)
        O("act", "activation", ["lg"] + R, R, out=pg, in_=lg[:, 0:4], func=AF.Exp, bias=ngmx, accum_out=gsum)
        O("dve", "tensor_scalar", R, R, out=pg, in0=gm, scalar1=1.0, scalar2=1e30, op0=ALU.subtract, op1=ALU.mult)
        for gg in range(4):
            O("dve", "tensor_scalar", ["lg"] + R, R, out=em[:, gg * 8:(gg + 1) * 8], in0=lg[:, 4 + gg * 8:12 + gg * 8], scalar1=pg[:, gg:gg + 1], scalar2=None, op0=ALU.add)
        O("dve", "reduce_max", R, R, out=m1, in_=em, axis=AX.X)
        O("dve", "tensor_scalar", R, R, out=nm1, in0=m1, scalar1=-1.0, scalar2=None, op0=ALU.mult)
        O("act", "activation", R, R, out=ee, in_=em, func=AF.Exp, bias=nm1)
        O("dve", "tensor_scalar", R, R, out=mk, in0=em, scalar1=m1, scalar2=None, op0=ALU.is_ge)
        O("dve", "scalar_tensor_tensor", R, R, out=em2, in0=mk, scalar=-1e30, in1=em, op0=ALU.mult, op1=ALU.add)
        O("dve", "reduce_max", R, R, out=m2, in_=em2, axis=AX.X)
        O("dve", "tensor_scalar", R, R, out=mk, in0=em, scalar1=m2, scalar2=None, op0=ALU.is_ge)
        O("dve", "tensor_tensor", R, R, out=ee, in0=ee, in1=mk, op=ALU.mult)
        O("dve", "reduce_sum", R, R, out=den, in_=ee, axis=AX.X)
        O("dve", "tensor_tensor", R, R, out=den, in0=den, in1=gsum, op=ALU.mult)
        O("dve", "reciprocal", R, R, out=scw, in_=den)
        O("dve", "tensor_scalar", R, ["dwb"], out=dwb, in0=ee, scalar1=scw, scalar2=None, op0=ALU.mult)
        TR(psb[7][0:32, 0:128], dwb, ["dwb"], [("ps", 7)])
        COPY("act", dwT[:, m * 128:(m + 1) * 128], psb[7][0:32, 0:128], [("ps", 7)], ["dwT"])
    e1_stage1(0)
    for m in range(NQT):
        if m + 1 < NQT:
            e1_stage1(m + 1)
        e1_stage2(m)
    S.barrier()
    if stop_after == "E1":
        S.emit(); return nc

    A.reset()
    x1T = A.get([16, 1024], BF16)
    yacc = A.get([8, D], F32)
    hTm = A.get([4, 1024], BF16)
    wb = A.get([1024], F32)
    sele = A.get([32, 128], BF16, parts=32)
    sgt = [A.get([512], F32) for _ in range(2)]; tt_ = [A.get([512], F32) for _ in range(2)]
    sm = A.get([16], F32)
    ov0 = A.off
    wgst = [A.get([16, 128], F32)]; wust = [A.get([16, 128], F32)]
    wgb = [A.get([16, 128], BF16) for _ in range(2)]; wub = [A.get([16, 128], BF16) for _ in range(2)]
    wdst = [A.get([4, 512], F32) for _ in range(2)]; wdb = [A.get([4, 512], BF16) for _ in range(2)]
    A.off = ov0
    g2 = A.get([D], F32); be2 = A.get([D], F32)
    x1r = [A.get([D], F32) for _ in range(2)]; outt = [A.get([D], F32) for _ in range(2)]
    lnjunk = [A.get([D], BF16) for _ in range(2)]
    DM("c15", sele, c_sele, [], ["sele"])
    O("dve", "memset", [], ["ln_eps"], sm[:, 7:8], EPS)
    O("dve", "memset", [], ["ln_eps"], sm[:, 15:16], EPS)
    wi = 0; di = 0; cntd = {"i": 0}
    for hb in range(2):
        DM("x1Tld", x1T, x1T_s[:, :, hb * 1024:(hb + 1) * 1024].rearrange("i p t -> p i t"), [("x1T_s", m) for m in range(16)], ["x1T"])
        chunks = []
        for e in range(32):
            for fc in range(4):
                s_ = wi % 2; wi += 1

                def load(e=e, fc=fc):
                    DM("wgst0", wgst[0], wg[e, :, fc * 128:(fc + 1) * 128].rearrange("(c p) f -> p c f", p=128), [], [("wgst", 0)])
                    DM("wust0", wust[0], wu[e, :, fc * 128:(fc + 1) * 128].rearrange("(c p) f -> p c f", p=128), [], [("wust", 0)])

                def cast(s_=s_):
                    COPY("dve", wgb[s_], wgst[0], [("wgst", 0)], [("wgb", s_)])
                    COPY("act", wub[s_], wust[0], [("wust", 0)], [("wub", s_)])

                def compute(e=e, fc=fc, s_=s_, hb=hb):
                    if fc == 0:
                        for blk in range(2):
                            MM(ps[4 + blk], sele[:, e, :], dwT[:, hb * 1024 + blk * 512: hb * 1024 + (blk + 1) * 512], True, True, ["sele", "dwT"], [("ps", 4 + blk)])
                            COPY("act", wb[:, blk * 512:(blk + 1) * 512], ps[4 + blk], [("ps", 4 + blk)], ["wb"])
                    for blk in range(2):
                        for dc in range(16):
                            MM(ps[blk], wgb[s_][:, dc, :], x1T[:, dc, blk * 512:(blk + 1) * 512], dc == 0, dc == 15, [("wgb", s_), "x1T"], [("ps", blk)])
                        for dc in range(16):
                            MM(ps[2 + blk], wub[s_][:, dc, :], x1T[:, dc, blk * 512:(blk + 1) * 512], dc == 0, dc == 15, [("wub", s_), "x1T"], [("ps", 2 + blk)])
                        O("act", "activation", [("ps", blk)], [("sgt", blk)], out=sgt[blk], in_=ps[blk], func=AF.Silu)
                        O("dve", "tensor_tensor", [("ps", 2 + blk), "wb"], [("tt_", blk)], out=tt_[blk], in0=ps[2 + blk], in1=wb[:, blk * 512:(blk + 1) * 512], op=ALU.mult)
                        O("pool", "tensor_tensor", [("sgt", blk), ("tt_", blk)], ["hTm"], out=hTm[:, fc, blk * 512:(blk + 1) * 512], in0=sgt[blk], in1=tt_[blk], op=ALU.mult)
                chunks.append((load, cast, compute))
            for dbk in range(4):
                s_ = di % 2; di += 1

                def load(e=e, dbk=dbk, s_=s_):
                    DM("wdst%d" % s_, wdst[s_], wd[e, :, dbk * 512:(dbk + 1) * 512].rearrange("(c p) d -> p c d", p=128), [], [("wdst", s_)])

                def cast(dbk=dbk, s_=s_):
                    COPY("act" if dbk % 2 == 0 else "dve", wdb[s_], wdst[s_], [("wdst", s_)], [("wdb", s_)])

                def compute(e=e, dbk=dbk, s_=s_):
                    for t8 in range(8):
                        bank = 4 + cntd["i"] % 4; cntd["i"] += 1
                        for fc in range(4):
                            MM(ps[bank], hTm[:, fc, t8 * 128:(t8 + 1) * 128], wdb[s_][:, fc, :], fc == 0, fc == 3, ["hTm", ("wdb", s_)], [("ps", bank)])
                        ysl = yacc[:, t8, dbk * 512:(dbk + 1) * 512]
                        if e == 0:
                            COPY("dve", ysl, ps[bank], [("ps", bank)], [("yacc", t8)])
                        else:
                            O("dve", "tensor_tensor", [("ps", bank), ("yacc", t8)], [("yacc", t8)], out=ysl, in0=ps[bank], in1=ysl, op=ALU.add)
                chunks.append((load, cast, compute))
        chunks[0][0](); chunks[0][1]()
        for k in range(len(chunks)):
            if k + 1 < len(chunks):
                chunks[k + 1][0](); chunks[k + 1][1]()
            chunks[k][2]()
        S.barrier()
        DM("c11", g2, ln2_g.partition_broadcast(128), [], ["lnp"]); DM("c12", be2, ln2_b.partition_broadcast(128), [], ["lnp"])
        for t8 in range(8):
            m = hb * 8 + t8
            p2 = t8 % 2
            DM("x1r%d" % p2, x1r[p2], x1_s[m * 128:(m + 1) * 128, :], [("x1_s", m)], [("x1r", p2)])
            O("dve", "scalar_tensor_tensor", [("x1r", p2), ("yacc", t8)], [("yacc", t8)], out=yacc[:, t8, :], in0=x1r[p2], scalar=DN_ALPHA, in1=yacc[:, t8, :], op0=ALU.mult, op1=ALU.add)
            layernorm(yacc[:, t8, :], g2, be2, outt[p2], sm, ("yacc", t8), ("outt", p2), ["lnp"], tag=p2)
            DM("outst%d" % p2, out[m * 128:(m + 1) * 128, :], outt[p2], [("outt", p2)], [("out", m)])
        S.barrier()
    S.emit()
    return nc


def _constants(r):
    bf = ml_dtypes.bfloat16
    c = {}
    inv_freq = (np.float32(500000.0) ** (-np.arange(0, 32, 2, dtype=np.float32) / np.float32(32))).astype(np.float32)
    ang = (np.arange(T, dtype=np.float32)[:, None] * inv_freq[None, :]).astype(np.float32)
    cs, sn = np.cos(ang).astype(np.float32), np.sin(ang).astype(np.float32)
    C = np.concatenate([cs.T, cs.T], axis=0)
    Sg = np.concatenate([-sn.T, sn.T], axis=0)
    rope_k = np.stack([C, Sg], axis=0).astype(np.float32)
    own = (np.arange(16)[:, None] * 4 + r) * 128 + np.arange(128)[None, :]
    own = own.reshape(-1)
    c["rope_k"] = np.ascontiguousarray(rope_k)
    c["rope_q"] = np.ascontiguousarray(rope_k[:, :, own])
    c["c_ident"] = np.eye(128, dtype=np.float32).astype(bf)
    rsw = np.zeros((32, 32), np.float32)
    for m in range(32):
        rsw[(m + 16) % 32, m] = 1.0
    c["c_rsw"] = rsw.astype(bf)
    ki = np.arange(128)[:, None]; qi = np.arange(128)[None, :]
    dm = np.zeros((128, 4, 128), np.float32)
    for i in range(4):
        dm[:, i, :] = 1.0 if i < r else ((ki <= qi).astype(np.float32) if i == r else 0.0)
    c["c_dmask4"] = np.tile(dm, (1, 1, 4)).astype(bf)
    cm = np.zeros((128, 5, 128), np.float32)
    for i in range(5):
        cm[:, i, :] = (16 * ki + 31 <= 128 * (4 * i + r) + qi).astype(np.float32)
    c["c_cmask4"] = np.tile(cm, (1, 1, 4)).astype(bf)
    wm = np.zeros((128, 8, 128), np.float32)
    for j in range(8):
        d = r + 4 - j
        if d == 4:
            wm[:, j, :] = (ki > qi)
        elif 1 <= d <= 3:
            wm[:, j, :] = 1.0
        elif d == 0:
            wm[:, j, :] = (ki <= qi)
    c["c_wmask4"] = np.tile(wm, (1, 1, 4)).astype(bf)
    svm = np.zeros((128, 16, 128), np.float32); sfb = np.zeros((128, 16, 128), np.float32)
    s_idx = np.arange(128)[None, :]
    for m in range(16):
        qpos = (4 * m + r) * 128 + np.arange(128)[:, None]
        cur = qpos // 64
        valid = s_idx <= cur
        forced = (s_idx == 0) | (s_idx == cur) | (s_idx == cur - 1)
        svm[:, m, :] = (valid & ~forced)
        sfb[:, m, :] = np.where(valid & forced, 1e30, np.where(valid, 0.0, -1e30))
    c["c_selvm"] = svm; c["c_selfb"] = sfb
    ov = np.zeros((512, 129), np.float32)
    ci = np.arange(511)[:, None] * 16; sj = np.arange(128)[None, :] * 64
    ov[:511, :128] = ((ci < sj + 64) & (ci + 32 > sj))
    ov[:, 128] = 1.0
    c["c_ovl1"] = ov.astype(bf)
    E = (np.arange(T)[None, :] // 64 == np.arange(128)[:, None]).astype(np.float32)
    c["c_E"] = E.astype(bf)
    se = np.zeros((32, 32, 128), np.float32)
    for e in range(32):
        se[e, e, :] = 1.0
    c["c_sele"] = se.astype(bf)
    return c


_NC_CACHE = {}


def kernel(x, w_in, diff_lambda, diff_subln_g, cmp_pos, cmp_w1, cmp_b1, cmp_w2, cmp_b2, w_out,
           ln1_g, ln1_b, router_group, router_expert, expert_w_gate, expert_w_up, expert_w_down,
           ln2_g, ln2_b, _stop_after=None, _debug=False):
    f = lambda a: np.ascontiguousarray(np.asarray(a, dtype=np.float32))
    x = f(x)
    shared = dict(
        w_in=f(w_in)[0], w_out=f(w_out)[0], diff_lambda=f(diff_lambda)[0], diff_subln_g=f(diff_subln_g)[0],
        cmp_pos=f(cmp_pos)[0], cmp_w1=f(cmp_w1)[0], cmp_b1=f(cmp_b1)[0], cmp_w2=f(cmp_w2)[0], cmp_b2=f(cmp_b2)[0],
        ln1_g=f(ln1_g)[0], ln1_b=f(ln1_b)[0], ln2_g=f(ln2_g)[0], ln2_b=f(ln2_b)[0],
        router_group=f(router_group)[0], router_expert=f(router_expert)[0],
        expert_w_gate=f(expert_w_gate)[0], expert_w_up=f(expert_w_up)[0], expert_w_down=f(expert_w_down)[0])
    consts = [_constants(r) for r in range(4)]
    in_maps = []
    for c in range(8):
        b, r = c // 4, c % 4
        xbb = x[b]
        xo = np.ascontiguousarray(xbb.reshape(64, 128, D)[r::4].reshape(TO, D))
        d = dict(shared)
        if _stop_after in ("A1", "A2", "A", "B", "C0", "C", "E1"):
            for kk in ("expert_w_gate", "expert_w_up", "expert_w_down"):
                d.pop(kk)
        d.update(consts[r])
        d["xb"] = xbb
        d["xo"] = xo
        in_maps.append(d)
    key = (_stop_after, _debug)
    nc = build_nc(stop_after=_stop_after, debug=_debug)
    res = run_bass_kernel_spmd(nc, in_maps, core_ids=list(range(8)))
    if _debug:
        return res
    outp = np.zeros((2, T, D), np.float32)
    for c in range(8):
        b, r = c // 4, c % 4
        outp[b].reshape(64, 128, D)[r::4] = res.results[c]["out"].reshape(16, 128, D)
    return outp
```

```python
import math
import numpy as np
import ml_dtypes
import concourse.bass as bass
import concourse.mybir as mybir
from concourse.bass_utils import run_bass_kernel_spmd

F32 = mybir.dt.float32
BF16 = mybir.dt.bfloat16
ALU = mybir.AluOpType
AF = mybir.ActivationFunctionType
AX = mybir.AxisListType

D = 2048
T = 8192
TO = 2048
NQT = 16
SCALE = 128 ** -0.5
EPS = 1e-5
DN_ALPHA = 2.0 ** 0.25
LAMBDA_INIT = 0.8 - 0.6 * math.exp(0.0)
NEG_BIAS = -30000.0


class Sched:
    ENGS = ("pe", "act", "dve", "pool", "sp")

    def __init__(self, nc):
        self.nc = nc
        self.ops = []
        self.last_writer = {}
        self.readers = {}
        self.dma_sems = {}
        self.last_on_eng = {}

    def _add(self, eng, fn, reads, writes, dma_sem=None, ndma=0, extra_deps=()):
        idx = len(self.ops)
        writes = tuple(writes) + tuple(k for k in reads if isinstance(k, tuple) and k[0] == "ps" and k not in writes)
        deps = set(extra_deps)
        for k in reads:
            w = self.last_writer.get(k)
            if w is not None:
                deps.add(w)
        for k in writes:
            w = self.last_writer.get(k)
            if w is not None:
                deps.add(w)
            lastc = {}
            for rd in self.readers.get(k, ()):
                ro = self.ops[rd]
                if ro["dma_sem"] is not None:
                    deps.add(rd)
                else:
                    lastc[ro["eng"]] = max(lastc.get(ro["eng"], -1), rd)
            deps.update(lastc.values())
        deps.discard(idx)
        for k in writes:
            self.last_writer[k] = idx
            self.readers[k] = []
        for k in reads:
            self.readers.setdefault(k, []).append(idx)
        self.ops.append(dict(eng=eng, fn=fn, deps=deps, dma_sem=dma_sem, ndma=ndma, sig=False))
        if fn is not None:
            self.last_on_eng[eng if dma_sem is None else ("dma", dma_sem)] = idx
        return idx

    def op(self, eng, fn, reads=(), writes=()):
        return self._add(eng, fn, tuple(reads), tuple(writes))

    def dma(self, eng, sem_name, fns, reads=(), writes=()):
        if sem_name not in self.dma_sems:
            self.dma_sems[sem_name] = None
        return self._add(eng, fns, tuple(reads), tuple(writes), dma_sem=sem_name, ndma=len(fns))

    def barrier(self):
        lasts = list(self.last_on_eng.values())
        for e in self.ENGS:
            self._add(e, None, (), (), extra_deps=lasts)

    def emit(self):
        nc = self.nc
        for o in self.ops:
            for d in o["deps"]:
                do = self.ops[d]
                if do["dma_sem"] is None:
                    if not (do["eng"] == "pe" and o["eng"] == "pe" and o["dma_sem"] is None):
                        do["sig"] = True
        cnt = {e: 0 for e in self.ENGS}
        dcnt = {k: 0 for k in self.dma_sems}
        for o in self.ops:
            if o["dma_sem"] is not None:
                dcnt[o["dma_sem"]] += 16 * o["ndma"]
                o["ev"] = ("d", o["dma_sem"], dcnt[o["dma_sem"]])
            elif o["sig"]:
                cnt[o["eng"]] += 1
                o["ev"] = ("e", o["eng"], cnt[o["eng"]])
            else:
                o["ev"] = None
        import contextlib
        with contextlib.ExitStack() as st:
            esem = {e: st.enter_context(nc.semaphore("s_" + e)) for e in self.ENGS}
            dsem = {k: st.enter_context(nc.semaphore("d_" + k)) for k in self.dma_sems}
            block = st.enter_context(nc.Block())
            ops = self.ops

            def run(ename):
                def body(eng):
                    waited = {}
                    for o in ops:
                        if o["eng"] != ename:
                            continue
                        for d in sorted(o["deps"]):
                            do = ops[d]
                            ev = do["ev"]
                            if ev is None:
                                continue
                            if ev[0] == "e" and ev[1] == "pe" and ename == "pe" and o["dma_sem"] is None:
                                continue
                            key = (ev[0], ev[1])
                            if waited.get(key, 0) >= ev[2]:
                                continue
                            waited[key] = ev[2]
                            sem = esem[ev[1]] if ev[0] == "e" else dsem[ev[1]]
                            eng.wait_ge(sem, ev[2])
                        if o["fn"] is None:
                            if o["ev"] is not None:
                                eng.nop().then_inc(esem[ename], 1)
                            continue
                        if o["dma_sem"] is not None:
                            for f in o["fn"]:
                                f(eng).then_inc(dsem[o["dma_sem"]], 16)
                        else:
                            ins = o["fn"](eng)
                            if o["ev"] is not None:
                                ins.then_inc(esem[ename], 1)
                return body

            block.tensor(run("pe"))
            block.scalar(run("act"))
            block.vector(run("dve"))
            block.gpsimd(run("pool"))
            block.sync(run("sp"))


class Arena:
    def __init__(self, nc, nelem_bf16):
        self.ap = nc.alloc_sbuf_tensor("arena", [128, nelem_bf16], BF16).ap()
        self.n = nelem_bf16
        self.off = 0

    def reset(self):
        self.off = 0

    def get(self, shape, dtype, parts=128):
        n = 1
        for s in shape:
            n *= s
        sz = n * (2 if dtype == F32 else 1)
        sz = (sz + 15) // 16 * 16
        assert self.off + sz <= self.n, ("arena overflow", self.off, sz, self.n)
        v = self.ap[0:parts, self.off:self.off + sz]
        self.off += sz
        if dtype == F32:
            v = v.bitcast(F32)
        v = v[:, 0:n]
        if len(shape) == 2:
            v = v.rearrange("p (a b) -> p a b", b=shape[1])
        elif len(shape) == 3:
            v = v.rearrange("p (a b c) -> p a b c", b=shape[1], c=shape[2])
        return v


C_DQ, C_DK, C_DV, C_NQ, C_KC, C_VC, C_KS, C_VS, C_KW, C_VW, C_NG = (
    0, 1024, 2048, 3072, 4096, 4352, 4608, 4864, 5120, 5376, 5632)


def build_nc(stop_after=None, debug=False):
    nc = bass.Bass("TRN2", target_bir_lowering=False)
    S = Sched(nc)

    def din(name, shape, dt=F32):
        return nc.dram_tensor(name, list(shape), dt, kind="ExternalInput").ap()

    xb = din("xb", [T, D]); xo = din("xo", [TO, D])
    w_in = din("w_in", [D, 5656]); w_out = din("w_out", [D, D])
    diff_lambda = din("diff_lambda", [4, 128]); subln_g = din("diff_subln_g", [256])
    cmp_pos = din("cmp_pos", [2, 32, 128]); cmp_w1 = din("cmp_w1", [2, 4096, 256])
    cmp_b1 = din("cmp_b1", [2, 256]); cmp_w2 = din("cmp_w2", [2, 256, 128]); cmp_b2 = din("cmp_b2", [2, 128])
    ln1_g = din("ln1_g", [D]); ln1_b = din("ln1_b", [D]); ln2_g = din("ln2_g", [D]); ln2_b = din("ln2_b", [D])
    r_group = din("router_group", [D, 4]); r_expert = din("router_expert", [D, 32])
    lite = stop_after in ("A1", "A2", "A", "B", "C0", "C", "E1")
    if not lite:
        wg = din("expert_w_gate", [32, D, 512]); wu = din("expert_w_up", [32, D, 512]); wd = din("expert_w_down", [32, 512, D])
    rope_k = din("rope_k", [2, 32, T]); rope_q = din("rope_q", [2, 32, TO])
    c_ident = din("c_ident", [128, 128], BF16); c_rsw = din("c_rsw", [32, 32], BF16)
    c_dmask4 = din("c_dmask4", [128, 4, 512], BF16); c_cmask4 = din("c_cmask4", [128, 5, 512], BF16)
    c_wmask4 = din("c_wbias4", [128, 8, 512], BF16)
    c_selvm = din("c_selvm", [128, 16, 128]); c_selfb = din("c_selfb", [128, 16, 128])
    c_ovl1 = din("c_ovl1", [512, 129], BF16); c_E = din("c_E", [128, T], BF16)
    c_sele = din("c_sele", [32, 32, 128], BF16)
    out = nc.dram_tensor("out", [TO, D], F32, kind="ExternalOutput").ap()
    skind = "ExternalOutput" if debug else "Internal"
    xT_s = nc.dram_tensor("xT_s", [16, 128, 16 * 512], BF16).ap()
    KT_s = nc.dram_tensor("KT_s", [16, 128, T], BF16, kind=skind).ap()
    V_s = nc.dram_tensor("V_s", [T, 1536], BF16, kind=skind).ap()
    QT_s = nc.dram_tensor("QT_s", [24, 128, TO], BF16, kind=skind).ap()
    oT_s = nc.dram_tensor("oT_s", [16, 128, TO], BF16, kind=skind).ap()
    x1_s = nc.dram_tensor("x1_s", [TO, D], F32, kind=skind).ap()
    x1T_s = nc.dram_tensor("x1T_s", [16, 128, TO], BF16).ap()

    def sb(name, shape, dt):
        return nc.alloc_sbuf_tensor(name, list(shape), dt).ap()

    ident = sb("ident", [128, 128], BF16); rsw = sb("rsw", [32, 32], BF16)
    gates = sb("gates", [128, 16, 24], F32)
    ones_bf = sb("ones_bf", [128, 128], BF16)
    kcT = [sb("kcT%d" % g, [128, 512], BF16) for g in range(2)]
    VCB = [sb("VCB%d" % g, [128, 4, 128], BF16) for g in range(2)]
    dwT = sb("dwT", [32, TO], BF16)
    A = Arena(nc, 92 * 1024)
    ps = [nc.alloc_psum_tensor("ps%d" % i, [128, 512], F32).ap() for i in range(8)]
    psb = [p.bitcast(BF16) for p in ps]

    def O(eng, method, reads, writes, *a, **kw):
        return S.op(eng, lambda e: getattr(e, method)(*a, **kw), reads, writes)

    def DM(sem, out_, in_, reads, writes, eng="sp", **kw):
        return S.dma(eng, sem, [lambda e: e.dma_start(out=out_, in_=in_, **kw)], reads, writes)

    def DMS(sem, out_, in_, nsplit, reads, writes):
        nt = out_.shape[1]
        step = nt // nsplit
        fns = [(lambda e, i=i: e.dma_start(out=out_[:, i * step:(i + 1) * step, :], in_=in_[:, i * step:(i + 1) * step, :])) for i in range(nsplit)]
        return S.dma("sp", sem, fns, reads, writes)

    def MM(out_, lhsT, rhs, start, stop, reads, writes):
        return S.op("pe", lambda e: e.matmul(out_, lhsT=lhsT, rhs=rhs, start=start, stop=stop, skip_group_check=True), reads, writes)

    def ps3(bank):
        return ps[bank].rearrange("p (h k) -> p h k", k=128)

    def TR(out_, in_, reads, writes):
        return S.op("pe", lambda e: e.transpose(out=out_, in_=in_, identity=ident), list(reads) + ["ident"], writes)

    rr = {"c": 0}

    def cast_eng(choices=("pool", "dve", "act")):
        rr["c"] += 1
        return choices[rr["c"] % len(choices)]

    def COPY(eng, out_, in_, reads, writes):
        if eng == "act":
            return O("act", "activation", reads, writes, out=out_, in_=in_, func=AF.Copy)
        return O(eng, "tensor_copy", reads, writes, out=out_, in_=in_)

    DM("c0", ident, c_ident, [], ["ident"])
    DM("c1", rsw, c_rsw, [], ["rsw"])
    O("pool", "memset", [], ["ones_bf"], ones_bf, 1.0)

    def load_weight_cols(Wsb, col_ranges, wkey, nm):
        ncols = sum(w for _, w in col_ranges)
        stg = [A.get([ncols], F32) for _ in range(2)]
        for dc in range(16):
            s = dc % 2
            fns = []
            off = 0
            for (c0, w) in col_ranges:
                fns.append(lambda e, c0=c0, w=w, off=off, s=s, dc=dc: e.dma_start(
                    out=stg[s][:, off:off + w], in_=w_in[dc * 128:(dc + 1) * 128, c0:c0 + w]))
                off += w
            S.dma("sp", "wst%s%d" % (nm, s), fns, [], [("wstg", nm, s)])
            COPY(cast_eng(), Wsb[:, dc, :], stg[s], [("wstg", nm, s)], [wkey])

    def x_tile_to_xT(xsrc, tc, t, xT, xTkey, bufs, nm):
        xst, xbf = bufs
        s = (tc * 4 + t) % 2
        DM("xst%s%d" % (nm, s), xst[s], xsrc[tc * 512 + t * 128: tc * 512 + (t + 1) * 128, :], [], [("xst", s)])
        COPY(cast_eng(("dve", "act")), xbf[s], xst[s], [("xst", s)], [("xbf", s)])
        for hb in range(2):
            bank = hb
            for j in range(8):
                dc = hb * 8 + j
                TR(psb[bank][:, j * 128:(j + 1) * 128], xbf[s][:, dc * 128:(dc + 1) * 128], [("xbf", s)], [("ps", bank)])
            COPY(cast_eng(("act", "dve")), xT[:, hb * 8:(hb + 1) * 8, t * 128:(t + 1) * 128],
                 psb[bank].rearrange("p (j k) -> p j k", k=128), [("ps", bank)], [xTkey])

    def rope_fix(dst32, psacc32, ropet, s, keys_r, keys_w, tmpA, tmpB):
        MM(ps[6][0:32, :], rsw[:, :], dst32, True, True, ["rsw"] + keys_w, [("ps", 6)])
        O("dve", "tensor_tensor", [("ps", 6), ("ropet", s)], ["tmpA"], out=tmpA, in0=ps[6][0:32, :], in1=ropet[:, 1, :], op=ALU.mult)
        O("dve", "tensor_tensor", keys_r + [("ropet", s)], ["tmpB"], out=tmpB, in0=psacc32, in1=ropet[:, 0, :], op=ALU.mult)
        O("dve", "tensor_tensor", ["tmpA", "tmpB"], keys_w, out=dst32, in0=tmpA, in1=tmpB, op=ALU.add)

    A.reset()
    Wk = A.get([16, 2048], BF16)
    load_weight_cols(Wk, [(C_DK, 1024), (C_KC, 256), (C_KS, 256), (C_KW, 256), (C_VC, 256)], "Wk", "k")
    xst = [A.get([2048], F32) for _ in range(2)]
    xbf = [A.get([2048], BF16) for _ in range(2)]
    xTs = [A.get([16, 512], BF16) for _ in range(2)]
    Kst = A.get([16, 512], BF16)
    ropet = [A.get([2, 512], F32, parts=32) for _ in range(2)]
    tmpA = A.get([512], F32, parts=32); tmpB = A.get([512], F32, parts=32)
    rope_tiles_k = set(range(0, 8)) | {10, 11, 12, 13}
    for tc in range(16):
        s = tc % 2
        xT = xTs[s]
        if tc == 0:
            for t in range(4):
                x_tile_to_xT(xb, 0, t, xT, ("xT", s), (xst, xbf), "a")
        DM("xTst%d" % s, xT_s[tc].rearrange("p (c t) -> p c t", t=512), xT, [("xT", s)], [("xT_s", tc)])
        DM("rope%d" % s, ropet[s], rope_k[:, :, tc * 512:(tc + 1) * 512].rearrange("a p t -> p a t"), [], [("ropet", s)])
        pend = None
        for ct in range(16):
            bank = 2 + ct % 4
            for dc in range(16):
                MM(ps[bank], Wk[:, dc, ct * 128:(ct + 1) * 128], xT[:, dc, :], dc == 0, dc == 15, ["Wk", ("xT", s)], [("ps", bank)])
            COPY(cast_eng(("act", "dve")), Kst[:, ct, :], ps[bank], [("ps", bank)], [("Kst", ct)])
            if pend is not None:
                pend(); pend = None
            if ct in rope_tiles_k:
                pend = (lambda ct=ct, bank=bank, s=s: rope_fix(Kst[0:32, ct, :], ps[bank][0:32, :], ropet[s], s, [("ps", bank)], [("Kst", ct)], tmpA, tmpB))
            if ct in (2, 5, 8, 11) and tc + 1 < 16:
                x_tile_to_xT(xb, tc + 1, (ct - 2) // 3, xTs[1 - s], ("xT", 1 - s), (xst, xbf), "a")
        if pend is not None:
            pend(); pend = None
        DM("Kstst", KT_s[:, :, tc * 512:(tc + 1) * 512].rearrange("i p t -> p i t"), Kst,
           [("Kst", ct) for ct in range(16)], [("KT_s", tc)])
    S.barrier()
    if stop_after == "A1":
        S.emit(); return nc

    A.reset()
    Wv = A.get([16, 1536], BF16)
    load_weight_cols(Wv, [(C_DV, 1024), (C_VS, 256), (C_VW, 256)], "Wv", "v")
    xTs = [A.get([16, 512], BF16) for _ in range(2)]
    Vst = [A.get([4, 1536], BF16) for _ in range(2)]
    for tc in range(16):
        s = tc % 2
        xT = xTs[s]
        if tc == 0:
            DM("xTld%d" % s, xT, xT_s[tc].rearrange("p (c t) -> p c t", t=512), [("xT_s", tc)], [("xT", s)])
        if tc + 1 < 16:
            DM("xTld%d" % (1 - s), xTs[1 - s], xT_s[tc + 1].rearrange("p (c t) -> p c t", t=512), [("xT_s", tc + 1)], [("xT", 1 - s)])
        for t in range(4):
            for nb in range(3):
                bank = 2 + (t * 3 + nb) % 4
                for dc in range(16):
                    MM(ps[bank], xT[:, dc, t * 128:(t + 1) * 128], Wv[:, dc, nb * 512:(nb + 1) * 512], dc == 0, dc == 15,
                       ["Wv", ("xT", s)], [("ps", bank)])
                COPY(cast_eng(("act", "dve")), Vst[s][:, t, nb * 512:(nb + 1) * 512], ps[bank], [("ps", bank)], [("Vst", s)])
        DM("Vstst%d" % s, V_s[tc * 512:(tc + 1) * 512, :].rearrange("(t p) c -> p t c", p=128), Vst[s], [("Vst", s)], [("V_s", tc)])
    S.barrier()
    if stop_after == "A2":
        S.emit(); return nc

    A.reset()
    Wq = A.get([16, 2048], BF16)
    load_weight_cols(Wq, [(C_DQ, 1024), (C_NQ, 1024)], "Wq", "q")
    Wgt = A.get([16, 24], BF16)
    load_weight_cols(Wgt, [(C_NG, 24)], "Wgt", "g")
    xst = [A.get([2048], F32) for _ in range(2)]
    xbf = [A.get([2048], BF16) for _ in range(2)]
    xTs = [A.get([16, 512], BF16) for _ in range(2)]
    Qst = A.get([24, 512], BF16)
    ropet = [A.get([2, 512], F32, parts=32) for _ in range(2)]
    tmpA = A.get([512], F32, parts=32); tmpB = A.get([512], F32, parts=32)
    for oc in range(4):
        s = oc % 2
        xT = xTs[s]
        if oc == 0:
            for t in range(4):
                x_tile_to_xT(xo, 0, t, xT, ("xT", s), (xst, xbf), "q")
        DM("rope%d" % s, ropet[s], rope_q[:, :, oc * 512:(oc + 1) * 512].rearrange("a p t -> p a t"), [], [("ropet", s)])
        pend = None
        for ct in range(16):
            bank = 2 + ct % 4
            for dc in range(16):
                MM(ps[bank], Wq[:, dc, ct * 128:(ct + 1) * 128], xT[:, dc, :], dc == 0, dc == 15, ["Wq", ("xT", s)], [("ps", bank)])
            if ct < 8:
                COPY(cast_eng(("act", "dve")), Qst[:, ct, :], ps[bank], [("ps", bank)], [("Qst", ct)])
                dstt = ct
            else:
                COPY("act", Qst[:, ct, :], ps[bank], [("ps", bank)], [("Qst", ct)])
                COPY("dve", Qst[:, ct + 8, :], ps[bank], [("ps", bank)], [("Qst", ct + 8)])
                dstt = ct + 8
            if pend is not None:
                pend(); pend = None
            pend = (lambda dstt=dstt, bank=bank, s=s: rope_fix(Qst[0:32, dstt, :], ps[bank][0:32, :], ropet[s], s, [("ps", bank)], [("Qst", dstt)], tmpA, tmpB))
            if ct in (2, 5, 8, 11) and oc + 1 < 4:
                x_tile_to_xT(xo, oc + 1, (ct - 2) // 3, xTs[1 - s], ("xT", 1 - s), (xst, xbf), "q")
        if pend is not None:
            pend(); pend = None
        for t in range(4):
            for dc in range(16):
                MM(ps[7][:, 0:24], xT[:, dc, t * 128:(t + 1) * 128], Wgt[:, dc, :], dc == 0, dc == 15, ["Wgt", ("xT", s)], [("ps", 7)])
            O("act", "activation", [("ps", 7)], ["gates"], out=gates[:, oc * 4 + t, :], in_=ps[7][:, 0:24], func=AF.Sigmoid)
        DM("Qstst", QT_s[:, :, oc * 512:(oc + 1) * 512].rearrange("i p t -> p i t"), Qst,
           [("Qst", ct) for ct in range(24)], [("QT_s", oc)])
    S.barrier()
    if stop_after == "A":
        S.emit(); return nc

    A.reset()
    KT2 = [A.get([2, T], BF16) for _ in range(2)]
    Vh = [A.get([64, 257], BF16) for _ in range(2)]
    QT2 = [A.get([2, TO], BF16) for _ in range(2)]
    oTh = [A.get([2, TO], BF16) for _ in range(2)]
    PT = [A.get([512], BF16) for _ in range(3)]
    dmask4 = A.get([4, 512], BF16)
    lamb = A.get([512], F32); lt = A.get([256], F32); g08 = A.get([256], F32)
    sm = A.get([16], F32)
    od = A.get([256], F32); junk = A.get([256], F32); obf = A.get([256], BF16)
    DM("c2", dmask4, c_dmask4, [], ["dmask4"])
    DM("c3", lamb, diff_lambda.rearrange("a d -> (a d)").partition_broadcast(128), [], ["lamb"])
    DM("c4", g08, subln_g.partition_broadcast(128), [], ["g08"])
    O("dve", "tensor_scalar", ["g08"], ["g08"], out=g08, in0=g08, scalar1=1.0 - LAMBDA_INIT, scalar2=None, op0=ALU.mult)
    for i in range(2):
        O("dve", "tensor_tensor", ["lamb"], ["lt"], out=lt[:, 0:128], in0=lamb[:, i * 256:i * 256 + 128], in1=lamb[:, i * 256 + 128:i * 256 + 256], op=ALU.mult)
        O("dve", "reduce_sum", ["lt"], [("sm", i)], out=sm[:, i:i + 1], in_=lt[:, 0:128], axis=AX.X)
        O("act", "activation", [("sm", i)], [("sm", i)], out=sm[:, i:i + 1], in_=sm[:, i:i + 1], func=AF.Exp)
    O("dve", "tensor_tensor", [("sm", 0), ("sm", 1)], ["nlam"], out=sm[:, 2:3], in0=sm[:, 1:2], in1=sm[:, 0:1], op=ALU.subtract)
    O("dve", "tensor_scalar", ["nlam"], ["nlam"], out=sm[:, 2:3], in0=sm[:, 2:3], scalar1=-LAMBDA_INIT, scalar2=None, op0=ALU.add)
    for s in range(2):
        O("pool", "memset", [], [("Vh1", s)], Vh[s][:, :, 256:257], 1.0)
    O("dve", "memset", [], ["epsb"], sm[:, 8:9], EPS)
    steps = []
    cnt = {"pt": 0, "sb": 0}
    def mk_pre_head(h):
        s = h % 2

        def pre_head():
            DM("kt2_%d" % s, KT2[s], KT_s[2 * h:2 * h + 2].rearrange("i p t -> p i t"), [("KT_s", tc) for tc in range(16)], [("KT2", s)])
            DMS("vh_%d" % s, Vh[s][:, :, 0:256], V_s[:, 256 * h:256 * h + 256].rearrange("(t p) c -> p t c", p=128), 8,
                [("V_s", tc) for tc in range(16)], [("Vh", s)])
            DM("qt2_%d" % s, QT2[s], QT_s[2 * h:2 * h + 2].rearrange("i p t -> p i t"), [("QT_s", oc) for oc in range(4)], [("QT2", s)])
        return pre_head

    for h in range(4):
        s = h % 2
        pre_head = mk_pre_head(h)
        nxt_pre = mk_pre_head(h + 1) if h + 1 < 4 else None

        def _unused(h=h, s=s):
            DM("kt2_%d" % s, KT2[s], KT_s[2 * h:2 * h + 2].rearrange("i p t -> p i t"), [("KT_s", tc) for tc in range(16)], [("KT2", s)])
            DMS("vh_%d" % s, Vh[s][:, :, 0:256], V_s[:, 256 * h:256 * h + 256].rearrange("(t p) c -> p t c", p=128), 8,
                [("V_s", tc) for tc in range(16)], [("Vh", s)])
            DM("qt2_%d" % s, QT2[s], QT_s[2 * h:2 * h + 2].rearrange("i p t -> p i t"), [("QT_s", oc) for oc in range(4)], [("QT2", s)])

        for m in range(NQT):
            ob = (m % 2) * 2
            nkt = 4 * m + 4
            for kp in range(nkt // 2):
                sbank = 4 + cnt["sb"] % 2; cnt["sb"] += 1
                pti = cnt["pt"] % 3; cnt["pt"] += 1
                pt = PT[pti]; ptk = ("PT", pti)
                st = {}
                if h == 0 and m == 0 and kp == 0:
                    st["pre"] = pre_head
                if m == 8 and kp == 0 and nxt_pre is not None:
                    st["pre"] = nxt_pre

                def qk(s=s, m=m, kp=kp, sbank=sbank):
                    for j in range(2):
                        kt = 2 * kp + j
                        for c in range(2):
                            MM(ps[sbank][:, (j * 2 + c) * 128:(j * 2 + c + 1) * 128], KT2[s][:, c, kt * 128:(kt + 1) * 128],
                               QT2[s][:, c, m * 128:(m + 1) * 128], True, True, [("KT2", s), ("QT2", s)], [("ps", sbank)])

                def em(m=m, kp=kp, sbank=sbank, pt=pt, ptk=ptk):
                    O("act", "activation", [("ps", sbank)], [ptk], out=pt, in_=ps[sbank], func=AF.Exp, scale=SCALE)
                    for j in range(2):
                        kt = 2 * kp + j
                        if kt >= 4 * m:
                            i = kt - 4 * m
                            O("dve", "tensor_tensor", [ptk, "dmask4"], [ptk], out=pt[:, j * 256:(j + 1) * 256],
                              in0=pt[:, j * 256:(j + 1) * 256], in1=dmask4[:, i, 0:256], op=ALU.mult)

                def pv(s=s, m=m, kp=kp, pt=pt, ptk=ptk, ob=ob, nkt=nkt):
                    for j in range(2):
                        kt = 2 * kp + j
                        for c in range(2):
                            MM(ps[ob + c][:, 0:257], pt[:, (j * 2 + c) * 128:(j * 2 + c + 1) * 128], Vh[s][:, kt, :], kt == 0, kt == nkt - 1,
                               [ptk, ("Vh", s), ("Vh1", s)], [("ps", ob + c)])

                st["qk"] = qk; st["em"] = em; st["pv"] = pv
                if kp == nkt // 2 - 1:
                    def post(h=h, s=s, m=m, ob=ob):
                        O("dve", "reciprocal", [("ps", ob)], ["rl0"], out=sm[:, 4:5], in_=ps[ob][:, 256:257])
                        O("dve", "reciprocal", [("ps", ob + 1)], ["rl1"], out=sm[:, 5:6], in_=ps[ob + 1][:, 256:257])
                        O("dve", "tensor_tensor", ["rl1", "nlam"], ["rl1"], out=sm[:, 5:6], in0=sm[:, 5:6], in1=sm[:, 2:3], op=ALU.mult)
                        O("dve", "tensor_scalar", [("ps", ob), "rl0"], ["od"], out=od, in0=ps[ob][:, 0:256], scalar1=sm[:, 4:5], scalar2=None, op0=ALU.mult)
                        O("dve", "scalar_tensor_tensor", [("ps", ob + 1), "rl1", "od"], ["od"], out=od, in0=ps[ob + 1][:, 0:256], scalar=sm[:, 5:6], in1=od,
                          op0=ALU.mult, op1=ALU.add)
                        O("act", "activation", ["od"], ["junk", "ss"], out=junk, in_=od, func=AF.Square, accum_out=sm[:, 6:7])
                        O("act", "activation", ["ss", "epsb"], ["ss"], out=sm[:, 6:7], in_=sm[:, 6:7], func=AF.Ln, scale=1.0 / 256.0, bias=sm[:, 8:9])
                        O("act", "activation", ["ss"], ["ss"], out=sm[:, 6:7], in_=sm[:, 6:7], func=AF.Exp, scale=-0.5)
                        O("dve", "scalar_tensor_tensor", ["od", "ss", "g08"], ["obf"], out=obf, in0=od, scalar=sm[:, 6:7], in1=g08, op0=ALU.mult, op1=ALU.mult)

                    def post_pe(h=h, s=s, m=m):
                        for c in range(2):
                            TR(psb[6][:, c * 128:(c + 1) * 128], obf[:, c * 128:(c + 1) * 128], ["obf"], [("ps", 6)])
                        COPY("act", oTh[s][:, :, m * 128:(m + 1) * 128], psb[6][:, 0:256].rearrange("p (c k) -> p c k", k=128), [("ps", 6)], [("oTh", s)])
                        if m == NQT - 1:
                            DM("oTst%d" % s, oT_s[2 * h:2 * h + 2].rearrange("i p t -> p i t"), oTh[s], [("oTh", s)], [("oT_s", "d", h)])
                    st["post"] = post; st["post_pe"] = post_pe
                steps.append(st)

    def run_pipeline(steps, defer=2):
        pending = []
        n = len(steps)

        def finish(i):
            nonlocal pending
            steps[i]["pv"]()
            pending = [(c - 1, f) for (c, f) in pending]
            for (c, f) in [p for p in pending if p[0] <= 0]:
                f()
            pending = [p for p in pending if p[0] > 0]
            if "post" in steps[i]:
                steps[i]["post"]()
            if "post_pe" in steps[i]:
                pending.append((defer, steps[i]["post_pe"]))

        if "pre" in steps[0]:
            steps[0]["pre"]()
        steps[0]["qk"]()
        for i in range(n):
            steps[i]["em"]()
            if i + 1 < n:
                if "pre" in steps[i + 1]:
                    steps[i + 1]["pre"]()
                steps[i + 1]["qk"]()
            if i >= 1:
                finish(i - 1)
        finish(n - 1)
        for (c, f) in pending:
            f()

    run_pipeline(steps)
    S.barrier()
    if stop_after == "B":
        S.emit(); return nc

    A.reset()
    XcT = A.get([T], BF16)
    W1 = A.get([32, 256], BF16)
    w1st = [A.get([8, 256], F32) for _ in range(2)]
    posf = A.get([128], F32, parts=32); posb = A.get([128], BF16, parts=32); posT = A.get([32], BF16)
    W2f = A.get([2, 128], F32); W2 = A.get([2, 128], BF16)
    b1 = A.get([2], F32); b2c = A.get([1], F32)
    b2rf = A.get([128], F32, parts=1); b2r = A.get([128], BF16, parts=1)
    hT = A.get([2, 512], BF16)
    u = A.get([512], F32); u2 = A.get([512], F32); sg = A.get([512], F32)
    O("pool", "memset", [], ["hT"], hT, 0.0)
    for g in range(2):
        O("pool", "memset", [], [("kcT", g)], kcT[g], 0.0)
    for j in range(2):
        DM("cp0", posf, cmp_pos[j], [], ["posf"])
        COPY("dve", posb, posf, ["posf"], ["posb"])
        S.op("pe", lambda e: e.transpose(out=psb[2][:, 0:32], in_=posb, identity=ident[0:32, 0:32]), ["posb", "ident"], [("ps", 2)])
        COPY("dve", posT, psb[2][:, 0:32], [("ps", 2)], ["posT"])
        DM("cp1", W2f, cmp_w2[j].rearrange("(c p) d -> p c d", p=128), [], ["W2f"])
        COPY("dve", W2, W2f, ["W2f"], ["W2"])
        DM("cp2", b1, cmp_b1[j].rearrange("(c p) -> p c", p=128), [], ["b1"], allow_slow_non_contiguous=True)
        if j == 0:
            DM("cp3", b2c, cmp_b2[0].rearrange("(p o) -> p o", o=1), [], ["b2c"], allow_slow_non_contiguous=True)
        else:
            DM("cp3", b2rf, cmp_b2[1].rearrange("(o d) -> o d", o=1), [], ["b2rf"])
            COPY("dve", b2r, b2rf, ["b2rf"], ["b2r"])
        for q4 in range(4):
            s = q4 % 2
            DM("w1st%d" % s, w1st[s], cmp_w1[j, q4 * 1024:(q4 + 1) * 1024, :].rearrange("(l d) h -> d l h", d=128), [], [("w1st", s)])
            COPY(cast_eng(("pool", "dve")), W1[:, q4 * 8:(q4 + 1) * 8, :], w1st[s], [("w1st", s)], ["W1"])
        for hc in range(2):
            for l in range(32):
                MM(ps[2][:, hc:hc + 1], W1[:, l, hc * 128:(hc + 1) * 128], posT[:, l:l + 1], l == 0, l == 31, ["W1", "posT"], [("ps", 2)])
        O("dve", "tensor_tensor", [("ps", 2), "b1"], ["b1"], out=b1, in0=ps[2][:, 0:2], in1=b1, op=ALU.add)
        for g in range(2):
            DM("xct", XcT, KT_s[(8 if j == 0 else 14) + g], [("KT_s", tc) for tc in range(16)], ["XcT"])
            for hc in range(2):
                for l in range(32):
                    MM(ps[hc][:, 0:511], W1[:, l, hc * 128:(hc + 1) * 128], XcT[:, l:l + 16 * 510 + 1:16], l == 0, l == 31, ["W1", "XcT"], [("ps", hc)])
                O("act", "activation", [("ps", hc), "b1"], ["u"], out=u[:, 0:511], in_=ps[hc][:, 0:511], func=AF.Identity, bias=b1[:, hc:hc + 1])
                O("dve", "tensor_tensor", ["u"], ["u2"], out=u2[:, 0:511], in0=u[:, 0:511], in1=u[:, 0:511], op=ALU.mult)
                O("dve", "tensor_scalar", ["u2"], ["u2"], out=u2[:, 0:511], in0=u2[:, 0:511], scalar1=0.044715, scalar2=1.0, op0=ALU.mult, op1=ALU.add)
                O("dve", "tensor_tensor", ["u2", "u"], ["u2"], out=u2[:, 0:511], in0=u2[:, 0:511], in1=u[:, 0:511], op=ALU.mult)
                O("act", "activation", ["u2"], ["sg"], out=sg[:, 0:511], in_=u2[:, 0:511], func=AF.Sigmoid, scale=1.5957691216057308)
                O("dve", "tensor_tensor", ["u", "sg"], ["hT"], out=hT[:, hc, 0:511], in0=u[:, 0:511], in1=sg[:, 0:511], op=ALU.mult)
            if j == 0:
                for hc in range(2):
                    MM(ps[3][:, 0:511], W2[:, hc, :], hT[:, hc, 0:511], hc == 0, hc == 1, ["W2", "hT"], [("ps", 3)])
                O("act", "activation", [("ps", 3), "b2c"], [("kcT", g)], out=kcT[g][:, 0:511], in_=ps[3][:, 0:511], func=AF.Identity, bias=b2c[:, 0:1])
            else:
                for ct in range(4):
                    for hc in range(2):
                        MM(ps[3][:, ct * 128:(ct + 1) * 128], hT[:, hc, ct * 128:(ct + 1) * 128], W2[:, hc, :], hc == 0, False, ["W2", "hT"], [("ps", 3)])
                    MM(ps[3][:, ct * 128:(ct + 1) * 128], ones_bf[0:1, :], b2r[0:1, :], False, True, ["ones_bf", "b2r"], [("ps", 3)])
                COPY("act", VCB[g], ps[3].rearrange("p (c k) -> p c k", k=128), [("ps", 3)], [("VCB", g)])
    S.barrier()
    if stop_after == "C0":
        S.emit(); return nc

    A.reset()
    QTg = A.get([4, TO], BF16); QRg = A.get([4, TO], BF16)
    KsT = A.get([T], BF16); KwT = A.get([T], BF16)
    Vs1 = A.get([64, 129], BF16); Vw1 = A.get([64, 129], BF16)
    Eexp = A.get([T], BF16)
    oTg = A.get([4, TO], BF16)
    dmask4 = A.get([4, 512], BF16); cmask4 = A.get([5, 512], BF16); wmask4 = A.get([8, 512], BF16)
    selvm = A.get([16, 128], F32); selfb = A.get([16, 128], F32)
    VCA = A.get([4, 129], BF16)
    PT = [A.get([512], BF16) for _ in range(3)]
    selbT4 = A.get([4, 128], BF16)
    imp = A.get([128], F32); sc2 = A.get([128], F32); selb = A.get([128], BF16)
    mx = A.get([16], F32); sm = A.get([32], F32)
    onsa = A.get([4, 128], F32); onb = A.get([4, 128], BF16)
    DM("c2", dmask4, c_dmask4, [], ["dmask4"]); DM("c5", cmask4, c_cmask4, [], ["cmask4"]); DM("c6", wmask4, c_wmask4, [], ["wmask4"])
    DM("c7", selvm, c_selvm, [], ["selvm"]); DM("c8", selfb, c_selfb, [], ["selfb"])
    DM("c9", VCA, c_ovl1.rearrange("(t p) c -> p t c", p=128), [], ["VCA"])
    DM("c10", Eexp, c_E, [], ["Eexp"])
    O("pool", "memset", [], ["Vs1o"], Vs1[:, :, 128:129], 1.0)
    O("pool", "memset", [], ["Vw1o"], Vw1[:, :, 128:129], 1.0)
    RA = [(0, 0), (0, 129), (0, 258), (1, 0)]
    RB = [(1, 129), (1, 257), (2, 0), (2, 128)]
    RS = [(3, 0), (3, 129), (3, 258), (4, 0)]
    RW = [(4, 129), (4, 258), (5, 0), (5, 129)]
    steps = []
    cnt = {"pt": 0, "sb": 0}

    def nxt_bufs():
        sbank = 6 + cnt["sb"] % 2; cnt["sb"] += 1
        pti = cnt["pt"] % 3; cnt["pt"] += 1
        return sbank, PT[pti], ("PT", pti)

    for g in range(2):
        allK = [("KT_s", tc) for tc in range(16)]; allV = [("V_s", tc) for tc in range(16)]; allQ = [("QT_s", oc) for oc in range(4)]

        def pre_group(g=g, allK=allK, allV=allV, allQ=allQ):
            DM("n0", QTg, QT_s[8 + 4 * g:12 + 4 * g].rearrange("i p t -> p i t"), allQ, ["QTg"])
            DM("n1", QRg, QT_s[16 + 4 * g:20 + 4 * g].rearrange("i p t -> p i t"), allQ, ["QRg"])
            DM("n2", KsT, KT_s[10 + g], allK, ["KsT"]); DM("n3", KwT, KT_s[12 + g], allK, ["KwT"])
            DMS("n4", Vs1[:, :, 0:128], V_s[:, 1024 + 128 * g:1024 + 128 * g + 128].rearrange("(t p) c -> p t c", p=128), 8, allV, ["Vs1"])
            DMS("n5", Vw1[:, :, 0:128], V_s[:, 1280 + 128 * g:1280 + 128 * g + 128].rearrange("(t p) c -> p t c", p=128), 8, allV, ["Vw1"])

        for m in range(NQT):
            started = {}
            qsl = slice(m * 128, (m + 1) * 128)

            def ACC(reg, width, lhsT, rhs, last, reads, started=started):
                bank, off = reg
                st_ = bank not in started
                started[bank] = True
                MM(ps[bank][:, off:off + width], lhsT, rhs, st_, last, reads, [("ps", bank)])

            nct = m // 4 + 1
            for ct in range(nct):
                sbank, pt, ptk = nxt_bufs()
                st = {}
                if m == 0 and ct == 0:
                    st["pre"] = pre_group

                def qk(g=g, ct=ct, sbank=sbank, qsl=qsl):
                    MM(ps3(sbank), kcT[g][:, ct * 128:(ct + 1) * 128], QTg[:, :, qsl], True, True, [("kcT", g), "QTg"], [("ps", sbank)])

                def em(m=m, ct=ct, sbank=sbank, pt=pt, ptk=ptk):
                    O("act", "activation", [("ps", sbank)], [ptk], out=pt, in_=ps[sbank], func=AF.Exp, scale=SCALE)
                    i = m - 4 * ct
                    if i <= 4:
                        O("dve", "tensor_tensor", [ptk, "cmask4"], [ptk], out=pt, in0=pt, in1=cmask4[:, i, :], op=ALU.mult)

                def pv(g=g, ct=ct, nct=nct, pt=pt, ptk=ptk, ACC=ACC):
                    for hh in range(4):
                        ACC(RA[hh], 129, pt[:, hh * 128:(hh + 1) * 128], VCA[:, ct, :], ct == nct - 1, [ptk, "VCA"])
                        ACC(RB[hh], 128, pt[:, hh * 128:(hh + 1) * 128], VCB[g][:, ct, :], ct == nct - 1, [ptk, ("VCB", g)])

                st["qk"] = qk; st["em"] = em; st["pv"] = pv
                if ct == nct - 1:
                    def post(g=g, m=m):
                        for hh in range(4):
                            bk, off = RA[hh]
                            O("dve", "tensor_scalar", [("ps", bk)], [("rc", hh)], out=sm[:, hh:hh + 1], in0=ps[bk][:, off + 128:off + 129], scalar1=1e-30, scalar2=None, op0=ALU.max)
                            O("dve", "reciprocal", [("rc", hh)], [("rc", hh)], out=sm[:, hh:hh + 1], in_=sm[:, hh:hh + 1])
                            if hh == 0:
                                O("dve", "tensor_scalar", [("ps", bk), ("rc", hh)], ["imp"], out=imp, in0=ps[bk][:, off:off + 128], scalar1=sm[:, hh:hh + 1], scalar2=None, op0=ALU.mult)
                            else:
                                O("dve", "scalar_tensor_tensor", [("ps", bk), ("rc", hh), "imp"], ["imp"], out=imp, in0=ps[bk][:, off:off + 128], scalar=sm[:, hh:hh + 1], in1=imp,
                                  op0=ALU.mult, op1=ALU.add)
                        O("dve", "tensor_tensor", ["imp", "selvm"], ["imp"], out=imp, in0=imp, in1=selvm[:, m, :], op=ALU.mult)
                        O("dve", "tensor_tensor", ["imp", "selfb"], ["imp"], out=imp, in0=imp, in1=selfb[:, m, :], op=ALU.add)
                        O("dve", "max", ["imp"], ["mx"], out=mx[:, 0:8], in_=imp)
                        O("dve", "match_replace", ["imp", "mx"], ["sc2"], out=sc2, in_to_replace=mx[:, 0:8], in_values=imp, imm_value=-3.0e38)
                        O("dve", "max", ["sc2"], ["mx2"], out=mx[:, 8:16], in_=sc2)
                        O("dve", "tensor_scalar", ["imp", "mx2", "sc2"], ["sc2"], out=sc2, in0=imp, scalar1=mx[:, 15:16], scalar2=1.0, op0=ALU.is_ge, op1=ALU.subtract)
                        O("dve", "tensor_scalar", ["sc2"], ["selb"], out=selb, in0=sc2, scalar1=-NEG_BIAS, scalar2=None, op0=ALU.mult)
                        for hh in range(4):
                            bk, off = RB[hh]
                            gcol = (4 * g + hh) * 3
                            O("dve", "tensor_tensor", [("rc", hh), "gates"], [("f", hh)], out=sm[:, 8 + hh:9 + hh], in0=sm[:, hh:hh + 1], in1=gates[:, m, gcol:gcol + 1], op=ALU.mult)
                            O("dve", "tensor_scalar", [("ps", bk), ("f", hh)], [("onsa", hh)], out=onsa[:, hh, :], in0=ps[bk][:, off:off + 128], scalar1=sm[:, 8 + hh:9 + hh], scalar2=None, op0=ALU.mult)
                    st["post"] = post
                steps.append(st)
            jl = [j for j in range(8) if 4 * m - 4 + j >= 0]
            for j in jl:
                tt = 4 * m - 4 + j
                sbank, pt, ptk = nxt_bufs()
                st = {}

                def qk(tt=tt, j=j, sbank=sbank, qsl=qsl):
                    MM(ps3(sbank), KwT[:, tt * 128:(tt + 1) * 128], QRg[:, :, qsl], True, False, ["KwT", "QRg"], [("ps", sbank)])
                    MM(ps[sbank], ident, wmask4[:, j, :], False, True, ["ident", "wmask4"], [("ps", sbank)])

                def em(j=j, sbank=sbank, pt=pt, ptk=ptk):
                    O("act", "activation", [("ps", sbank)], [ptk], out=pt, in_=ps[sbank], func=AF.Exp, scale=SCALE)

                def pv(j=j, tt=tt, jl=jl, pt=pt, ptk=ptk, ACC=ACC):
                    for hh in range(4):
                        ACC(RW[hh], 129, pt[:, hh * 128:(hh + 1) * 128], Vw1[:, tt, :], j == jl[-1], [ptk, "Vw1", "Vw1o"])

                st["qk"] = qk; st["em"] = em; st["pv"] = pv
                steps.append(st)
            ntt = 4 * m + 4
            for tt in range(ntt):
                sbank, pt, ptk = nxt_bufs()
                st = {}
                if tt == 0:
                    def pre_sel():
                        sbank_t, _, _ = nxt_bufs()
                        TR(psb[sbank_t][:, 0:128], selb, ["selb"], [("ps", sbank_t)])
                        for hh in range(4):
                            COPY("act", selbT4[:, hh, :], psb[sbank_t][:, 0:128], [("ps", sbank_t)], ["selbT4"])
                    st["pre"] = pre_sel

                def qk(tt=tt, sbank=sbank, qsl=qsl):
                    MM(ps3(sbank), KsT[:, tt * 128:(tt + 1) * 128], QRg[:, :, qsl], True, False, ["KsT", "QRg"], [("ps", sbank)])
                    MM(ps3(sbank), Eexp[:, tt * 128:(tt + 1) * 128], selbT4, False, True, ["Eexp", "selbT4"], [("ps", sbank)])

                def em(m=m, tt=tt, sbank=sbank, pt=pt, ptk=ptk):
                    O("act", "activation", [("ps", sbank)], [ptk], out=pt, in_=ps[sbank], func=AF.Exp, scale=SCALE)
                    if tt >= 4 * m:
                        O("dve", "tensor_tensor", [ptk, "dmask4"], [ptk], out=pt, in0=pt, in1=dmask4[:, tt - 4 * m, :], op=ALU.mult)

                def pv(tt=tt, ntt=ntt, pt=pt, ptk=ptk, ACC=ACC):
                    for hh in range(4):
                        ACC(RS[hh], 129, pt[:, hh * 128:(hh + 1) * 128], Vs1[:, tt, :], tt == ntt - 1, [ptk, "Vs1", "Vs1o"])

                st["qk"] = qk; st["em"] = em; st["pv"] = pv
                if tt == ntt - 1:
                    def post(g=g, m=m):
                        for hh in range(4):
                            gcol = (4 * g + hh) * 3
                            for (reg, gi, nm) in ((RS[hh], 1, "fs"), (RW[hh], 2, "fw")):
                                bk, off = reg
                                col = 16 + hh * 2 + (gi - 1)
                                O("dve", "reciprocal", [("ps", bk)], [(nm, hh)], out=sm[:, col:col + 1], in_=ps[bk][:, off + 128:off + 129])
                                O("dve", "tensor_tensor", [(nm, hh), "gates"], [(nm, hh)], out=sm[:, col:col + 1], in0=sm[:, col:col + 1], in1=gates[:, m, gcol + gi:gcol + gi + 1], op=ALU.mult)
                                O("dve", "scalar_tensor_tensor", [("ps", bk), (nm, hh), ("onsa", hh)], [("onsa", hh)], out=onsa[:, hh, :], in0=ps[bk][:, off:off + 128],
                                  scalar=sm[:, col:col + 1], in1=onsa[:, hh, :], op0=ALU.mult, op1=ALU.add)
                        COPY("act", onb, onsa, [("onsa", hh) for hh in range(4)], ["onb"])

                    def post_pe(g=g, m=m, qsl=qsl):
                        tb, _, _ = nxt_bufs()
                        for hh in range(4):
                            TR(psb[tb][:, hh * 128:(hh + 1) * 128], onb[:, hh, :], ["onb"], [("ps", tb)])
                        COPY("dve", oTg[:, :, qsl], psb[tb][:, 0:512].rearrange("p (c k) -> p c k", k=128), [("ps", tb)], ["oTg"])
                        if m == NQT - 1:
                            DM("oTgst", oT_s[8 + 4 * g:12 + 4 * g].rearrange("i p t -> p i t"), oTg, ["oTg"], [("oT_s", "n", g)])
                    st["post"] = post; st["post_pe"] = post_pe
                steps.append(st)
    run_pipeline(steps)
    S.barrier()
    if stop_after == "C":
        S.emit(); return nc

    def layernorm(z, gam, bet, outt, sm_, keyz, keyo, rd, tag=0):
        o = 8 * tag
        k = lambda n: (n, tag)
        jk = lnjunk[tag] if isinstance(lnjunk, list) else lnjunk
        O("act", "activation", [keyz], [k("lnjunk"), k("ln_s1")], out=jk, in_=z, func=AF.Copy, accum_out=sm_[:, o + 0:o + 1])
        O("act", "activation", [keyz], [k("lnjunk"), k("ln_s2")], out=jk, in_=z, func=AF.Square, accum_out=sm_[:, o + 1:o + 2])
        O("dve", "tensor_scalar", [k("ln_s1")], [k("ln_mu")], out=sm_[:, o + 2:o + 3], in0=sm_[:, o + 0:o + 1], scalar1=1.0 / D, scalar2=None, op0=ALU.mult)
        O("dve", "tensor_tensor", [k("ln_mu")], [k("ln_m2")], out=sm_[:, o + 3:o + 4], in0=sm_[:, o + 2:o + 3], in1=sm_[:, o + 2:o + 3], op=ALU.mult)
        O("dve", "scalar_tensor_tensor", [k("ln_s2"), k("ln_m2")], [k("ln_var")], out=sm_[:, o + 4:o + 5], in0=sm_[:, o + 1:o + 2], scalar=1.0 / D, in1=sm_[:, o + 3:o + 4],
          op0=ALU.mult, op1=ALU.subtract)
        O("act", "activation", [k("ln_var"), "ln_eps"], [k("ln_var")], out=sm_[:, o + 4:o + 5], in_=sm_[:, o + 4:o + 5], func=AF.Ln, bias=sm_[:, o + 7:o + 8])
        O("act", "activation", [k("ln_var")], [k("ln_rstd")], out=sm_[:, o + 5:o + 6], in_=sm_[:, o + 4:o + 5], func=AF.Exp, scale=-0.5)
        O("dve", "tensor_scalar", [keyz, k("ln_mu"), k("ln_rstd")], [keyz], out=z, in0=z, scalar1=sm_[:, o + 2:o + 3], scalar2=sm_[:, o + 5:o + 6], op0=ALU.subtract, op1=ALU.mult)
        O("dve", "tensor_tensor", [keyz] + rd, [keyz], out=z, in0=z, in1=gam, op=ALU.mult)
        O("dve", "tensor_tensor", [keyz] + rd, [keyo], out=outt, in0=z, in1=bet, op=ALU.add)

    A.reset()
    Wo = A.get([16, D], BF16)
    wost = [A.get([D], F32) for _ in range(2)]
    for c in range(16):
        s = c % 2
        DM("wost%d" % s, wost[s], w_out[c * 128:(c + 1) * 128, :], [], [("wost", s)])
        COPY(cast_eng(), Wo[:, c, :], wost[s], [("wost", s)], ["Wo"])
    g1 = A.get([D], F32); be1 = A.get([D], F32)
    DM("c11", g1, ln1_g.partition_broadcast(128), [], ["lnp"]); DM("c12", be1, ln1_b.partition_broadcast(128), [], ["lnp"])
    Wrf = A.get([16, 36], F32); Wr = A.get([16, 36], BF16)
    DM("c13", Wrf[:, :, 0:4], r_group.rearrange("(c p) g -> p c g", p=128), [], ["Wrf"], allow_slow_non_contiguous=True)
    DM("c14", Wrf[:, :, 4:36], r_expert.rearrange("(c p) g -> p c g", p=128), [], ["Wrf"], allow_slow_non_contiguous=True)
    COPY("dve", Wr, Wrf, ["Wrf"], ["Wr"])
    oTt = [A.get([16, 128], BF16) for _ in range(2)]
    xt = [A.get([D], F32) for _ in range(2)]
    z = [A.get([D], F32) for _ in range(2)]
    x1 = [A.get([D], F32) for _ in range(2)]
    x1b = A.get([D], BF16)
    x1Tt = [A.get([16, 128], BF16) for _ in range(2)]
    lnjunk = A.get([D], BF16)
    sm = A.get([16], F32)
    lg = A.get([36], F32); rt = A.get([160], F32); dwb = A.get([32], BF16)
    O("dve", "memset", [], ["ln_eps"], sm[:, 7:8], EPS)
    O("dve", "memset", [], ["ln_eps"], sm[:, 15:16], EPS)
    def e1_stage1(m):
        s = m % 2
        DM("oTt%d" % s, oTt[s], oT_s[:, :, m * 128:(m + 1) * 128].rearrange("i p t -> p i t"),
           [("oT_s", "d", h) for h in range(4)] + [("oT_s", "n", g) for g in range(2)], [("oTt", s)])
        DM("xt%d" % s, xt[s], xo[m * 128:(m + 1) * 128, :], [], [("xt", s)])
        for nb in range(4):
            for c in range(16):
                MM(ps[nb], oTt[s][:, c, :], Wo[:, c, nb * 512:(nb + 1) * 512], c == 0, c == 15, [("oTt", s), "Wo"], [("ps", nb)])
            O("dve", "scalar_tensor_tensor", [("xt", s), ("ps", nb)], [("z", s)], out=z[s][:, nb * 512:(nb + 1) * 512], in0=xt[s][:, nb * 512:(nb + 1) * 512],
              scalar=DN_ALPHA, in1=ps[nb], op0=ALU.mult, op1=ALU.add)
    def e1_stage2(m):
        s = m % 2
        layernorm(z[s], g1, be1, x1[s], sm, ("z", s), ("x1", s), ["lnp"], tag=s)
        DM("x1st%d" % s, x1_s[m * 128:(m + 1) * 128, :], x1[s], [("x1", s)], [("x1_s", m)])
        COPY("act", x1b, x1[s], [("x1", s)], ["x1b"])
        for hb in range(2):
            bank = 4 + hb
            for j in range(8):
                dc = hb * 8 + j
                TR(psb[bank][:, j * 128:(j + 1) * 128], x1b[:, dc * 128:(dc + 1) * 128], ["x1b"], [("ps", bank)])
            COPY("dve" if hb == 0 else "act", x1Tt[s][:, hb * 8:(hb + 1) * 8, :], psb[bank].rearrange("p (j k) -> p j k", k=128), [("ps", bank)], [("x1Tt", s)])
        DM("x1Tst%d" % s, x1T_s[:, :, m * 128:(m + 1) * 128].rearrange("i p t -> p i t"), x1Tt[s], [("x1Tt", s)], [("x1T_s", m)])
        for dc in range(16):
            MM(ps[6][:, 0:36], x1Tt[s][:, dc, :], Wr[:, dc, :], dc == 0, dc == 15, [("x1Tt", s), "Wr"], [("ps", 6)])
        COPY("dve", lg, ps[6][:, 0:36], [("ps", 6)], ["lg"])
        R = ["rt"]
        gmx, ngmx, gsum, pg, gm = rt[:, 0:1], rt[:, 1:2], rt[:, 2:3], rt[:, 4:8], rt[:, 8:12]
        em, ee, mk, em2 = rt[:, 16:48], rt[:, 48:80], rt[:, 80:112], rt[:, 112:144]
        m1, nm1, m2, den, scw = rt[:, 144:145], rt[:, 145:146], rt[:, 146:147], rt[:, 147:148], rt[:, 148:149]
        O("dve", "reduce_max", ["lg"], R, out=gmx, in_=lg[:, 0:4], axis=AX.X)
        O("dve", "tensor_scalar", R, R, out=ngmx, in0=gmx, scalar1=-1.0, scalar2=None, op0=ALU.mult)
        O("dve", "tensor_scalar", ["lg"] + R, R, out=gm, in0=lg[:, 0:4], scalar1=gmx, scalar2=None, op0=ALU.is_ge)
        O("act", "activation", ["lg"] + R, R, out=pg, in_=lg[:, 0:4], func=AF.Exp, bias=ngmx, accum_out=gsum)
        O("dve", "tensor_scalar", R, R, out=pg, in0=gm, scalar1=1.0, scalar2=1e30, op0=ALU.subtract, op1=ALU.mult)
        for gg in range(4):
            O("dve", "tensor_scalar", ["lg"] + R, R, out=em[:, gg * 8:(gg + 1) * 8], in0=lg[:, 4 + gg * 8:12 + gg * 8], scalar1=pg[:, gg:gg + 1], scalar2=None, op0=ALU.add)
        O("dve", "reduce_max", R, R, out=m1, in_=em, axis=AX.X)
        O("dve", "tensor_scalar", R, R, out=nm1, in0=m1, scalar1=-1.0, scalar2=None, op0=ALU.mult)
        O("act", "activation", R, R, out=ee, in_=em, func=AF.Exp, bias=nm1)
        O("dve", "tensor_scalar", R, R, out=mk, in0=em, scalar1=m1, scalar2=None, op0=ALU.is_ge)
        O("dve", "scalar_tensor_tensor", R, R, out=em2, in0=mk, scalar=-1e30, in1=em, op0=ALU.mult, op1=ALU.add)
        O("dve", "reduce_max", R, R, out=m2, in_=em2, axis=AX.X)
        O("dve", "tensor_scalar", R, R, out=mk, in0=em, scalar1=m2, scalar2=None, op0=ALU.is_ge)
        O("dve", "tensor_tensor", R, R, out=ee, in0=ee, in1=mk, op=ALU.mult)
        O("dve", "reduce_sum", R, R, out=den, in_=ee, axis=AX.X)
        O("dve", "tensor_tensor", R, R, out=den, in0=den, in1=gsum, op=ALU.mult)
        O("dve", "reciprocal", R, R, out=scw, in_=den)
        O("dve", "tensor_scalar", R, ["dwb"], out=dwb, in0=ee, scalar1=scw, scalar2=None, op0=ALU.mult)
        TR(psb[7][0:32, 0:128], dwb, ["dwb"], [("ps", 7)])
        COPY("act", dwT[:, m * 128:(m + 1) * 128], psb[7][0:32, 0:128], [("ps", 7)], ["dwT"])
    e1_stage1(0)
    for m in range(NQT):
        if m + 1 < NQT:
            e1_stage1(m + 1)
        e1_stage2(m)
    S.barrier()
    if stop_after == "E1":
        S.emit(); return nc

    A.reset()
    x1T = A.get([16, 1024], BF16)
    yacc = A.get([8, D], F32)
    hTm = A.get([4, 1024], BF16)
    wb = A.get([1024], F32)
    sele = A.get([32, 128], BF16, parts=32)
    sgt = [A.get([512], F32) for _ in range(2)]; tt_ = [A.get([512], F32) for _ in range(2)]
    sm = A.get([16], F32)
    ov0 = A.off
    wgst = [A.get([16, 128], F32)]; wust = [A.get([16, 128], F32)]
    wgb = [A.get([16, 128], BF16) for _ in range(2)]; wub = [A.get([16, 128], BF16) for _ in range(2)]
    wdst = [A.get([4, 512], F32) for _ in range(2)]; wdb = [A.get([4, 512], BF16) for _ in range(2)]
    A.off = ov0
    g2 = A.get([D], F32); be2 = A.get([D], F32)
    x1r = [A.get([D], F32) for _ in range(2)]; outt = [A.get([D], F32) for _ in range(2)]
    lnjunk = [A.get([D], BF16) for _ in range(2)]
    DM("c15", sele, c_sele, [], ["sele"])
    O("dve", "memset", [], ["ln_eps"], sm[:, 7:8], EPS)
    O("dve", "memset", [], ["ln_eps"], sm[:, 15:16], EPS)
    wi = 0; di = 0; cntd = {"i": 0}
    for hb in range(2):
        if hb == 0:
            DM("x1Tld", x1T, x1T_s[:, :, 0:1024].rearrange("i p t -> p i t"), [("x1T_s", m) for m in range(16)], ["x1T"])
        chunks = []
        for e in range(32):
            for fc in range(4):
                s_ = wi % 2; wi += 1

                def load(e=e, fc=fc):
                    DM("wgst0", wgst[0], wg[e, :, fc * 128:(fc + 1) * 128].rearrange("(c p) f -> p c f", p=128), [], [("wgst", 0)])
                    DM("wust0", wust[0], wu[e, :, fc * 128:(fc + 1) * 128].rearrange("(c p) f -> p c f", p=128), [], [("wust", 0)])

                def cast(s_=s_):
                    COPY("dve", wgb[s_], wgst[0], [("wgst", 0)], [("wgb", s_)])
                    COPY("act", wub[s_], wust[0], [("wust", 0)], [("wub", s_)])

                def compute(e=e, fc=fc, s_=s_, hb=hb):
                    if fc == 0:
                        for blk in range(2):
                            MM(ps[4 + blk], sele[:, e, :], dwT[:, hb * 1024 + blk * 512: hb * 1024 + (blk + 1) * 512], True, True, ["sele", "dwT"], [("ps", 4 + blk)])
                            COPY("act", wb[:, blk * 512:(blk + 1) * 512], ps[4 + blk], [("ps", 4 + blk)], ["wb"])
                    for blk in range(2):
                        for dc in range(16):
                            MM(ps[blk], wgb[s_][:, dc, :], x1T[:, dc, blk * 512:(blk + 1) * 512], dc == 0, dc == 15, [("wgb", s_), "x1T"], [("ps", blk)])
                        for dc in range(16):
                            MM(ps[2 + blk], wub[s_][:, dc, :], x1T[:, dc, blk * 512:(blk + 1) * 512], dc == 0, dc == 15, [("wub", s_), "x1T"], [("ps", 2 + blk)])
                        O("act", "activation", [("ps", blk)], [("sgt", blk)], out=sgt[blk], in_=ps[blk], func=AF.Silu)
                        O("dve", "tensor_tensor", [("ps", 2 + blk), "wb"], [("tt_", blk)], out=tt_[blk], in0=ps[2 + blk], in1=wb[:, blk * 512:(blk + 1) * 512], op=ALU.mult)
                        O("pool", "tensor_tensor", [("sgt", blk), ("tt_", blk)], ["hTm"], out=hTm[:, fc, blk * 512:(blk + 1) * 512], in0=sgt[blk], in1=tt_[blk], op=ALU.mult)
                chunks.append((load, cast, compute))
            for dbk in range(4):
                s_ = di % 2; di += 1

                def load(e=e, dbk=dbk, s_=s_):
                    DM("wdst%d" % s_, wdst[s_], wd[e, :, dbk * 512:(dbk + 1) * 512].rearrange("(c p) d -> p c d", p=128), [], [("wdst", s_)])

                def cast(dbk=dbk, s_=s_):
                    COPY("act" if dbk % 2 == 0 else "dve", wdb[s_], wdst[s_], [("wdst", s_)], [("wdb", s_)])

                def compute(e=e, dbk=dbk, s_=s_):
                    for t8 in range(8):
                        bank = 4 + cntd["i"] % 4; cntd["i"] += 1
                        for fc in range(4):
                            MM(ps[bank], hTm[:, fc, t8 * 128:(t8 + 1) * 128], wdb[s_][:, fc, :], fc == 0, fc == 3, ["hTm", ("wdb", s_)], [("ps", bank)])
                        ysl = yacc[:, t8, dbk * 512:(dbk + 1) * 512]
                        if e == 0:
                            COPY("dve", ysl, ps[bank], [("ps", bank)], [("yacc", t8)])
                        else:
                            O("dve", "tensor_tensor", [("ps", bank), ("yacc", t8)], [("yacc", t8)], out=ysl, in0=ps[bank], in1=ysl, op=ALU.add)
                chunks.append((load, cast, compute))
        chunks[0][0](); chunks[0][1]()
        for k in range(len(chunks)):
            if k + 1 < len(chunks):
                chunks[k + 1][0](); chunks[k + 1][1]()
            chunks[k][2]()
        S.barrier()
        if hb == 0:
            DM("x1Tld", x1T, x1T_s[:, :, 1024:2048].rearrange("i p t -> p i t"), [("x1T_s", m) for m in range(16)], ["x1T"])
        DM("c11", g2, ln2_g.partition_broadcast(128), [], ["lnp"]); DM("c12", be2, ln2_b.partition_broadcast(128), [], ["lnp"])
        for t8 in range(8):
            m = hb * 8 + t8
            p2 = t8 % 2
            DM("x1r%d" % p2, x1r[p2], x1_s[m * 128:(m + 1) * 128, :], [("x1_s", m)], [("x1r", p2)])
            O("dve", "scalar_tensor_tensor", [("x1r", p2), ("yacc", t8)], [("yacc", t8)], out=yacc[:, t8, :], in0=x1r[p2], scalar=DN_ALPHA, in1=yacc[:, t8, :], op0=ALU.mult, op1=ALU.add)
            layernorm(yacc[:, t8, :], g2, be2, outt[p2], sm, ("yacc", t8), ("outt", p2), ["lnp"], tag=p2)
            DM("outst%d" % p2, out[m * 128:(m + 1) * 128, :], outt[p2], [("outt", p2)], [("out", m)])
        S.barrier()
    S.emit()
    return nc


def _constants(r):
    bf = ml_dtypes.bfloat16
    c = {}
    inv_freq = (np.float32(500000.0) ** (-np.arange(0, 32, 2, dtype=np.float32) / np.float32(32))).astype(np.float32)
    ang = (np.arange(T, dtype=np.float32)[:, None] * inv_freq[None, :]).astype(np.float32)
    cs, sn = np.cos(ang).astype(np.float32), np.sin(ang).astype(np.float32)
    C = np.concatenate([cs.T, cs.T], axis=0)
    Sg = np.concatenate([-sn.T, sn.T], axis=0)
    rope_k = np.stack([C, Sg], axis=0).astype(np.float32)
    own = (np.arange(16)[:, None] * 4 + r) * 128 + np.arange(128)[None, :]
    own = own.reshape(-1)
    c["rope_k"] = np.ascontiguousarray(rope_k)
    c["rope_q"] = np.ascontiguousarray(rope_k[:, :, own])
    c["c_ident"] = np.eye(128, dtype=np.float32).astype(bf)
    rsw = np.zeros((32, 32), np.float32)
    for m in range(32):
        rsw[(m + 16) % 32, m] = 1.0
    c["c_rsw"] = rsw.astype(bf)
    ki = np.arange(128)[:, None]; qi = np.arange(128)[None, :]
    dm = np.zeros((128, 4, 128), np.float32)
    for i in range(4):
        dm[:, i, :] = 1.0 if i < r else ((ki <= qi).astype(np.float32) if i == r else 0.0)
    c["c_dmask4"] = np.tile(dm, (1, 1, 4)).astype(bf)
    cm = np.zeros((128, 5, 128), np.float32)
    for i in range(5):
        cm[:, i, :] = (16 * ki + 31 <= 128 * (4 * i + r) + qi).astype(np.float32)
    c["c_cmask4"] = np.tile(cm, (1, 1, 4)).astype(bf)
    wm = np.zeros((128, 8, 128), np.float32)
    for j in range(8):
        d = r + 4 - j
        if d == 4:
            wm[:, j, :] = (ki > qi)
        elif 1 <= d <= 3:
            wm[:, j, :] = 1.0
        elif d == 0:
            wm[:, j, :] = (ki <= qi)
    c["c_wbias4"] = ((np.tile(wm, (1, 1, 4)) - 1.0) * (-NEG_BIAS)).astype(bf)
    svm = np.zeros((128, 16, 128), np.float32); sfb = np.zeros((128, 16, 128), np.float32)
    s_idx = np.arange(128)[None, :]
    for m in range(16):
        qpos = (4 * m + r) * 128 + np.arange(128)[:, None]
        cur = qpos // 64
        valid = s_idx <= cur
        forced = (s_idx == 0) | (s_idx == cur) | (s_idx == cur - 1)
        svm[:, m, :] = (valid & ~forced)
        sfb[:, m, :] = np.where(valid & forced, 1e30, np.where(valid, 0.0, -1e30))
    c["c_selvm"] = svm; c["c_selfb"] = sfb
    ov = np.zeros((512, 129), np.float32)
    ci = np.arange(511)[:, None] * 16; sj = np.arange(128)[None, :] * 64
    ov[:511, :128] = ((ci < sj + 64) & (ci + 32 > sj))
    ov[:, 128] = 1.0
    c["c_ovl1"] = ov.astype(bf)
    E = (np.arange(T)[None, :] // 64 == np.arange(128)[:, None]).astype(np.float32)
    c["c_E"] = E.astype(bf)
    se = np.zeros((32, 32, 128), np.float32)
    for e in range(32):
        se[e, e, :] = 1.0
    c["c_sele"] = se.astype(bf)
    return c


_NC_CACHE = {}


def kernel(x, w_in, diff_lambda, diff_subln_g, cmp_pos, cmp_w1, cmp_b1, cmp_w2, cmp_b2, w_out,
           ln1_g, ln1_b, router_group, router_expert, expert_w_gate, expert_w_up, expert_w_down,
           ln2_g, ln2_b, _stop_after=None, _debug=False):
    f = lambda a: np.ascontiguousarray(np.asarray(a, dtype=np.float32))
    x = f(x)
    shared = dict(
        w_in=f(w_in)[0], w_out=f(w_out)[0], diff_lambda=f(diff_lambda)[0], diff_subln_g=f(diff_subln_g)[0],
        cmp_pos=f(cmp_pos)[0], cmp_w1=f(cmp_w1)[0], cmp_b1=f(cmp_b1)[0], cmp_w2=f(cmp_w2)[0], cmp_b2=f(cmp_b2)[0],
        ln1_g=f(ln1_g)[0], ln1_b=f(ln1_b)[0], ln2_g=f(ln2_g)[0], ln2_b=f(ln2_b)[0],
        router_group=f(router_group)[0], router_expert=f(router_expert)[0],
        expert_w_gate=f(expert_w_gate)[0], expert_w_up=f(expert_w_up)[0], expert_w_down=f(expert_w_down)[0])
    consts = [_constants(r) for r in range(4)]
    in_maps = []
    for c in range(8):
        b, r = c // 4, c % 4
        xbb = x[b]
        xo = np.ascontiguousarray(xbb.reshape(64, 128, D)[r::4].reshape(TO, D))
        d = dict(shared)
        if _stop_after in ("A1", "A2", "A", "B", "C0", "C", "E1"):
            for kk in ("expert_w_gate", "expert_w_up", "expert_w_down"):
                d.pop(kk)
        d.update(consts[r])
        d["xb"] = xbb
        d["xo"] = xo
        in_maps.append(d)
    key = (_stop_after, _debug)
    nc = build_nc(stop_after=_stop_after, debug=_debug)
    res = run_bass_kernel_spmd(nc, in_maps, core_ids=list(range(8)))
    if _debug:
        return res
    outp = np.zeros((2, T, D), np.float32)
    for c in range(8):
        b, r = c // 4, c % 4
        outp[b].reshape(64, 128, D)[r::4] = res.results[c]["out"].reshape(16, 128, D)
    return outp
```
